# Optimizing a Trainium2 kernel written in Bass

```python
import math
import jax, jax.numpy as jnp
from jax import lax
import numpy as np

D_MODEL = 1024
BATCH = 8
SEQ = 4096
DEPTH = 2

D_MIX = D_MODEL
RWKV_WIDTH = D_MIX // 4
RWKV_HEAD_DIM = 64
RWKV_HEADS = RWKV_WIDTH // RWKV_HEAD_DIM
RWKV_DECAY_RANK = 64
RWKV_A_RANK = 64
RWKV_V_RANK = 32
RWKV_GATE_RANK = 128
RWKV_GN_EPS = 64e-5
S5_WIDTH = D_MIX // 4
S5_GROUP_DIM = 16
S5_GROUPS = S5_WIDTH // S5_GROUP_DIM
S5_STATE = 64
S5_DT_MIN = 0.001
S5_DT_MAX = 0.1
GDN_WIDTH = D_MIX - RWKV_WIDTH - S5_WIDTH
GDN_HEAD_DIM = 128
GDN_HEADS = GDN_WIDTH // GDN_HEAD_DIM
GDN_CONV = 4
GDN_CHUNK = 64
D_FF = 2816
RMS_EPS = 1e-6
L2_EPS = 1e-12

RWKV_SPLITS = (RWKV_WIDTH, RWKV_WIDTH, RWKV_WIDTH, RWKV_DECAY_RANK, RWKV_A_RANK, RWKV_GATE_RANK)
RWKV_COLS = 3 * RWKV_WIDTH + RWKV_DECAY_RANK + RWKV_A_RANK + RWKV_GATE_RANK
S5_COLS = S5_WIDTH
GDN_COLS = 4 * GDN_WIDTH + 2 * GDN_HEADS
P_MAIN = RWKV_COLS + S5_COLS + GDN_COLS

kernel_name = 'hybrid_rwkv7_s5_gdn_macaron_sandwich'


def _split(z, sizes):
    idx = np.cumsum(sizes)[:-1].tolist()
    return jnp.split(z, idx, axis=-1)


def rms_norm(z, gain):
    zf = z.astype(jnp.float32)
    zf = zf * lax.rsqrt(jnp.mean(zf * zf, axis=-1, keepdims=True) + RMS_EPS)
    return (zf * gain.astype(jnp.float32)).astype(z.dtype)


def l2_normalize(z):
    return z / jnp.maximum(jnp.sqrt(jnp.sum(z * z, axis=-1, keepdims=True)), L2_EPS)


def swiglu(z, w1, w3, w2):
    return (jax.nn.silu(z @ w1) * (z @ w3)) @ w2


def token_shift(z):
    return jnp.pad(z[:, :-1], ((0, 0), (1, 0), (0, 0)))


def rwkv7_time_mix(cols, vres_cols, v_first, mu, w0, w_w2, a0, w_a2, w_g2, k_k, k_a, r_k, ln_w, ln_b, v0, w_v2):
    bsz, t_len, _ = cols.shape
    hn = (RWKV_HEADS, RWKV_HEAD_DIM)
    z = cols + mu * (token_shift(cols) - cols)
    r, k, v, wd, ad, gd = _split(z, RWKV_SPLITS)
    w_log = -jax.nn.softplus(-(w0 + jnp.tanh(wd) @ w_w2)) - 0.5
    decay = jnp.exp(-jnp.exp(w_log))
    a = jax.nn.sigmoid(a0 + ad @ w_a2)
    g = jax.nn.sigmoid(gd) @ w_g2
    if v_first is None:
        v_first = v
    else:
        v = v + (v_first - v) * jax.nn.sigmoid(v0 + vres_cols @ w_v2)
    heads = lambda t: t.reshape(bsz, t_len, RWKV_HEADS, RWKV_HEAD_DIM)
    r, k, v, decay, a = heads(r), heads(k), heads(v), heads(decay), heads(a)
    kk = l2_normalize(k * k_k.reshape(hn))
    k = k * (1.0 + (a - 1.0) * k_a.reshape(hn))

    def step(state, inp):
        r_t, w_t, k_t, v_t, nkk_t, b_t = inp
        sa = jnp.einsum('bhvk,bhk->bhv', state, nkk_t)
        state = (state * w_t[:, :, None, :] + sa[..., None] * b_t[:, :, None, :]
                 + v_t[..., None] * k_t[:, :, None, :])
        return state, jnp.einsum('bhvk,bhk->bhv', state, r_t)

    tm = lambda t: jnp.moveaxis(t, 1, 0)
    s0 = jnp.zeros((bsz, RWKV_HEADS, RWKV_HEAD_DIM, RWKV_HEAD_DIM), jnp.float32)
    _, y = lax.scan(step, s0, (tm(r), tm(decay), tm(k), tm(v), tm(-kk), tm(kk * a)))
    y = jnp.moveaxis(y, 0, 1)
    mean = jnp.mean(y, axis=-1, keepdims=True)
    var = jnp.mean(jnp.square(y - mean), axis=-1, keepdims=True)
    y = (y - mean) * lax.rsqrt(var + RWKV_GN_EPS) * ln_w.reshape(hn) + ln_b.reshape(hn)
    y = y + jnp.sum(r * k * r_k, axis=-1, keepdims=True) * v
    return y.reshape(bsz, t_len, RWKV_WIDTH) * g, v_first


def _complex_linear_combine(e1, e2):
    a1r, a1i, b1r, b1i = e1
    a2r, a2i, b2r, b2i = e2
    return (a2r * a1r - a2i * a1i, a2r * a1i + a2i * a1r,
            a2r * b1r - a2i * b1i + b2r, a2r * b1i + a2i * b1r + b2i)


def s5_mix(u, a_re, a_im, log_dt, b_re, b_im, c_re, c_im, d_skip, w_glu, b_glu):
    bsz, t_len, _ = u.shape
    a_re, a_im, log_dt = a_re.astype(jnp.float32), a_im.astype(jnp.float32), log_dt.astype(jnp.float32)
    ug = u.reshape(bsz, t_len, S5_GROUPS, S5_GROUP_DIM)
    dt = jnp.exp(log_dt)[:, None]
    mag = jnp.exp(dt * a_re)
    ang = dt * a_im
    abar_re, abar_im = mag * jnp.cos(ang), mag * jnp.sin(ang)
    den = a_re * a_re + a_im * a_im
    num_re = abar_re - 1.0
    coef_re = (num_re * a_re + abar_im * a_im) / den
    coef_im = (abar_im * a_re - num_re * a_im) / den
    bbar_re = coef_re[..., None] * b_re - coef_im[..., None] * b_im
    bbar_im = coef_re[..., None] * b_im + coef_im[..., None] * b_re
    bu_re = jnp.einsum('btgh,gnh->btgn', ug, bbar_re)
    bu_im = jnp.einsum('btgh,gnh->btgn', ug, bbar_im)
    shape = (1, t_len, S5_GROUPS, S5_STATE)
    elems = (jnp.broadcast_to(abar_re, shape), jnp.broadcast_to(abar_im, shape), bu_re, bu_im)
    _, _, x_re, x_im = lax.associative_scan(_complex_linear_combine, elems, axis=1)
    y = (jnp.einsum('btgn,ghn->btgh', x_re, c_re) - jnp.einsum('btgn,ghn->btgh', x_im, c_im)
         + d_skip.reshape(S5_GROUPS, S5_GROUP_DIM) * ug)
    z = jax.nn.gelu(y.reshape(bsz, t_len, S5_WIDTH))
    return z * jax.nn.sigmoid(z @ w_glu + b_glu)


def causal_depthwise_conv(z, w):
    width, ch = w.shape
    return lax.conv_general_dilated(z, w.astype(z.dtype)[:, None, :], window_strides=(1,),
                                    padding=((width - 1, 0),), dimension_numbers=('NWC', 'WIO', 'NWC'),
                                    feature_group_count=ch)


def chunk_gated_delta_rule(q, k, v, g, beta):
    bsz, t_len, nh, dk = q.shape
    dv = v.shape[-1]
    n_chunks = t_len // GDN_CHUNK
    chunks = lambda t: jnp.moveaxis(t.reshape(bsz, n_chunks, GDN_CHUNK, nh, -1), 3, 1)
    q, k, v = chunks(q), chunks(k), chunks(v)
    g = chunks(g[..., None])[..., 0]
    beta = chunks(beta[..., None])[..., 0]
    gam = jnp.cumsum(g, axis=-1)
    causal = jnp.tril(jnp.ones((GDN_CHUNK, GDN_CHUNK), dtype=bool))
    strict = jnp.tril(jnp.ones((GDN_CHUNK, GDN_CHUNK), dtype=bool), k=-1)
    decay = jnp.exp(jnp.where(causal, gam[..., :, None] - gam[..., None, :], -jnp.inf))
    k_beta = k * beta[..., None]
    a_low = jnp.where(strict, jnp.einsum('bhncd,bhnsd->bhncs', k_beta, k) * decay, 0.0)
    rhs = jnp.concatenate([v * beta[..., None], k_beta * jnp.exp(gam)[..., None]], axis=-1)
    eye = jnp.eye(GDN_CHUNK, dtype=a_low.dtype)
    sol = lax.linalg.triangular_solve(a_low + eye, rhs, left_side=True, lower=True, unit_diagonal=True)
    u, w = sol[..., :dv], sol[..., dv:]
    attn = jnp.where(causal, jnp.einsum('bhncd,bhnsd->bhncs', q, k) * decay, 0.0)
    q_dec = q * jnp.exp(gam)[..., None]
    k_dec = k * jnp.exp(gam[..., -1:] - gam)[..., None]
    chunk_decay = jnp.exp(gam[..., -1])

    def step(state, inp):
        u_c, w_c, q_c, k_c, attn_c, dec_c = inp
        v_new = u_c - jnp.einsum('bhck,bhkv->bhcv', w_c, state)
        out = jnp.einsum('bhck,bhkv->bhcv', q_c, state) + jnp.einsum('bhcs,bhsv->bhcv', attn_c, v_new)
        state = state * dec_c[..., None, None] + jnp.einsum('bhck,bhcv->bhkv', k_c, v_new)
        return state, out

    along = lambda t: jnp.moveaxis(t, 2, 0)
    s0 = jnp.zeros((bsz, nh, dk, dv), jnp.float32)
    _, o = lax.scan(step, s0, (along(u), along(w), along(q_dec), along(k_dec), along(attn), along(chunk_decay)))
    o = jnp.moveaxis(o, 0, 2)
    return jnp.moveaxis(o, 1, 3).reshape(bsz, t_len, nh, dv)


def gated_deltanet_mix(cols, conv_w, a_log, dt_bias, norm_w):
    bsz, t_len, _ = cols.shape
    qkv, gate, beta_logit, alpha_logit = _split(cols, (3 * GDN_WIDTH, GDN_WIDTH, GDN_HEADS, GDN_HEADS))
    qkv = jax.nn.silu(causal_depthwise_conv(qkv, conv_w))
    heads = lambda t: t.reshape(bsz, t_len, GDN_HEADS, GDN_HEAD_DIM)
    q, k, v = [heads(t) for t in _split(qkv, (GDN_WIDTH, GDN_WIDTH, GDN_WIDTH))]
    q = l2_normalize(q) * (GDN_HEAD_DIM ** -0.5)
    k = l2_normalize(k)
    beta = jax.nn.sigmoid(beta_logit)
    g = -jnp.exp(a_log.astype(jnp.float32)) * jax.nn.softplus(alpha_logit + dt_bias)
    o = chunk_gated_delta_rule(q, k, v, g, beta)
    o = o * lax.rsqrt(jnp.mean(o * o, axis=-1, keepdims=True) + RMS_EPS) * norm_w
    o = o * jax.nn.silu(heads(gate))
    return o.reshape(bsz, t_len, GDN_WIDTH)


def setup_inputs(seed: int = 0) -> dict:
    key = jax.random.key(seed)
    ks = iter(jax.random.split(key, 64))
    f32 = jnp.float32

    def nrm(shape, scale):
        return scale * jax.random.normal(next(ks), shape, f32)

    def uni(shape, lo, hi):
        return jax.random.uniform(next(ks), shape, f32, lo, hi)

    L, LV = DEPTH, DEPTH - 1
    W = RWKV_WIDTH
    G, N, HC = S5_GROUPS, S5_STATE, S5_GROUP_DIM
    log_dt_lo, log_dt_hi = math.log(S5_DT_MIN), math.log(S5_DT_MAX)
    ramp = jnp.linspace(0.0, 1.0, W, dtype=f32) ** 0.85
    gdn_dt = jnp.exp(uni((L, GDN_HEADS), log_dt_lo, log_dt_hi))
    return {
        'x': nrm((BATCH, SEQ, D_MODEL), 1.0),
        'norm_gain': 1.0 + nrm((L, 6, D_MODEL), 0.02),
        'ffn_w1': nrm((L, 2, D_MODEL, D_FF), D_MODEL ** -0.5),
        'ffn_w3': nrm((L, 2, D_MODEL, D_FF), D_MODEL ** -0.5),
        'ffn_w2': nrm((L, 2, D_FF, D_MODEL), D_FF ** -0.5),
        'w_in': nrm((L, D_MODEL, P_MAIN), D_MODEL ** -0.5),
        'w_in_vres': nrm((LV, D_MODEL, RWKV_V_RANK), D_MODEL ** -0.5),
        'w_out': nrm((L, D_MIX, D_MODEL), D_MIX ** -0.5),
        'rwkv_mu': uni((L, RWKV_COLS), 0.0, 1.0),
        'rwkv_w0': -5.5 + 5.0 * ramp + nrm((L, W), 0.05),
        'rwkv_w_w2': nrm((L, RWKV_DECAY_RANK, W), 0.1 * RWKV_DECAY_RANK ** -0.5),
        'rwkv_a0': nrm((L, W), 0.1),
        'rwkv_w_a2': nrm((L, RWKV_A_RANK, W), 0.1 * RWKV_A_RANK ** -0.5),
        'rwkv_w_g2': nrm((L, RWKV_GATE_RANK, W), RWKV_GATE_RANK ** -0.5),
        'rwkv_k_k': 0.85 + nrm((L, W), 0.02),
        'rwkv_k_a': 1.0 + nrm((L, W), 0.02),
        'rwkv_r_k': -0.04 + nrm((L, RWKV_HEADS, RWKV_HEAD_DIM), 0.02),
        'rwkv_ln_w': 1.0 + nrm((L, W), 0.02),
        'rwkv_ln_b': nrm((L, W), 0.02),
        'rwkv_v0': 1.0 + nrm((LV, W), 0.1),
        'rwkv_w_v2': nrm((LV, RWKV_V_RANK, W), 0.1 * RWKV_V_RANK ** -0.5),
        's5_a_re': -0.5 + nrm((L, G, N), 0.01),
        's5_a_im': jnp.broadcast_to(jnp.pi * jnp.arange(N, dtype=f32), (L, G, N)),
        's5_log_dt': uni((L, G), log_dt_lo, log_dt_hi),
        's5_b_re': nrm((L, G, N, HC), (2 * HC) ** -0.5),
        's5_b_im': nrm((L, G, N, HC), (2 * HC) ** -0.5),
        's5_c_re': nrm((L, G, HC, N), (2 * N) ** -0.5),
        's5_c_im': nrm((L, G, HC, N), (2 * N) ** -0.5),
        's5_d': nrm((L, S5_WIDTH), 1.0),
        's5_w_glu': nrm((L, S5_WIDTH, S5_WIDTH), S5_WIDTH ** -0.5),
        's5_b_glu': nrm((L, S5_WIDTH), 0.02),
        'gdn_conv_w': nrm((L, GDN_CONV, 3 * GDN_WIDTH), GDN_CONV ** -0.5),
        'gdn_a_log': jnp.log(uni((L, GDN_HEADS), 1.0, 16.0)),
        'gdn_dt_bias': gdn_dt + jnp.log(-jnp.expm1(-gdn_dt)),
        'gdn_norm_w': 1.0 + nrm((L, GDN_HEAD_DIM), 0.02),
    }


def reference(x, norm_gain, ffn_w1, ffn_w3, ffn_w2, w_in, w_in_vres, w_out,
              rwkv_mu, rwkv_w0, rwkv_w_w2, rwkv_a0, rwkv_w_a2, rwkv_w_g2, rwkv_k_k, rwkv_k_a,
              rwkv_r_k, rwkv_ln_w, rwkv_ln_b, rwkv_v0, rwkv_w_v2,
              s5_a_re, s5_a_im, s5_log_dt, s5_b_re, s5_b_im, s5_c_re, s5_c_im, s5_d, s5_w_glu, s5_b_glu,
              gdn_conv_w, gdn_a_log, gdn_dt_bias, gdn_norm_w):
    v_first = None
    for l in range(DEPTH):
        ff = swiglu(rms_norm(x, norm_gain[l, 0]), ffn_w1[l, 0], ffn_w3[l, 0], ffn_w2[l, 0])
        x = x + 0.5 * rms_norm(ff, norm_gain[l, 1])

        h = rms_norm(x, norm_gain[l, 2])
        w_proj = w_in[l] if l == 0 else jnp.concatenate([w_in[l], w_in_vres[l - 1]], axis=1)
        p = (h @ w_proj).astype(jnp.float32)
        rw_cols = p[..., :RWKV_COLS]
        s5_cols = p[..., RWKV_COLS:RWKV_COLS + S5_COLS]
        gdn_cols = p[..., RWKV_COLS + S5_COLS:P_MAIN]
        vres_cols = None if l == 0 else p[..., P_MAIN:]
        v0 = None if l == 0 else rwkv_v0[l - 1]
        w_v2 = None if l == 0 else rwkv_w_v2[l - 1]

        y_a, v_first = rwkv7_time_mix(rw_cols, vres_cols, v_first, rwkv_mu[l], rwkv_w0[l], rwkv_w_w2[l],
                                      rwkv_a0[l], rwkv_w_a2[l], rwkv_w_g2[l], rwkv_k_k[l], rwkv_k_a[l],
                                      rwkv_r_k[l], rwkv_ln_w[l], rwkv_ln_b[l], v0, w_v2)
        y_b = s5_mix(s5_cols, s5_a_re[l], s5_a_im[l], s5_log_dt[l], s5_b_re[l], s5_b_im[l],
                     s5_c_re[l], s5_c_im[l], s5_d[l], s5_w_glu[l], s5_b_glu[l])
        y_c = gated_deltanet_mix(gdn_cols, gdn_conv_w[l], gdn_a_log[l], gdn_dt_bias[l], gdn_norm_w[l])
        mixed = jnp.concatenate([y_a, y_b, y_c], axis=-1).astype(x.dtype) @ w_out[l]
        x = x + rms_norm(mixed, norm_gain[l, 3])

        ff = swiglu(rms_norm(x, norm_gain[l, 4]), ffn_w1[l, 1], ffn_w3[l, 1], ffn_w2[l, 1])
        x = x + 0.5 * rms_norm(ff, norm_gain[l, 5])
    return x
```

```python
import numpy as np
from contextlib import ExitStack as _ExitStack
import concourse.bass as bass
import concourse.mybir as mybir
from concourse.bass_utils import run_bass_kernel_spmd

F32 = mybir.dt.float32
BF16 = mybir.dt.bfloat16
AF = mybir.ActivationFunctionType
ALU = mybir.AluOpType

D = 1024
SEQ = 4096
DEPTH = 2
DFF = 2816
NF = DFF // 128
TT = 1024
NTT = SEQ // TT
RMS_EPS = 1e-6

CHUNKS = ([(i * 128, 128) for i in range(8)] + [(1024, 128), (1152, 128)]
          + [(1280 + i * 128, 128) for i in range(16)] + [(3328, 8), (0, 32)])
NCH = len(CHUNKS)
PO = {"mu": 0, "w0": 8, "a0": 10, "kk": 12, "ka": 14, "rk": 16, "lnw": 18, "lnb": 20, "v0": 22, "omk": 24,
      "conv": 26, "gnw": 74, "nA": 75, "dtb": 79, "bglu": 83}
NPAR = 88
NCONST = 3520


class Buf:
    __slots__ = ("name", "ap", "w", "r", "t", "excl")

    def __init__(self, name, ap, t=None, excl=False):
        self.name = name
        self.ap = ap
        self.t = t
        self.excl = excl
        self.w = None
        self.r = []


class VBuf:
    def __init__(self, parent, t):
        self.parent = parent
        self.t = t
        self.ap = None
        self.name = parent.name + "_v"

    @property
    def w(self):
        return self.parent.w

    @w.setter
    def w(self, v):
        self.parent.w = v

    @property
    def r(self):
        return self.parent.r

    @r.setter
    def r(self, v):
        self.parent.r = v


class _Shift:
    def __init__(self, t):
        self.t = t

    def __getitem__(self, idx):
        p, c, f = idx
        f = slice((f.start or 0) + 1, (f.stop if f.stop is not None else 512) + 1, f.step)
        return self.t[p, c, f]


class _Cut(Exception):
    pass


class ExitStack(_ExitStack):
    def __exit__(self, et, ev, tb):
        if et is _Cut:
            super().__exit__(None, None, None)
            return False
        return super().__exit__(et, ev, tb)


class Sched:
    ENG = ("pe", "act", "dve", "pool", "sp")
    NPOOL = 48

    def __init__(self, nc, es, needed=None):
        self.nc = nc
        self.es = es
        self.dry = needed is None
        self.needed = set() if needed is None else needed
        self.e = {"pe": nc.tensor, "act": nc.scalar, "dve": nc.vector,
                  "pool": nc.gpsimd, "sp": nc.sync}
        self.sem = {}
        self.cnt = {}
        self.seen = {k: {} for k in self.ENG}
        for k in self.ENG:
            self.sem[k] = es.enter_context(nc.semaphore("c_" + k))
            self.cnt[k] = 0
        self.ninst = 0
        self.rank = {}
        if not self.dry:
            for k in self.ENG:
                idxs = sorted(v for (kk, v) in self.needed if kk == k)
                self.rank[k] = {v: i + 1 for i, v in enumerate(idxs)}

    def dma_sem(self, key):
        if key not in self.sem:
            self.sem[key] = self.es.enter_context(self.nc.semaphore("d_" + key))
            self.cnt[key] = 0
        return key

    def _wait(self, eng, ev):
        if ev is None:
            return
        key, val = ev
        if self.seen[eng].get(key, 0) >= val:
            return
        self.seen[eng][key] = val
        self.ninst += 1
        if key in self.ENG:
            if self.dry:
                self.needed.add((key, val))
                return
            self.e[eng].wait_ge(self.sem[key], self.rank[key][val])
        else:
            if not self.dry:
                self.e[eng].wait_ge(self.sem[key], val)

    def _deps(self, eng, r, w):
        for b in r:
            ev = b.w
            if ev is not None:
                if ev[0] == eng and eng == "pe":
                    continue
                self._wait(eng, ev)
        for b in w:
            ev = b.w
            if ev is not None and not (ev[0] == eng and eng == "pe"):
                self._wait(eng, ev)
            for ev in b.r:
                if not (ev[0] == eng and eng == "pe"):
                    self._wait(eng, ev)

    def _record(self, ev, r, w):
        for b in r:
            b.r.append(ev)
            if len(b.r) > 24:
                last = {}
                for k, v in b.r:
                    if last.get(k, 0) < v:
                        last[k] = v
                b.r = list(last.items())
        for b in w:
            b.w = ev
            b.r = []

    def op(self, eng, fn, r=(), w=()):
        if any(getattr(b, "excl", False) for b in r):
            w = list(w) + [b for b in r if getattr(b, "excl", False)]
            r = [b for b in r if not getattr(b, "excl", False)]
        self._deps(eng, r, w)
        self.cnt[eng] += 1
        self.ninst += 1
        if not self.dry:
            ins = fn(self.e[eng])
            if (eng, self.cnt[eng]) in self.needed:
                ins.then_inc(self.sem[eng], 1)
        self._record((eng, self.cnt[eng]), r, w)

    def dma(self, key, out, in_, r=(), w=(), eng="sp", **kw):
        idx = getattr(self, "dma_rr", 0)
        self.dma_rr = idx + 1
        key = f"p{idx % self.NPOOL}"
        self.dma_sem(key)
        if self.cnt[key] > 0:
            self._wait(eng, (key, self.cnt[key]))
        self._deps(eng, r, w)
        self.cnt[key] += 16
        self.ninst += 1
        if not self.dry:
            ins = self.e[eng].dma_start(out=out, in_=in_, **kw)
            ins.then_inc(self.sem[key], 16)
        self._record((key, self.cnt[key]), r, w)

    def barrier(self):
        keys = list(self.cnt.keys())
        for eng in self.ENG:
            for k in keys:
                if k != eng and self.cnt[k] > 0:
                    self._wait(eng, (k, self.cnt[k]))

    def finish(self, out_keys):
        for k in list(self.cnt.keys()):
            if k not in self.ENG and self.cnt[k] > 0:
                self._wait("sp", (k, self.cnt[k]))


def _consts():
    c = np.zeros((128, NCONST), np.float32)
    p = np.arange(128)[:, None]
    f = np.arange(128)[None, :]
    c[:, 0:128] = np.eye(128, dtype=np.float32)
    c[:, 128:256] = 1.0
    c[:, 256] = RMS_EPS
    c[:, 257] = 64e-5
    c[:, 259] = 1.0
    c[:, 384:512] = (p <= f)
    c[:, 512:640] = (p > f)
    c[:, 640:768] = (p >= f)
    c[:, 768:896] = np.where(f <= p, 0.0, -30000.0)
    c[:, 896:1024] = ((p // 64) == (f // 64))
    rm = np.ones(512, np.float32)
    rm[::128] = 0.0
    c[:, 1024:1536] = rm[None, :]
    c[:, 1536:1664] = -(p > f).astype(np.float32)
    c[:, 1664:1792] = (p > f)
    c[:, 1792:1920] = (p > f)
    c[:, 1920:2048] = (p >= f)
    c[:, 2048:2176] = (p >= f)
    for k in range(4):
        c[:, 3008 + k * 128: 3008 + (k + 1) * 128] = -(p > f).astype(np.float32)
    cc = np.arange(256)[None, :]
    for kt in range(2):
        c[:, 2304 + kt * 256: 2304 + (kt + 1) * 256] = ((cc // 16) >= (kt * 8 + p // 16))
    tau = np.concatenate([15 - np.arange(16), -np.arange(16), np.arange(16), 1 + np.arange(16)]).astype(np.float32)
    c[:, 2816:2880] = tau[None, :]
    c[:, 2880:2944] = np.arange(64, dtype=np.float32)[None, :]
    m0 = np.ones(64, np.float32)
    m0[0] = 0.0
    c[:, 2944:3008] = m0[None, :]
    return c


class K:
    def __init__(self, stop=None, ntt=NTT, mix_enable=(1, 1, 1), dbg_mix=False, needed=None):
        self.needed = needed
        self.stop = stop
        self.ntt = ntt
        self.mix_enable = mix_enable
        self.dbg_mix = dbg_mix
        self.ps_i = 0
        self.cut = None
        self.nc = bass.Bass("TRN2", target_bir_lowering=False)
        self.es = ExitStack()

    def sb(self, name, shape, dt=F32, es=None):
        self.uid = getattr(self, "uid", 0) + 1
        t = (es or self.es).enter_context(self.nc.sbuf_tensor(f"{name}_u{self.uid}", shape, dt))
        return t

    def nb(self, name, shape, dt=F32, es=None):
        t = self.sb(name, shape, dt, es)
        return Buf(name, t[:], t)

    def mm(self, ps, out_ap, lhsT, rhs, r, start=True, stop=True):
        self.S.op("pe", lambda e: e.matmul(out_ap, lhsT, rhs, start=start, stop=stop), r=r, w=[ps])

    def tr(self, ps, out_ap, in_ap, r, f32=False):
        k = in_ap.shape[0]
        idn = (self.ident if f32 else self.identb)[0:k, 0:k]
        cb = self.Bcst if f32 else self.Bcstb
        self.S.op("pe", lambda e: e.transpose(out_ap, in_ap, idn), r=list(r) + [cb], w=[ps])

    def build(self):
        nc = self.nc
        es = self.es
        S = self.S = Sched(nc, es, self.needed)
        dram = lambda n, s, dt=F32, kind="ExternalInput": nc.dram_tensor(n, s, dt, kind=kind).ap()
        self.x_d = dram("x", [SEQ, D])
        self.y_d = dram("y", [SEQ, D], kind="ExternalOutput")
        self.gain_d = dram("gain", [128, DEPTH * 6 * 8])
        self.w1_d = dram("w1", [DEPTH * 2, D, DFF])
        self.w3_d = dram("w3", [DEPTH * 2, D, DFF])
        self.w2_d = dram("w2", [DEPTH * 2, DFF, D])
        self.const_d = dram("consts", [128, NCONST])
        self.wup_s = dram("wup_s", [DEPTH * 2 * NF, 128, 2048], BF16, kind="Internal")
        self.wdn_s = dram("wdn_s", [DEPTH * 2 * 8, 128, DFF], BF16, kind="Internal")

        self.cst = self.sb("cst", [128, NCONST])
        self.cstb = self.sb("cstb", [128, 2304], BF16)
        self.gain = self.sb("gain_sb", [128, DEPTH * 6 * 8])
        self.ghalf = self.sb("ghalf_sb", [128, DEPTH * 6 * 8])
        self.xT_t = self.sb("xT", [128, 8, TT])
        self.xT = [[Buf(f"xT{d}_{h}", self.xT_t[:, d, h * 512:(h + 1) * 512]) for h in range(2)]
                   for d in range(8)]
        self.wring_t = [self.sb(f"wring{i}", [128, 3072], BF16) for i in range(4)]
        self.wring = [Buf(f"wring{i}", self.wring_t[i]) for i in range(4)]
        self.wring_i = 0
        self.psum_t = [es.enter_context(nc.psum_tensor(f"ps{i}", [128, 512], F32)) for i in range(8)]
        self.ps = [Buf(f"ps{i}", self.psum_t[i][:], excl=True) for i in range(8)]
        self.Bcst = Buf("cst", self.cst)
        self.Bcstb = Buf("cstb", self.cstb)
        self.Bgain = Buf("gain", self.gain)

        S.dma("par", self.cst[:], self.const_d[:, :], w=[self.Bcst])
        S.dma("par", self.gain[:], self.gain_d[:, :], w=[self.Bgain])
        S.op("dve", lambda e: e.tensor_copy(self.cstb[:], self.cst[:, 0:2304]), r=[self.Bcst], w=[self.Bcstb])
        S.op("act", lambda e: e.mul(self.ghalf[:], self.gain[:], 0.5), r=[self.Bgain], w=[self.Bgain])
        self.ident = self.cst[:, 0:128]
        self.identb = self.cstb[:, 0:128]
        self.onesb = self.cstb[:, 128:256]
        self.eps_ap = self.cst[:, 256:257]

        self.mixer_setup()
        self.prologue()
        for tt in range(self.ntt):
            self.load_x(tt)
            done = False
            for l in range(DEPTH):
                for step in ("ffn0", "mix", "ffn1"):
                    if step == "ffn0":
                        self.ffn(l, 0)
                    elif step == "mix":
                        self.mixer(l, tt)
                    else:
                        self.ffn(l, 1)
                    if self.stop == (step, l):
                        done = True
                        break
                if done:
                    break
            self.store_x(tt)
        S.finish(["xst"])
        return nc

    def prologue(self):
        S = self.S
        jobs = []
        for lj in range(DEPTH * 2):
            for f in range(NF):
                for k, wd in enumerate((self.w1_d, self.w3_d)):
                    src = wd[lj][:, f * 128:(f + 1) * 128].rearrange("(dt p) c -> p dt c", p=128)
                    dst = self.wup_s[lj * NF + f][:, k * 1024:(k + 1) * 1024]
                    jobs.append((src, dst, 8, 128))
            for m in range(8):
                for (f0, nf) in ((0, 8), (8, 8), (16, 6)):
                    src = self.w2_d[lj][f0 * 128:(f0 + nf) * 128, m * 128:(m + 1) * 128].rearrange(
                        "(f p) c -> p f c", p=128)
                    dst = self.wdn_s[lj * 8 + m][:, f0 * 128:(f0 + nf) * 128]
                    jobs.append((src, dst, nf, 128))
        jobs += self.extra_convert_jobs()
        with ExitStack() as es:
            stg = [self.sb(f"pstg{i}", [128, 1024], F32, es) for i in range(4)]
            stgB = [Buf(f"pstg{i}", stg[i]) for i in range(4)]
            ob = [self.sb(f"pob{i}", [128, 1024], BF16, es) for i in range(4)]
            obB = [Buf(f"pob{i}", ob[i]) for i in range(4)]
            engs = ["dve", "act", "pool"]
            for i, (src, dst, a, b) in enumerate(jobs):
                s = i % 4
                n = a * b
                sv = stg[s][:, 0:n].rearrange("p (a b) -> p a b", b=b)
                S.dma(f"pst{s}", sv, src, w=[stgB[s]])
                eng = engs[i % 3]
                if eng == "act":
                    S.op("act", lambda e, o=ob[s], i_=stg[s], n=n: e.copy(o[:, 0:n], i_[:, 0:n]), r=[stgB[s]], w=[obB[s]])
                else:
                    S.op(eng, lambda e, o=ob[s], i_=stg[s], n=n: e.tensor_copy(o[:, 0:n], i_[:, 0:n]),
                         r=[stgB[s]], w=[obB[s]])
                S.dma(f"pob{s}", dst, ob[s][:, 0:n], r=[obB[s]])
            S.barrier()


    def load_x(self, tt):
        S = self.S
        with ExitStack() as es:
            xtok = self.sb("xtok", [128, 8, D], F32, es)
            Bx = [Buf(f"xtok{i}", xtok[:, i, :]) for i in range(8)]
            for i in range(8):
                S.dma("xld", xtok[:, i, :], self.x_d[tt * TT + i * 128: tt * TT + (i + 1) * 128, :], w=[Bx[i]])
            k = 0
            for d in range(8):
                for h in range(2):
                    ps = self.ps[k % 4]
                    for q in range(4):
                        i = h * 4 + q
                        S.op("pe", lambda e, ps=ps, q=q, i=i, d=d: e.transpose(
                            ps.ap[:, q * 128:(q + 1) * 128], xtok[:, i, d * 128:(d + 1) * 128], self.ident),
                            r=[Bx[i], self.Bcst], w=[ps])
                    dstB = self.xT[d][h]
                    if k % 2 == 0:
                        S.op("act", lambda e, a=dstB.ap, b=ps.ap: e.copy(a, b), r=[ps], w=[dstB])
                    else:
                        S.op("dve", lambda e, a=dstB.ap, b=ps.ap: e.tensor_copy(a, b), r=[ps], w=[dstB])
                    k += 1
            S.barrier()

    def store_x(self, tt):
        S = self.S
        with ExitStack() as es:
            xtok = self.sb("xtok_o", [128, 8, D], F32, es)
            Bx = [Buf(f"xtoko{i}", xtok[:, i, :]) for i in range(8)]
            k = 0
            for i in range(8):
                h, q = divmod(i, 4)
                for dh in range(2):
                    ps = self.ps[k % 4]
                    for dq in range(4):
                        d = dh * 4 + dq
                        S.op("pe", lambda e, ps=ps, dq=dq, d=d, h=h, q=q: e.transpose(
                            ps.ap[:, dq * 128:(dq + 1) * 128],
                            self.xT_t[:, d, h * 512 + q * 128: h * 512 + (q + 1) * 128], self.ident),
                            r=[self.xT[d][h], self.Bcst], w=[ps])
                    dst = xtok[:, i, dh * 512:(dh + 1) * 512]
                    if k % 2 == 0:
                        S.op("act", lambda e, a=dst, b=ps.ap: e.copy(a, b), r=[ps], w=[Bx[i]])
                    else:
                        S.op("dve", lambda e, a=dst, b=ps.ap: e.tensor_copy(a, b), r=[ps], w=[Bx[i]])
                    k += 1
                S.dma("xst", self.y_d[tt * TT + i * 128: tt * TT + (i + 1) * 128, :], xtok[:, i, :], r=[Bx[i]])
            S.barrier()

    def ring_load(self, src_ap, n):
        S = self.S
        slot = self.wring_i % 4
        self.wring_i += 1
        S.dma(f"wr{slot}", self.wring_t[slot][:, 0:n], src_ap, w=[self.wring[slot]])
        return slot

    def rstd_from_ps(self, ps, Brs):
        S = self.S
        S.op("act", lambda e, a=Brs.ap, ps=ps: e.activation(out=a, in_=ps.ap, func=AF.Sqrt,
                                                             scale=1.0 / D, bias=self.eps_ap),
             r=[ps, self.Bcst], w=[Brs])
        S.op("dve", lambda e, a=Brs.ap: e.reciprocal(a, a), r=[Brs], w=[Brs])

    def ffn(self, l, j):
        S = self.S
        lj = l * 2 + j
        gi_pre = (l * 6 + (0 if j == 0 else 4)) * 8
        gi_post = (l * 6 + (1 if j == 0 else 5)) * 8
        with ExitStack() as es:
            xn = self.sb("xn", [128, 8, TT], BF16, es)
            Bxn = [[Buf(f"xn{d}_{h}", xn[:, d, h * 512:(h + 1) * 512]) for h in range(2)] for d in range(8)]
            hid = self.sb("hid", [128, NF, TT], BF16, es)
            Bhid = [[Buf(f"hid{f}_{h}", hid[:, f, h * 512:(h + 1) * 512]) for h in range(2)] for f in range(NF)]
            ff = self.sb("ff", [128, 8, TT], F32, es)
            Bff = [[Buf(f"ff{d}_{h}", ff[:, d, h * 512:(h + 1) * 512]) for h in range(2)] for d in range(8)]
            sq = self.sb("sq", [128, 2, 512], BF16, es)
            Bsq = [Buf(f"sq{i}", sq[:, i, :]) for i in range(2)]
            rstd = self.sb("rstd", [128, 2, 512], F32, es)
            Brstd = [Buf(f"rstd{h}", rstd[:, h, :]) for h in range(2)]
            sil = self.sb("sil", [128, 2, 512], F32, es)
            Bsil = [Buf(f"sil{i}", sil[:, i, :]) for i in range(2)]
            tmp = self.sb("tmpf", [128, 2, 512], F32, es)
            Btmp = [Buf(f"tmpf{i}", tmp[:, i, :]) for i in range(2)]

            PRE = 3
            slots = {}
            for f in range(PRE):
                slots[f] = self.ring_load(self.wup_s[lj * NF + f], 2048)

            for h in range(2):
                ps = self.ps[6 + h]
                for d in range(8):
                    sb_ = Bsq[d % 2]
                    S.op("act", lambda e, a=sb_.ap, b=self.xT[d][h].ap: e.activation(out=a, in_=b, func=AF.Square),
                         r=[self.xT[d][h]], w=[sb_])
                    S.op("pe", lambda e, ps=ps, a=sb_.ap, d=d: e.matmul(ps.ap, self.onesb, a, start=(d == 0), stop=(d == 7)),
                         r=[sb_, self.Bcstb], w=[ps])
                self.rstd_from_ps(ps, Brstd[h])
                for d in range(8):
                    S.op("dve", lambda e, d=d, h=h: e.scalar_tensor_tensor(
                        out=Bxn[d][h].ap, in0=self.xT[d][h].ap, scalar=self.gain[:, gi_pre + d: gi_pre + d + 1],
                        in1=Brstd[h].ap, op0=ALU.mult, op1=ALU.mult),
                        r=[self.xT[d][h], Brstd[h], self.Bgain], w=[Bxn[d][h]])

            k = 0
            dn_slots = {}
            for f in range(NF):
                slot = slots[f]
                wt = self.wring_t[slot]
                wB = self.wring[slot]
                for h in range(2):
                    p1 = self.ps[(k % 2) * 2]
                    p3 = self.ps[(k % 2) * 2 + 1]
                    for (pp, off) in ((p1, 0), (p3, 1024)):
                        for d in range(8):
                            S.op("pe", lambda e, pp=pp, off=off, d=d, h=h, wt=wt: e.matmul(
                                pp.ap, wt[:, off + d * 128: off + (d + 1) * 128], Bxn[d][h].ap,
                                start=(d == 0), stop=(d == 7)),
                                r=[wB, Bxn[d][h]], w=[pp])
                    sB = Bsil[k % 2]
                    S.op("act", lambda e, a=sB.ap, b=p1.ap: e.activation(out=a, in_=b, func=AF.Silu), r=[p1], w=[sB])
                    S.op("dve", lambda e, a=Bhid[f][h].ap, b=sB.ap, c=p3.ap: e.tensor_tensor(a, b, c, ALU.mult),
                         r=[sB, p3], w=[Bhid[f][h]])
                    k += 1
                nf_ = f + PRE
                if nf_ < NF:
                    slots[nf_] = self.ring_load(self.wup_s[lj * NF + nf_], 2048)
                elif nf_ - NF < 8:
                    m = nf_ - NF
                    dn_slots[m] = self.ring_load(self.wdn_s[lj * 8 + m], DFF)

            k = 0
            for m in range(8):
                slot = dn_slots[m]
                wt = self.wring_t[slot]
                wB = self.wring[slot]
                for h in range(2):
                    pp = self.ps[4 + (k % 2)]
                    for f in range(NF):
                        S.op("pe", lambda e, pp=pp, f=f, h=h, wt=wt: e.matmul(
                            pp.ap, wt[:, f * 128:(f + 1) * 128], Bhid[f][h].ap, start=(f == 0), stop=(f == NF - 1)),
                            r=[wB, Bhid[f][h]], w=[pp])
                    S.op("act", lambda e, a=Bff[m][h].ap, b=pp.ap: e.copy(a, b), r=[pp], w=[Bff[m][h]])
                    sb_ = Bsq[k % 2]
                    S.op("act", lambda e, a=sb_.ap, b=pp.ap: e.activation(out=a, in_=b, func=AF.Square), r=[pp], w=[sb_])
                    S.op("pe", lambda e, h=h, a=sb_.ap, m=m: e.matmul(self.ps[6 + h].ap, self.onesb, a,
                                                                      start=(m == 0), stop=(m == 7)),
                         r=[sb_, self.Bcstb], w=[self.ps[6 + h]])
                    k += 1
                if m + PRE < 8:
                    dn_slots[m + PRE] = self.ring_load(self.wdn_s[lj * 8 + m + PRE], DFF)
            for h in range(2):
                self.rstd_from_ps(self.ps[6 + h], Brstd[h])
                for d in range(8):
                    tB = Btmp[d % 2]
                    S.op("dve", lambda e, d=d, h=h, tB=tB: e.scalar_tensor_tensor(
                        out=tB.ap, in0=Bff[d][h].ap, scalar=self.ghalf[:, gi_post + d: gi_post + d + 1],
                        in1=Brstd[h].ap, op0=ALU.mult, op1=ALU.mult),
                        r=[Bff[d][h], Brstd[h], self.Bgain], w=[tB])
                    S.op("pool", lambda e, d=d, h=h, tB=tB: e.tensor_tensor(
                        self.xT[d][h].ap, self.xT[d][h].ap, tB.ap, ALU.add),
                        r=[tB, self.xT[d][h]], w=[self.xT[d][h]])
            S.barrier()


    def ck(self, name):
        if self.cut == name:
            self.S.barrier()
            raise _Cut()

    def pb(self):
        b = self.ps[self.ps_i % 8]
        self.ps_i += 1
        return b

    def inproj_fm(self, l, chunk, ncols, hT, Bh, tok0, ntok, ps, out_ap=None):
        S = self.S
        slot = self.ring_load(self.win_s[l * NCH + chunk][:, 0:8 * ncols], 8 * ncols)
        wt = self.wring_t[slot]
        wB = self.wring[slot]
        oap = ps.ap[0:ncols, 0:ntok] if out_ap is None else out_ap
        for dt in range(8):
            self.mm(ps, oap, wt[:, dt * ncols:(dt + 1) * ncols], hT[:, dt, tok0:tok0 + ntok],
                    r=[wB, Bh], start=(dt == 0), stop=(dt == 7))

    def rwkv(self, l, tt, hT, Bh, mixT, Bmix):
        S = self.S
        P = self.par[l]
        BP = self.Bpar[l]
        pc = lambda name, i=0, n=1: P[:, PO[name] + i: PO[name] + i + n]
        cst = self.cst
        LEm = cst[:, 384:512]
        GTm = cst[:, 512:640]
        GEm = cst[:, 640:768]
        BLK = self.cstb[:, 896:1024]
        RESET = cst[:, 1024:1536]
        AM = cst[:, 1664:2176].rearrange("p (a b) -> p a b", b=128)
        with ExitStack() as es:
            nb = lambda n, s, dt=F32: self.nb("rw_" + n, s, dt, es)
            Pb = nb("P", [128, 8, 513])
            Z = VBuf(Pb, _Shift(Pb.t))
            t6 = nb("t6", [128, 512], BF16)
            sgd = nb("sgd", [128, 512], BF16)
            vres = nb("vres", [32, 512], BF16)
            a_ = nb("a", [128, 1, 512])
            lw = nb("lw", [128, 1, 512])
            kk = nb("kk", [128, 1, 512])
            kmod = nb("kmod", [128, 1, 512])
            tmp = [nb(f"tmp{i}", [128, 512]) for i in range(4)]
            tmpb = [nb(f"tmpb{i}", [128, 512], BF16) for i in range(2)]
            ex = [nb(f"ex{i}", [128, 512]) for i in range(4)]
            ops_ = {n: nb(n, [128, 2, 512], BF16) for n in ("alT", "btT", "ktT", "rbT", "KhT", "BhT", "vbT")}
            g_ = nb("g", [128, 2, 512], BF16)
            rks = nb("rks", [128, 2, 512], BF16)
            ybuf = nb("ybuf", [128, 2, 512])
            pads = {n: [[nb(f"{n}{i}{hh}", [128, 128], BF16) for hh in range(2)] for i in range(2)]
                    for n in ("Ap", "Kp", "Bp", "Vp")}
            for n in pads:
                for i in range(2):
                    for hh in range(2):
                        S.op("pool", lambda e, b=pads[n][i][hh]: e.memset(b.ap, 0.0), w=[pads[n][i][hh]])
            Amat = [nb(f"Amat{i}", [128, 4, 128], BF16) for i in range(2)]
            NnB = nb("Nn", [128, 2, 128], BF16)
            MmB = nb("Mm", [128, 2, 128], BF16)
            QB = nb("Q", [128, 2, 128], BF16)
            N2B = nb("N2", [128, 2, 128], BF16)
            M2B = nb("M2", [128, 2, 128], BF16)
            ArT = nb("ArT", [128, 2, 2, 128], BF16)
            WtT = nb("WtT", [128, 2, 128], BF16)
            PT = nb("PT", [128, 2, 128], BF16)
            Ut = nb("Ut", [128, 2, 128])
            Up = nb("Up", [128, 2, 128], BF16)
            Sb = self.rw_Sb[l]
            Sf = self.rw_Sf[l]
            halo = self.rw_halo[l]
            for st in range(2):
                t0 = st * 512
                S.op("act", lambda e: e.copy(Pb.t[:, :, 0], halo.ap), r=[halo], w=[Pb])
                for c in range(8):
                    ps = self.pb()
                    self.inproj_fm(l, c, 128, hT, Bh, t0, 512, ps)
                    S.op("act", lambda e, c=c, ps=ps: e.copy(Pb.t[:, c, 1:513], ps.ap), r=[ps], w=[Pb])
                S.op("act", lambda e: e.copy(halo.ap, Pb.t[:, :, 512]), r=[Pb], w=[halo])
                for c in range(8):
                    tq_ = tmp[c % 2]
                    S.op("dve", lambda e, c=c: e.tensor_tensor(tq_.ap, Pb.t[:, c, 0:512], Pb.t[:, c, 1:513], ALU.subtract),
                         r=[Pb], w=[tq_])
                    S.op("dve", lambda e, c=c: e.scalar_tensor_tensor(out=Pb.t[:, c, 1:513], in0=tq_.ap, scalar=pc("mu", c),
                                                                    in1=Pb.t[:, c, 1:513], op0=ALU.mult, op1=ALU.add),
                         r=[Pb, tq_, BP], w=[Pb])
                self.ck('rwA')
                S.op("act", lambda e: e.activation(out=t6.t[0:64, :], in_=Z.t[0:64, 6, :], func=AF.Tanh), r=[Z], w=[t6])
                S.op("act", lambda e: e.copy(t6.t[64:128, :], Z.t[64:128, 6, :]), r=[Z], w=[t6])
                S.op("act", lambda e: e.activation(out=sgd.ap, in_=Z.t[:, 7, :], func=AF.Sigmoid), r=[Z], w=[sgd])
                LW = self.lorab[l]
                BLW = self.Blorab[l]
                for ct in range(2):
                    ps = self.pb()
                    self.mm(ps, ps.ap, LW[:, 256 + ct * 128: 256 + (ct + 1) * 128], sgd.ap, r=[BLW, sgd])
                    S.op("act", lambda e, ps=ps, ct=ct: e.copy(g_.t[:, ct, :], ps.ap), r=[ps], w=[g_])
                vT = lambda ct: Z.t[:, 4 + ct, :]
                if l == 0:
                    for ct in range(2):
                        S.op("pool", lambda e, ct=ct: e.tensor_copy(self.vfirst.t[:, ct, t0:t0 + 512], vT(ct)),
                             r=[Z], w=[self.vfirst])
                else:
                    ps = self.pb()
                    self.inproj_fm(l, 27, 32, hT, Bh, t0, 512, ps)
                    S.op("act", lambda e, ps=ps: e.copy(vres.ap, ps.ap[0:32, :]), r=[ps], w=[vres])
                    for ct in range(2):
                        ps = self.pb()
                        self.mm(ps, ps.ap, LW[0:32, 512 + ct * 128: 512 + (ct + 1) * 128], vres.ap, r=[BLW, vres])
                        S.op("act", lambda e, ps=ps, ct=ct: e.activation(out=tmp[0].ap, in_=ps.ap, func=AF.Sigmoid,
                                                                          bias=pc("v0", ct)), r=[ps, BP], w=[tmp[0]])
                        S.op("dve", lambda e, ct=ct: e.tensor_tensor(tmp[1].ap, self.vfirst.t[:, ct, t0:t0 + 512], vT(ct), ALU.subtract),
                             r=[self.vfirst, Z], w=[tmp[1]])
                        S.op("dve", lambda e: e.tensor_tensor(tmp[1].ap, tmp[1].ap, tmp[0].ap, ALU.mult),
                             r=[tmp[0], tmp[1]], w=[tmp[1]])
                        S.op("dve", lambda e, ct=ct: e.tensor_tensor(vT(ct), vT(ct), tmp[1].ap, ALU.add), r=[tmp[1], Z], w=[Z])
                self.ck('rwC')
                for ct in range(2):
                    rT = Z.t[:, ct, :]
                    kT = Z.t[:, 2 + ct, :]
                    ps = self.pb()
                    self.mm(ps, ps.ap, LW[0:64, ct * 128:(ct + 1) * 128], t6.t[0:64, :], r=[BLW, t6])
                    S.op("act", lambda e, ps=ps, ct=ct: e.activation(out=lw.t[:, 0, :], in_=ps.ap, func=AF.Sigmoid,
                                                                      bias=pc("w0", ct)), r=[ps, BP], w=[lw])
                    ps = self.pb()
                    self.mm(ps, ps.ap, LW[64:128, ct * 128:(ct + 1) * 128], t6.t[64:128, :], r=[BLW, t6])
                    S.op("act", lambda e, ps=ps, ct=ct: e.activation(out=a_.t[:, 0, :], in_=ps.ap, func=AF.Sigmoid,
                                                                      bias=pc("a0", ct)), r=[ps, BP], w=[a_])
                    S.op("dve", lambda e: e.tensor_scalar(kk.t[:, 0, :], kT, pc("kk", ct), None, ALU.mult), r=[Z, BP], w=[kk])
                    S.op("act", lambda e: e.activation(out=tmpb[0].ap, in_=kk.t[:, 0, :], func=AF.Square), r=[kk], w=[tmpb[0]])
                    ps = self.pb()
                    self.mm(ps, ps.ap, BLK, tmpb[0].ap, r=[tmpb[0], self.Bcstb])
                    S.op("act", lambda e, ps=ps: e.activation(out=tmp[0].ap, in_=ps.ap, func=AF.Sqrt), r=[ps], w=[tmp[0]])
                    S.op("dve", lambda e: e.tensor_scalar(tmp[0].ap, tmp[0].ap, 1e-12, None, ALU.max), r=[tmp[0]], w=[tmp[0]])
                    S.op("dve", lambda e: e.reciprocal(tmp[0].ap, tmp[0].ap), r=[tmp[0]], w=[tmp[0]])
                    S.op("dve", lambda e: e.tensor_tensor(kk.t[:, 0, :], kk.t[:, 0, :], tmp[0].ap, ALU.mult), r=[kk, tmp[0]], w=[kk])
                    S.op("dve", lambda e: e.tensor_scalar(tmp[1].ap, a_.t[:, 0, :], pc("ka", ct), pc("omk", ct), ALU.mult, ALU.add),
                         r=[a_, BP], w=[tmp[1]])
                    S.op("dve", lambda e: e.tensor_tensor(kmod.t[:, 0, :], tmp[1].ap, kT, ALU.mult), r=[tmp[1], Z], w=[kmod])
                    S.op("pool", lambda e: e.tensor_scalar(lw.t[:, 0, :], lw.t[:, 0, :], -0.6065306597126334, None, ALU.mult),
                         r=[lw], w=[lw])
                    S.op("dve", lambda e: e.tensor_tensor_scan(tmp[2].ap, RESET, lw.t[:, 0, :], 0.0, ALU.mult, ALU.add),
                         r=[lw, self.Bcst], w=[tmp[2]])
                    Lam = tmp[2]
                    S.op("act", lambda e: e.activation(out=ex[0].ap, in_=Lam.ap, func=AF.Exp), r=[Lam], w=[ex[0]])
                    S.op("dve", lambda e: e.tensor_tensor(tmp[3].ap, Lam.ap, lw.t[:, 0, :], ALU.subtract), r=[Lam, lw], w=[tmp[3]])
                    S.op("act", lambda e: e.activation(out=ex[1].ap, in_=tmp[3].ap, func=AF.Exp), r=[tmp[3]], w=[ex[1]])
                    S.op("act", lambda e: e.activation(out=ex[2].ap, in_=Lam.ap, func=AF.Exp, scale=-1.0), r=[Lam], w=[ex[2]])
                    for cc in range(4):
                        S.op("dve", lambda e, cc=cc: e.tensor_scalar(tmp[3].t[:, cc * 128:(cc + 1) * 128], Lam.t[:, cc * 128:(cc + 1) * 128],
                                                                     -1.0, Lam.t[:, cc * 128 + 127: cc * 128 + 128], ALU.mult, ALU.add),
                             r=[Lam, tmp[3]], w=[tmp[3]])
                    S.op("act", lambda e: e.activation(out=ex[3].ap, in_=tmp[3].ap, func=AF.Exp), r=[tmp[3]], w=[ex[3]])
                    o = ops_
                    S.op("dve", lambda e: e.scalar_tensor_tensor(out=o["alT"].t[:, ct, :], in0=kk.t[:, 0, :], scalar=-1.0, in1=ex[1].ap,
                                                                 op0=ALU.mult, op1=ALU.mult), r=[kk, ex[1]], w=[o["alT"]])
                    S.op("pool", lambda e: e.tensor_tensor(tmp[1].ap, kk.t[:, 0, :], a_.t[:, 0, :], ALU.mult), r=[kk, a_, tmp[1]], w=[tmp[1]])
                    S.op("dve", lambda e: e.tensor_tensor(o["btT"].t[:, ct, :], tmp[1].ap, ex[2].ap, ALU.mult), r=[tmp[1], ex[2]], w=[o["btT"]])
                    S.op("pool", lambda e: e.tensor_tensor(o["BhT"].t[:, ct, :], tmp[1].ap, ex[3].ap, ALU.mult), r=[tmp[1], ex[3]], w=[o["BhT"]])
                    S.op("dve", lambda e: e.tensor_tensor(o["ktT"].t[:, ct, :], kmod.t[:, 0, :], ex[2].ap, ALU.mult), r=[kmod, ex[2]], w=[o["ktT"]])
                    S.op("pool", lambda e: e.tensor_tensor(o["KhT"].t[:, ct, :], kmod.t[:, 0, :], ex[3].ap, ALU.mult), r=[kmod, ex[3]], w=[o["KhT"]])
                    S.op("dve", lambda e: e.tensor_tensor(o["rbT"].t[:, ct, :], rT, ex[0].ap, ALU.mult), r=[Z, ex[0]], w=[o["rbT"]])
                    S.op("act", lambda e: e.copy(o["vbT"].t[:, ct, :], vT(ct)), r=[Z], w=[o["vbT"]])
                    S.op("dve", lambda e: e.scalar_tensor_tensor(out=tmpb[1].ap, in0=rT, scalar=pc("rk", ct), in1=kmod.t[:, 0, :],
                                                                 op0=ALU.mult, op1=ALU.mult), r=[Z, kmod, BP], w=[tmpb[1]])
                    ps = self.pb()
                    self.mm(ps, ps.ap, BLK, tmpb[1].ap, r=[tmpb[1], self.Bcstb])
                    S.op("act", lambda e, ps=ps: e.copy(rks.t[:, ct, :], ps.ap), r=[ps], w=[rks])
                    S.op("pool", lambda e: e.tensor_copy(self.rw_gc.t[:, ct, :], ex[0].t[:, 127:512:128]), r=[ex[0]], w=[self.rw_gc])

                self.ck('rwD')
                for cc in range(4):
                    csl = slice(cc * 128, (cc + 1) * 128)
                    for ct in range(2):
                        pi = (cc * 2 + ct) % 2
                        o = ops_
                        pst = self.pb()
                        psb = pst.ap.bitcast(BF16)
                        for j, n in enumerate(("alT", "KhT", "BhT", "vbT")):
                            self.tr(pst, psb[:, j * 128:(j + 1) * 128], o[n].t[:, ct, csl], r=[o[n]])
                        self.ck('rwE1a')
                        for j, n in enumerate(("Ap", "Kp", "Bp", "Vp")):
                            if j == 1:
                                self.ck('rwE1b')
                            for hh in range(2):
                                dst = pads[n][pi][hh]
                                eng = "act" if (j + hh) % 2 == 0 else "dve"
                                import os as _os
                                if _os.environ.get("PADENG"):
                                    eng = _os.environ["PADENG"]
                                src_ap = psb[:, j * 128 + hh * 64: j * 128 + hh * 64 + 64]
                                if eng == "act":
                                    S.op("act", lambda e, d=dst, s_=src_ap, hh=hh: e.copy(d.t[:, hh * 64: hh * 64 + 64], s_), r=[pst], w=[dst])
                                else:
                                    S.op("dve", lambda e, d=dst, s_=src_ap, hh=hh: e.tensor_copy(d.t[:, hh * 64: hh * 64 + 64], s_), r=[pst], w=[dst])
                        self.ck('rwE1')
                        for hh in range(2):
                            hsl = slice(hh * 64, hh * 64 + 64)
                            pa = self.pb()
                            for j, (ln, rn) in enumerate((("alT", "btT"), ("alT", "ktT"), ("rbT", "ktT"), ("rbT", "btT"))):
                                self.mm(pa, pa.ap[:, j * 128:(j + 1) * 128], o[ln].t[hsl, ct, csl], o[rn].t[hsl, ct, csl], r=[o[ln], o[rn]])
                            Am = Amat[hh]
                            S.op("dve", lambda e, pa=pa, Am=Am: e.tensor_tensor(Am.ap, pa.ap.rearrange("p (a b) -> p a b", b=128), AM, ALU.mult),
                                 r=[pa, self.Bcst], w=[Am])
                            S.op("pool", lambda e, Am=Am, hh=hh: e.tensor_copy(NnB.t[:, hh, :], Am.t[:, 0, :]), r=[Am], w=[NnB])
                        self.ck('rwE2')
                        pst = self.pb()
                        psb = pst.ap.bitcast(BF16)
                        for hh in range(2):
                            self.tr(pst, psb[:, hh * 128:(hh + 1) * 128], Amat[hh].t[:, 0, :], r=[Amat[hh]])
                            self.tr(pst, psb[:, 256 + hh * 256: 256 + hh * 256 + 128], Amat[hh].t[:, 2, :], r=[Amat[hh]])
                            self.tr(pst, psb[:, 256 + hh * 256 + 128: 256 + hh * 256 + 256], Amat[hh].t[:, 3, :], r=[Amat[hh]])
                        S.op("act", lambda e, psb=psb: e.copy(MmB.ap.rearrange("p a b -> p (a b)"), psb[:, 0:256]), r=[pst], w=[MmB])
                        S.op("dve", lambda e, psb=psb: e.tensor_copy(ArT.ap.rearrange("p a b c -> p (a b c)"), psb[:, 256:768]), r=[pst], w=[ArT])
                        self.ck('rwE3')
                        self.neumann2("rw", NnB, MmB, N2B, M2B, QB, 2)
                        self.ck('rwE4')
                        pw = self.pb()
                        for hh in range(2):
                            self.mm(pw, pw.ap[:, hh * 128:(hh + 1) * 128], pads["Ap"][pi][hh].ap, QB.t[:, hh, :], r=[pads["Ap"][pi][hh], QB])
                            self.mm(pw, pw.ap[:, 256 + hh * 128: 256 + (hh + 1) * 128], Amat[hh].t[:, 1, :], QB.t[:, hh, :], r=[Amat[hh], QB])
                        S.op("act", lambda e, pw=pw: e.copy(WtT.ap.rearrange("p a b -> p (a b)"), pw.ap[:, 0:256]), r=[pw], w=[WtT])
                        S.op("dve", lambda e, pw=pw: e.tensor_copy(PT.ap.rearrange("p a b -> p (a b)"), pw.ap[:, 256:512]), r=[pw], w=[PT])
                        pu = self.pb()
                        for hh in range(2):
                            self.mm(pu, pu.ap[:, hh * 128:(hh + 1) * 128], PT.t[:, hh, :], pads["Vp"][pi][hh].ap, r=[PT, pads["Vp"][pi][hh]])
                        S.op("act", lambda e, pu=pu: e.copy(Ut.ap.rearrange("p a b -> p (a b)"), pu.ap[:, 0:256]), r=[pu], w=[Ut])
                        pws = self.pb()
                        for hh in range(2):
                            self.mm(pws, pws.ap[:, hh * 128:(hh + 1) * 128], WtT.t[:, hh, :], Sb.t[:, ct, :], r=[WtT, Sb])
                        S.op("dve", lambda e, pws=pws: e.tensor_tensor(Up.ap.rearrange("p a b -> p (a b)"), Ut.ap.rearrange("p a b -> p (a b)"),
                                                                      pws.ap[:, 0:256], ALU.add), r=[pws, Ut], w=[Up])
                        py = self.pb()
                        self.mm(py, py.ap[:, 0:128], Sb.t[:, ct, :], o["rbT"].t[:, ct, csl], r=[Sb, o["rbT"]], start=True, stop=False)
                        for hh in range(2):
                            self.mm(py, py.ap[:, 0:128], pads["Vp"][pi][hh].ap, ArT.t[:, hh, 0, :], r=[pads["Vp"][pi][hh], ArT], start=False, stop=False)
                            self.mm(py, py.ap[:, 0:128], Up.t[:, hh, :], ArT.t[:, hh, 1, :], r=[Up, ArT], start=False, stop=(hh == 1))
                        S.op("act", lambda e, py=py, ct=ct, csl=csl: e.copy(ybuf.t[:, ct, csl], py.ap[:, 0:128]), r=[py], w=[ybuf])
                        pss = self.pb()
                        for hh in range(2):
                            self.mm(pss, pss.ap[:, 0:128], pads["Kp"][pi][hh].ap, pads["Vp"][pi][hh].ap,
                                    r=[pads["Kp"][pi][hh], pads["Vp"][pi][hh]], start=(hh == 0), stop=False)
                            self.mm(pss, pss.ap[:, 0:128], pads["Bp"][pi][hh].ap, Up.t[:, hh, :],
                                    r=[pads["Bp"][pi][hh], Up], start=False, stop=(hh == 1))
                        S.op("dve", lambda e, pss=pss, ct=ct, cc=cc: e.scalar_tensor_tensor(
                            out=Sf.t[:, ct, :], in0=Sf.t[:, ct, :], scalar=self.rw_gc.t[:, ct, cc:cc + 1], in1=pss.ap[:, 0:128],
                            op0=ALU.mult, op1=ALU.add), r=[pss, Sf, self.rw_gc], w=[Sf])
                        S.op("act", lambda e, ct=ct: e.copy(Sb.t[:, ct, :], Sf.t[:, ct, :]), r=[Sf], w=[Sb])
                self.ck('rwF')
                for ct in range(2):
                    S.op("act", lambda e: e.copy(tmpb[0].ap, ybuf.t[:, ct, :]), r=[ybuf], w=[tmpb[0]])
                    ps = self.pb()
                    self.mm(ps, ps.ap, BLK, tmpb[0].ap, r=[tmpb[0], self.Bcstb])
                    S.op("dve", lambda e, ps=ps: e.scalar_tensor_tensor(out=tmp[0].ap, in0=ps.ap, scalar=-1.0 / 64, in1=ybuf.t[:, ct, :],
                                                                       op0=ALU.mult, op1=ALU.add), r=[ps, ybuf], w=[tmp[0]])
                    S.op("act", lambda e: e.activation(out=tmpb[1].ap, in_=tmp[0].ap, func=AF.Square), r=[tmp[0]], w=[tmpb[1]])
                    ps = self.pb()
                    self.mm(ps, ps.ap, BLK, tmpb[1].ap, r=[tmpb[1], self.Bcstb])
                    S.op("act", lambda e, ps=ps: e.activation(out=tmp[1].ap, in_=ps.ap, func=AF.Sqrt, scale=1.0 / 64, bias=cst[:, 257:258]),
                         r=[ps, self.Bcst], w=[tmp[1]])
                    S.op("dve", lambda e: e.reciprocal(tmp[1].ap, tmp[1].ap), r=[tmp[1]], w=[tmp[1]])
                    S.op("dve", lambda e: e.tensor_tensor(tmp[0].ap, tmp[0].ap, tmp[1].ap, ALU.mult), r=[tmp[0], tmp[1]], w=[tmp[0]])
                    S.op("act", lambda e: e.activation(out=tmp[0].ap, in_=tmp[0].ap, func=AF.Identity, scale=pc("lnw", ct), bias=pc("lnb", ct)),
                         r=[tmp[0], BP], w=[tmp[0]])
                    S.op("dve", lambda e: e.tensor_tensor(tmp[1].ap, rks.t[:, ct, :], vT(ct), ALU.mult), r=[rks, Z, tmp[1]], w=[tmp[1]])
                    S.op("dve", lambda e: e.tensor_tensor(tmp[0].ap, tmp[0].ap, tmp[1].ap, ALU.add), r=[tmp[0], tmp[1]], w=[tmp[0]])
                    S.op("dve", lambda e: e.tensor_tensor(mixT[:, ct, t0:t0 + 512], tmp[0].ap, g_.t[:, ct, :], ALU.mult),
                         r=[tmp[0], g_], w=[Bmix])
            S.barrier()

    def gdn(self, l, tt, hT, Bh, mixT, Bmix):
        S = self.S
        P = self.par[l]
        BP = self.Bpar[l]
        pc = lambda name, i=0, n=1: P[:, PO[name] + i: PO[name] + i + n]
        cst = self.cst
        LEm = cst[:, 384:512]
        GTm = cst[:, 512:640]
        NEG = cst[:, 768:896]
        ONESF = cst[:, 128:256]
        NGT4 = cst[:, 3008:3520].rearrange("p (a b) -> p a b", b=128)
        with ExitStack() as es:
            nb = lambda n, s, dt=F32: self.nb("gd_" + n, s, dt, es)
            diag = nb("diag", [128, 12, 4, 128], BF16)
            zb = nb("zb", [128, 12, 515], BF16)
            qn = nb("qn", [128, 4, 512], BF16)
            kn = nb("kn", [128, 4, 512], BF16)
            vb = nb("vb", [128, 4, 512], BF16)
            sg = nb("sg", [128, 4, 512], BF16)
            tf = [nb(f"tf{i}", [128, 512]) for i in range(3)]
            tb = [nb(f"tb{i}", [128, 512], BF16) for i in range(2)]
            gt = nb("gt", [128, 8])
            ge = nb("ge", [128, 8])
            beg = nb("beg", [128, 4])
            Lh = nb("Lh", [128, 4, 128])
            dec = nb("dec", [128, 4, 128])
            dsn = nb("dsn", [128, 4, 128])
            egB = nb("egB", [128, 4, 128])
            NnB = nb("Nn", [128, 4, 128], F32)
            MmB = nb("Mm", [128, 4, 128], F32)
            N2 = nb("N2", [128, 4, 128], F32)
            M2 = nb("M2", [128, 4, 128], F32)
            QB = nb("Q", [128, 4, 128], F32)
            Qbf = nb("Qbf", [128, 4, 128], BF16)
            att = nb("att", [128, 4, 128], BF16)
            attT = nb("attT", [128, 4, 128], BF16)
            bv = nb("bv", [128, 4, 128], BF16)
            kbg = nb("kbg", [128, 4, 128], BF16)
            kdec = nb("kdec", [128, 4, 128], BF16)
            u_ = nb("u", [128, 4, 128])
            wT = nb("wT", [128, 4, 128], BF16)
            qdT = nb("qdT", [128, 4, 128], BF16)
            vnew = nb("vnew", [128, 4, 128], BF16)
            osb = nb("osb", [128, 4, 128])
            Sb = self.gd_Sb[l]
            Sf = self.gd_Sf[l]
            halo = self.gd_halo[l]
            for ti in range(12):
                for j in range(4):
                    S.op("pool" if (ti + j) % 2 else "dve",
                         lambda e, ti=ti, j=j: e.tensor_scalar(diag.t[:, ti, j, :], self.ident, pc("conv", ti * 4 + j), None, ALU.mult),
                         r=[self.Bcst, BP], w=[diag])
            for st in range(2):
                t0 = st * 512
                S.op("act", lambda e: e.copy(zb.t[:, :, 0:3], halo.ap), r=[halo], w=[zb])
                for ti in range(12):
                    ps = self.pb()
                    self.inproj_fm(l, 10 + ti, 128, hT, Bh, t0, 512, ps)
                    S.op("act", lambda e, ti=ti, ps=ps: e.copy(zb.t[:, ti, 3:515], ps.ap), r=[ps], w=[zb])
                S.op("act", lambda e: e.copy(halo.ap, zb.t[:, :, 512:515]), r=[zb], w=[halo])
                for ti in range(12):
                    kind, hd = divmod(ti, 4)
                    ps = self.pb()
                    for j in range(4):
                        self.mm(ps, ps.ap, diag.t[:, ti, j, :], zb.t[:, ti, j:j + 512], r=[diag, zb], start=(j == 0), stop=(j == 3))
                    if kind == 2:
                        S.op("act", lambda e, ps=ps, hd=hd: e.activation(out=vb.t[:, hd, :], in_=ps.ap, func=AF.Silu), r=[ps], w=[vb])
                        continue
                    S.op("act", lambda e, ps=ps: e.activation(out=tf[0].ap, in_=ps.ap, func=AF.Silu), r=[ps], w=[tf[0]])
                    S.op("act", lambda e: e.activation(out=tb[0].ap, in_=tf[0].ap, func=AF.Square), r=[tf[0]], w=[tb[0]])
                    p2 = self.pb()
                    self.mm(p2, p2.ap, self.onesb, tb[0].ap, r=[tb[0], self.Bcstb])
                    S.op("act", lambda e, p2=p2: e.activation(out=tf[1].ap, in_=p2.ap, func=AF.Sqrt), r=[p2], w=[tf[1]])
                    S.op("dve", lambda e: e.tensor_scalar(tf[1].ap, tf[1].ap, 1e-12, None, ALU.max), r=[tf[1]], w=[tf[1]])
                    S.op("dve", lambda e: e.reciprocal(tf[1].ap, tf[1].ap), r=[tf[1]], w=[tf[1]])
                    if kind == 0:
                        S.op("dve", lambda e, hd=hd: e.scalar_tensor_tensor(out=qn.t[:, hd, :], in0=tf[0].ap, scalar=128.0 ** -0.5, in1=tf[1].ap,
                                                                           op0=ALU.mult, op1=ALU.mult), r=[tf[0], tf[1]], w=[qn])
                    else:
                        S.op("dve", lambda e, hd=hd: e.tensor_tensor(kn.t[:, hd, :], tf[0].ap, tf[1].ap, ALU.mult), r=[tf[0], tf[1]], w=[kn])
                self.ck('gdA')
                for hd in range(4):
                    ps = self.pb()
                    self.inproj_fm(l, 22 + hd, 128, hT, Bh, t0, 512, ps)
                    S.op("act", lambda e, ps=ps, hd=hd: e.activation(out=sg.t[:, hd, :], in_=ps.ap, func=AF.Silu), r=[ps], w=[sg])
                gslot = self.ring_load(self.win_s[l * NCH + 26][:, 0:64], 64)
                gw = self.wring_t[gslot]
                gB = self.wring[gslot]
                for cc in range(4):
                    c0 = t0 + cc * 128
                    csl = slice(cc * 128, (cc + 1) * 128)
                    pg = self.pb()
                    for dt in range(8):
                        self.mm(pg, pg.ap[:, 0:8], hT[:, dt, c0:c0 + 128], gw[:, dt * 8:(dt + 1) * 8], r=[gB, Bh], start=(dt == 0), stop=(dt == 7))
                    S.op("act", lambda e, pg=pg: e.activation(out=gt.t[:, 0:4], in_=pg.ap[:, 0:4], func=AF.Sigmoid), r=[pg], w=[gt])
                    S.op("dve", lambda e, pg=pg: e.tensor_tensor(gt.t[:, 4:8], pg.ap[:, 4:8], pc("dtb", 0, 4), ALU.add), r=[pg, BP, gt], w=[gt])
                    S.op("act", lambda e: e.activation(out=gt.t[:, 4:8], in_=gt.t[:, 4:8], func=AF.Exp), r=[gt], w=[gt])
                    S.op("act", lambda e: e.activation(out=gt.t[:, 4:8], in_=gt.t[:, 4:8], func=AF.Ln, bias=cst[:, 259:260]), r=[gt, self.Bcst], w=[gt])
                    S.op("dve", lambda e: e.tensor_tensor(gt.t[:, 4:8], gt.t[:, 4:8], pc("nA", 0, 4), ALU.mult), r=[gt, BP], w=[gt])
                    self.ck('gdB')
                    pq = self.pb()
                    self.mm(pq, pq.ap[:, 0:4], LEm, gt.t[:, 4:8], r=[gt, self.Bcst])
                    self.mm(pq, pq.ap[:, 4:8], GTm, gt.t[:, 4:8], r=[gt, self.Bcst])
                    S.op("act", lambda e, pq=pq: e.activation(out=ge.ap, in_=pq.ap[:, 0:8], func=AF.Exp), r=[pq], w=[ge])
                    S.op("dve", lambda e: e.tensor_tensor(beg.ap, gt.t[:, 0:4], ge.t[:, 0:4], ALU.mult), r=[gt, ge], w=[beg])
                    for hd in range(4):
                        S.op("pool" if hd % 2 else "dve", lambda e, hd=hd: e.tensor_scalar(Lh.t[:, hd, :], LEm, gt.t[:, 4 + hd: 5 + hd], None, ALU.mult),
                             r=[gt, self.Bcst], w=[Lh])
                    pd = self.pb()
                    pe_ = self.pb()
                    for hd in range(4):
                        hs = slice(hd * 128, (hd + 1) * 128)
                        self.mm(pd, pd.ap[:, hs], Lh.t[:, hd, :], GTm, r=[Lh, self.Bcst], start=True, stop=False)
                        self.mm(pd, pd.ap[:, hs], self.ident, NEG, r=[self.Bcst], start=False, stop=True)
                        self.mm(pe_, pe_.ap[:, hs], ONESF, Lh.t[:, hd, :], r=[Lh, self.Bcst])
                    S.op("act", lambda e, pd=pd: e.activation(out=dec.ap.rearrange("p a b -> p (a b)"), in_=pd.ap, func=AF.Exp), r=[pd], w=[dec])
                    S.op("act", lambda e, pe_=pe_: e.activation(out=egB.ap.rearrange("p a b -> p (a b)"), in_=pe_.ap, func=AF.Exp), r=[pe_], w=[egB])
                    S.op("pool", lambda e: e.tensor_tensor(dsn.ap, dec.ap, NGT4, ALU.mult), r=[dec, self.Bcst], w=[dsn])
                    self.ck('gdC')
                    pk = self.pb()
                    pqk = self.pb()
                    for hd in range(4):
                        hs = slice(hd * 128, (hd + 1) * 128)
                        self.mm(pk, pk.ap[:, hs], kn.t[:, hd, csl], kn.t[:, hd, csl], r=[kn])
                        self.mm(pqk, pqk.ap[:, hs], qn.t[:, hd, csl], kn.t[:, hd, csl], r=[qn, kn])
                    for hd in range(4):
                        hs = slice(hd * 128, (hd + 1) * 128)
                        S.op("dve", lambda e, hd=hd, hs=hs, pk=pk: e.scalar_tensor_tensor(out=NnB.t[:, hd, :], in0=pk.ap[:, hs], scalar=gt.t[:, hd:hd + 1],
                                                                                         in1=dsn.t[:, hd, :], op0=ALU.mult, op1=ALU.mult),
                             r=[pk, gt, dsn], w=[NnB])
                    S.op("dve", lambda e, pqk=pqk: e.tensor_tensor(att.ap.rearrange("p a b -> p (a b)"), pqk.ap, dec.ap.rearrange("p a b -> p (a b)"), ALU.mult),
                         r=[pqk, dec], w=[att])
                    self.ck('gdD')
                    pt1 = self.pb()
                    pt2 = self.pb()
                    ptn = self.pb()
                    b1 = pt1.ap.bitcast(BF16)
                    b2 = pt2.ap.bitcast(BF16)
                    for hd in range(4):
                        hs = slice(hd * 128, (hd + 1) * 128)
                        self.tr(ptn, ptn.ap[:, hs], NnB.t[:, hd, :], r=[NnB], f32=True)
                        self.tr(pt1, b1[:, 512 + hd * 128: 512 + (hd + 1) * 128], att.t[:, hd, :], r=[att])
                        self.tr(pt2, b2[:, hs], kn.t[:, hd, csl], r=[kn])
                        self.tr(pt2, b2[:, 512 + hd * 128: 512 + (hd + 1) * 128], vb.t[:, hd, csl], r=[vb])
                    S.op("act", lambda e: e.copy(MmB.ap.rearrange("p a b -> p (a b)"), ptn.ap), r=[ptn], w=[MmB])
                    S.op("dve", lambda e: e.tensor_copy(attT.ap.rearrange("p a b -> p (a b)"), b1[:, 512:1024]), r=[pt1], w=[attT])
                    for hd in range(4):
                        hs = slice(hd * 128, (hd + 1) * 128)
                        S.op("act", lambda e, hd=hd, hs=hs: e.activation(out=kbg.t[:, hd, :], in_=b2[:, hs], func=AF.Copy, scale=beg.t[:, hd:hd + 1]),
                             r=[pt2, beg], w=[kbg])
                        S.op("dve", lambda e, hd=hd, hs=hs: e.tensor_scalar(kdec.t[:, hd, :], b2[:, hs], ge.t[:, 4 + hd: 5 + hd], None, ALU.mult),
                             r=[pt2, ge], w=[kdec])
                        S.op("pool" if False else "act", lambda e, hd=hd: e.activation(out=bv.t[:, hd, :], in_=b2[:, 512 + hd * 128: 512 + (hd + 1) * 128],
                                                                                         func=AF.Copy, scale=gt.t[:, hd:hd + 1]),
                             r=[pt2, gt], w=[bv])
                    self.ck('gdE')
                    self.neumann2("gd", NnB, MmB, N2, M2, QB, 4, f32=True)
                    S.op("act", lambda e: e.copy(Qbf.ap, QB.ap), r=[QB], w=[Qbf])
                    self.ck('gdF')
                    pu = self.pb()
                    pw = self.pb()
                    for hd in range(4):
                        hs = slice(hd * 128, (hd + 1) * 128)
                        self.mm(pu, pu.ap[:, hs], Qbf.t[:, hd, :], bv.t[:, hd, :], r=[Qbf, bv])
                        self.mm(pw, pw.ap[:, hs], kbg.t[:, hd, :], Qbf.t[:, hd, :], r=[Qbf, kbg])
                    S.op("act", lambda e, pu=pu: e.copy(u_.ap.rearrange("p a b -> p (a b)"), pu.ap), r=[pu], w=[u_])
                    S.op("dve", lambda e, pw=pw: e.tensor_copy(wT.ap.rearrange("p a b -> p (a b)"), pw.ap), r=[pw], w=[wT])
                    for hd in range(4):
                        S.op("pool", lambda e, hd=hd: e.tensor_tensor(qdT.t[:, hd, :], qn.t[:, hd, csl], egB.t[:, hd, :], ALU.mult), r=[qn, egB], w=[qdT])
                    self.ck('gdG')
                    pws = self.pb()
                    for hd in range(4):
                        hs = slice(hd * 128, (hd + 1) * 128)
                        self.mm(pws, pws.ap[:, hs], wT.t[:, hd, :], Sb.t[:, hd, :], r=[wT, Sb])
                    S.op("dve", lambda e, pws=pws: e.tensor_tensor(vnew.ap.rearrange("p a b -> p (a b)"), u_.ap.rearrange("p a b -> p (a b)"), pws.ap, ALU.subtract),
                         r=[pws, u_], w=[vnew])
                    po = self.pb()
                    pkv = self.pb()
                    for hd in range(4):
                        hs = slice(hd * 128, (hd + 1) * 128)
                        self.mm(po, po.ap[:, hs], Sb.t[:, hd, :], qdT.t[:, hd, :], r=[Sb, qdT], start=True, stop=False)
                        self.mm(po, po.ap[:, hs], vnew.t[:, hd, :], attT.t[:, hd, :], r=[vnew, attT], start=False, stop=True)
                        self.mm(pkv, pkv.ap[:, hs], kdec.t[:, hd, :], vnew.t[:, hd, :], r=[kdec, vnew])
                    for hd in range(4):
                        hs = slice(hd * 128, (hd + 1) * 128)
                        S.op("dve", lambda e, hd=hd, hs=hs, pkv=pkv: e.scalar_tensor_tensor(out=Sf.t[:, hd, :], in0=Sf.t[:, hd, :], scalar=egB.t[:, hd, 127:128],
                                                                                          in1=pkv.ap[:, hs], op0=ALU.mult, op1=ALU.add),
                             r=[pkv, Sf, egB], w=[Sf])
                    S.op("act", lambda e: e.copy(Sb.ap, Sf.ap), r=[Sf], w=[Sb])
                    S.op("act", lambda e, po=po: e.copy(osb.ap.rearrange("p a b -> p (a b)"), po.ap), r=[po], w=[osb])
                    S.op("act", lambda e, po=po: e.activation(out=tb[1].ap, in_=po.ap, func=AF.Square), r=[po], w=[tb[1]])
                    pn = self.pb()
                    self.mm(pn, pn.ap, self.onesb, tb[1].ap, r=[tb[1], self.Bcstb])
                    S.op("act", lambda e, pn=pn: e.activation(out=tf[2].ap, in_=pn.ap, func=AF.Sqrt, scale=1.0 / 128, bias=self.eps_ap), r=[pn, self.Bcst], w=[tf[2]])
                    S.op("dve", lambda e: e.reciprocal(tf[2].ap, tf[2].ap), r=[tf[2]], w=[tf[2]])
                    S.op("dve", lambda e: e.scalar_tensor_tensor(out=tf[2].ap, in0=osb.ap.rearrange("p a b -> p (a b)"), scalar=pc("gnw"), in1=tf[2].ap,
                                                                 op0=ALU.mult, op1=ALU.mult), r=[osb, tf[2], BP], w=[tf[2]])
                    for hd in range(4):
                        S.op("pool" if hd % 2 else "dve", lambda e, hd=hd: e.tensor_tensor(mixT[:, 4 + hd, c0:c0 + 128], tf[2].t[:, hd * 128:(hd + 1) * 128], sg.t[:, hd, csl], ALU.mult),
                             r=[tf[2], sg], w=[Bmix])
            S.barrier()

    def neumann2(self, tag, Nn, Mm, N2, M2, Q, nb, f32=False):
        S = self.S
        idb = (self.ident if f32 else self.identb).unsqueeze(1).to_broadcast([128, nb, 128])
        S.op("pool", lambda e: e.tensor_tensor(Q.ap, Mm.ap, idb, ALU.add), r=[Mm, self.Bcst if f32 else self.Bcstb], w=[Q])
        cur = (Nn, Mm)
        nxt = (N2, M2)
        W = nb * 128
        for lev in range(6):
            last = (lev == 5)
            pn = self.pb()
            for b in range(nb):
                self.mm(pn, pn.ap[:, b * 128:(b + 1) * 128], cur[1].t[:, b, :], cur[0].t[:, b, :], r=[cur[0], cur[1]])
            S.op("act", lambda e, pn=pn, o=nxt[0]: e.copy(o.ap.rearrange("p a b -> p (a b)"), pn.ap[:, 0:W]), r=[pn], w=[nxt[0]])
            if not last:
                pm = self.pb()
                for b in range(nb):
                    self.mm(pm, pm.ap[:, b * 128:(b + 1) * 128], cur[0].t[:, b, :], cur[1].t[:, b, :], r=[cur[0], cur[1]])
                S.op("dve", lambda e, pm=pm, o=nxt[1]: e.tensor_copy(o.ap.rearrange("p a b -> p (a b)"), pm.ap[:, 0:W]), r=[pm], w=[nxt[1]])
            pq = self.pb()
            for b in range(nb):
                self.mm(pq, pq.ap[:, b * 128:(b + 1) * 128], nxt[0].t[:, b, :], Q.t[:, b, :], r=[nxt[0], Q])
            S.op("dve", lambda e, pq=pq: e.tensor_tensor(Q.ap.rearrange("p a b -> p (a b)"), pq.ap[:, 0:W], Q.ap.rearrange("p a b -> p (a b)"), ALU.add),
                 r=[pq, Q], w=[Q])
            cur, nxt = nxt, cur

    def sincos(self, es, tag, ang, n, out_s, out_c, shape):
        S = self.S
        TWO_PI = 6.283185307179586
        es = self.es_sc = ExitStack()
        k = self.nb(tag + "_k", [64, n], F32, es)
        ki = self.nb(tag + "_ki", [64, n], mybir.dt.int32, es)
        r = self.nb(tag + "_r", [64, n], F32, es)
        m = self.nb(tag + "_m", [64, n], F32, es)
        for (off, out) in ((0.0, out_s), (1.5707963267948966, out_c)):
            S.op("dve", lambda e: e.tensor_scalar(k.ap, ang.ap, off, 1.0 / TWO_PI, ALU.add, ALU.mult), r=[ang], w=[k])
            S.op("dve", lambda e: e.tensor_copy(ki.ap, k.ap), r=[k], w=[ki])
            S.op("dve", lambda e: e.tensor_copy(k.ap, ki.ap), r=[ki], w=[k])
            S.op("dve", lambda e: e.tensor_scalar(r.ap, ang.ap, off, None, ALU.add), r=[ang], w=[r])
            S.op("dve", lambda e: e.scalar_tensor_tensor(out=r.ap, in0=k.ap, scalar=-TWO_PI, in1=r.ap, op0=ALU.mult, op1=ALU.add), r=[k, r], w=[r])
            S.op("dve", lambda e: e.tensor_scalar(m.ap, r.ap, 3.141592653589793, None, ALU.is_gt), r=[r], w=[m])
            S.op("dve", lambda e: e.scalar_tensor_tensor(out=r.ap, in0=m.ap, scalar=-TWO_PI, in1=r.ap, op0=ALU.mult, op1=ALU.add), r=[m, r], w=[r])
            S.op("dve", lambda e: e.tensor_scalar(m.ap, r.ap, -3.141592653589793, None, ALU.is_lt), r=[r], w=[m])
            S.op("dve", lambda e: e.scalar_tensor_tensor(out=r.ap, in0=m.ap, scalar=TWO_PI, in1=r.ap, op0=ALU.mult, op1=ALU.add), r=[m, r], w=[r])
            S.op("dve", lambda e: e.tensor_scalar(r.ap, r.ap, 3.1415925, -3.1415925, ALU.min, ALU.max), r=[r], w=[r])
            S.op("act", lambda e, out=out: e.activation(out=out.ap, in_=r.ap, func=AF.Sin), r=[r], w=[out])
        S.barrier()
        es.close()

    def cmul(self, o_re, o_im, a_re, a_im, b_re, b_im, t1, t2, r, w, neg_im=False):
        S = self.S
        S.op("dve", lambda e: e.tensor_tensor(t1, a_re, b_re, ALU.mult), r=r, w=w)
        S.op("pool", lambda e: e.tensor_tensor(t2, a_im, b_im, ALU.mult), r=r, w=w)
        S.op("dve", lambda e: e.tensor_tensor(o_re, t1, t2, ALU.subtract), r=r + w, w=w)
        S.op("dve", lambda e: e.tensor_tensor(t1, a_re, b_im, ALU.mult), r=r + w, w=w)
        S.op("pool", lambda e: e.tensor_tensor(t2, a_im, b_re, ALU.mult), r=r + w, w=w)
        if neg_im:
            S.op("dve", lambda e: e.scalar_tensor_tensor(out=o_im, in0=t1, scalar=-1.0, in1=t2, op0=ALU.mult, op1=ALU.subtract), r=r + w, w=w)
        else:
            S.op("dve", lambda e: e.tensor_tensor(o_im, t1, t2, ALU.add), r=r + w, w=w)

    def s5_setup(self, l):
        S = self.S
        cst = self.cst
        with ExitStack() as es:
            nb = lambda n, s, dt=F32: self.nb("s5s_" + n, s, dt, es)
            sp = nb("sp", [64, 1328])
            S.dma("par", sp.ap, self.s5p_d[l], w=[sp])
            TAU = cst[0:64, 2816:2880]
            MIDX = cst[0:64, 2880:2944]
            M0 = cst[0:64, 2944:3008]
            are, aim, ldt = sp.t[:, 0:16], sp.t[:, 16:32], sp.t[:, 32:48]
            g4 = lambda ap, a, b: ap.rearrange("p (a b) -> p a b", b=b)
            bre, bim = g4(sp.t[:, 48:304], 16, 16), g4(sp.t[:, 304:560], 16, 16)
            cre, cim = g4(sp.t[:, 560:816], 16, 16), g4(sp.t[:, 816:1072], 16, 16)
            sm = nb("sm", [64, 16, 16])
            sl = lambda i: sm.t[:, i, :]
            S.op("act", lambda e: e.activation(out=sl(0), in_=ldt, func=AF.Exp), r=[sp], w=[sm])
            S.op("dve", lambda e: e.tensor_tensor(sl(1), sl(0), are, ALU.mult), r=[sm, sp], w=[sm])
            S.op("dve", lambda e: e.tensor_tensor(sl(2), sl(0), aim, ALU.mult), r=[sm, sp], w=[sm])
            ang = nb("ang", [64, 1024])
            mag = nb("mag", [64, 1024])
            pwr = nb("pwr", [64, 1024])
            pwi = nb("pwi", [64, 1024])
            v3 = lambda b: b.ap.rearrange("p (a b) -> p a b", b=64)
            taub = TAU.unsqueeze(1).to_broadcast([64, 16, 64])
            S.op("dve", lambda e: e.tensor_tensor(v3(ang), sl(2).unsqueeze(2).to_broadcast([64, 16, 64]), taub, ALU.mult), r=[sm, self.Bcst], w=[ang])
            S.op("dve", lambda e: e.tensor_tensor(v3(mag), sl(1).unsqueeze(2).to_broadcast([64, 16, 64]), taub, ALU.mult), r=[sm, self.Bcst], w=[mag])
            S.op("act", lambda e: e.activation(out=mag.ap, in_=mag.ap, func=AF.Exp), r=[mag], w=[mag])
            self.sincos(es, "sc1", ang, 1024, pwi, pwr, None)
            S.op("dve", lambda e: e.tensor_tensor(pwr.ap, pwr.ap, mag.ap, ALU.mult), r=[pwr, mag], w=[pwr])
            S.op("dve", lambda e: e.tensor_tensor(pwi.ap, pwi.ap, mag.ap, ALU.mult), r=[pwi, mag], w=[pwi])
            PWr, PWi = v3(pwr), v3(pwi)
            a1r, a1i = PWr[:, :, 48], PWi[:, :, 48]
            S.op("dve", lambda e: e.tensor_scalar(sl(3), a1r, -1.0, None, ALU.add), r=[pwr], w=[sm])
            S.op("dve", lambda e: e.tensor_tensor(sl(4), are, are, ALU.mult), r=[sp], w=[sm])
            S.op("dve", lambda e: e.tensor_tensor(sl(5), aim, aim, ALU.mult), r=[sp], w=[sm])
            S.op("dve", lambda e: e.tensor_tensor(sl(4), sl(4), sl(5), ALU.add), r=[sm], w=[sm])
            S.op("dve", lambda e: e.reciprocal(sl(4), sl(4)), r=[sm], w=[sm])
            S.op("dve", lambda e: e.tensor_tensor(sl(5), sl(3), are, ALU.mult), r=[sm, sp], w=[sm])
            S.op("dve", lambda e: e.tensor_tensor(sl(6), a1i, aim, ALU.mult), r=[pwi, sp], w=[sm])
            S.op("dve", lambda e: e.tensor_tensor(sl(5), sl(5), sl(6), ALU.add), r=[sm], w=[sm])
            S.op("dve", lambda e: e.tensor_tensor(sl(5), sl(5), sl(4), ALU.mult), r=[sm], w=[sm])
            S.op("dve", lambda e: e.tensor_tensor(sl(6), a1i, are, ALU.mult), r=[pwi, sp], w=[sm])
            S.op("dve", lambda e: e.tensor_tensor(sl(7), sl(3), aim, ALU.mult), r=[sm, sp], w=[sm])
            S.op("dve", lambda e: e.tensor_tensor(sl(6), sl(6), sl(7), ALU.subtract), r=[sm], w=[sm])
            S.op("dve", lambda e: e.tensor_tensor(sl(6), sl(6), sl(4), ALU.mult), r=[sm], w=[sm])
            bbr = nb("bbr", [64, 16, 16])
            bbi = nb("bbi", [64, 16, 16])
            ta = nb("ta", [64, 2048])
            tb_ = nb("tb", [64, 2048])
            bc = lambda ap: ap.unsqueeze(2).to_broadcast([64, 16, 16])
            t3 = lambda b: b.t[:, 0:256].rearrange("p (a b) -> p a b", b=16)
            self.cmul(bbr.ap, bbi.ap, bc(sl(5)), bc(sl(6)), bre, bim, t3(ta), t3(tb_), [sm, sp], [bbr, bbi, ta, tb_])
            big_r = nb("big_r", [64, 8, 16, 16])
            big_i = nb("big_i", [64, 8, 16, 16])
            rm_r = nb("rm_r", [64, 8, 16, 16])
            rm_i = nb("rm_i", [64, 8, 16, 16])
            t4 = lambda b: b.ap.rearrange("p (a b c) -> p a b c", b=16, c=16)
            tabs = self.s5tb_s[l]
            stg = nb("stg", [128, 4096], BF16)
            Bbig = [pwr, pwi, bbr, bbi, sp]
            for gh in range(2):
                gs = slice(gh * 8, (gh + 1) * 8)
                pw4 = lambda T, i0: T[:, gs, i0:i0 + 16].unsqueeze(3).to_broadcast([64, 8, 16, 16])
                x4 = lambda ap: ap[:, gs, :].unsqueeze(2).to_broadcast([64, 8, 16, 16])
                self.cmul(big_r.ap, big_i.ap, pw4(PWr, 0), pw4(PWi, 0), x4(bbr.ap), x4(bbi.ap), t4(ta), t4(tb_), Bbig, [big_r, big_i, ta, tb_])
                for ri, big in enumerate((big_r, big_i)):
                    for kt in range(2):
                        ps = self.pb()
                        for gg in range(8):
                            self.tr(ps, ps.ap[:, gg * 64:(gg + 1) * 64], big.t[:, gg, kt * 8:(kt + 1) * 8, :].rearrange("p a b -> p (a b)"), r=[big], f32=True)
                        dst = stg.t[:, (ri * 2 + kt) * 512:(ri * 2 + kt + 1) * 512]
                        S.op("act", lambda e, ps=ps, dst=dst: e.copy(dst, ps.ap), r=[ps], w=[stg])
                for ri in range(2):
                    for kt in range(2):
                        o0 = ri * 2048 + kt * 1024 + gh * 512
                        S.dma("s5t", tabs[:, o0:o0 + 512], stg.t[:, (ri * 2 + kt) * 512:(ri * 2 + kt + 1) * 512], r=[stg])
                self.cmul(big_r.ap, big_i.ap, pw4(PWr, 16), pw4(PWi, 16), x4(bbr.ap), x4(bbi.ap), t4(ta), t4(tb_), Bbig, [big_r, big_i, ta, tb_])
                self.cmul(rm_r.ap, rm_i.ap, pw4(PWr, 32), pw4(PWi, 32), x4(cre), x4(cim), t4(ta), t4(tb_), Bbig, [rm_r, rm_i, ta, tb_], neg_im=True)
                for kt in range(2):
                    MK = cst[:, 2304 + kt * 256: 2304 + (kt + 1) * 256]
                    for g2 in range(4):
                        ps = self.pb()
                        for gg in range(2):
                            g = g2 * 2 + gg
                            lr = big_r.t[:, g, kt * 8:(kt + 1) * 8, :].rearrange("p a b -> p (a b)")
                            li = big_i.t[:, g, kt * 8:(kt + 1) * 8, :].rearrange("p a b -> p (a b)")
                            self.mm(ps, ps.ap[:, gg * 256:(gg + 1) * 256], lr, rm_r.t[:, g, :, :].rearrange("p a b -> p (a b)"), r=[big_r, rm_r], start=True, stop=False)
                            self.mm(ps, ps.ap[:, gg * 256:(gg + 1) * 256], li, rm_i.t[:, g, :, :].rearrange("p a b -> p (a b)"), r=[big_i, rm_i], start=False, stop=True)
                        dst = stg.t[:, kt * 2048 + g2 * 512: kt * 2048 + (g2 + 1) * 512].rearrange("p (a b) -> p a b", b=256)
                        S.op("dve", lambda e, ps=ps, dst=dst, MK=MK: e.tensor_tensor(dst, ps.ap.rearrange("p (a b) -> p a b", b=256),
                                                                                   MK.unsqueeze(1).to_broadcast([128, 2, 256]), ALU.mult),
                             r=[ps, self.Bcst], w=[stg])
                    o0 = 4096 + kt * 4096 + gh * 2048
                    S.dma("s5t", tabs[:, o0:o0 + 2048], stg.t[:, kt * 2048:(kt + 1) * 2048], r=[stg])
                self.cmul(big_r.ap, big_i.ap, pw4(PWr, 48), pw4(PWi, 48), x4(cre), x4(cim), t4(ta), t4(tb_), Bbig, [big_r, big_i, ta, tb_], neg_im=True)
                S.op("act", lambda e: e.copy(stg.t[0:64, 0:2048], big_r.ap.rearrange("p a b c -> p (a b c)")), r=[big_r], w=[stg])
                S.op("act", lambda e: e.copy(stg.t[0:64, 2048:4096], big_i.ap.rearrange("p a b c -> p (a b c)")), r=[big_i], w=[stg])
                for ri in range(2):
                    o0 = 12288 + ri * 4096 + gh * 2048
                    S.dma("s5t", tabs[0:64, o0:o0 + 2048], stg.t[0:64, ri * 2048:(ri + 1) * 2048], r=[stg])
            ft = nb("ft", [64, 3 * 1024 + 64])
            S.op("pool", lambda e: e.memset(ft.ap, 0.0), w=[ft])
            S.op("act", lambda e: e.activation(out=sl(8), in_=sl(1), func=AF.Exp, scale=16.0), r=[sm], w=[sm])
            a16 = nb("a16", [64, 16])
            s16 = nb("s16", [64, 16])
            c16 = nb("c16", [64, 16])
            S.op("dve", lambda e: e.tensor_scalar(a16.ap, sl(2), 16.0, None, ALU.mult), r=[sm], w=[a16])
            self.sincos(es, "sc2", a16, 16, s16, c16, None)
            cr = ft.t[:, 0:1024].rearrange("p (a b) -> p a b", b=64)
            sr = ft.t[:, 1024:2048].rearrange("p (a b) -> p a b", b=64)
            S.op("dve", lambda e: e.memset(cr[:, :, 0:1], 1.0), w=[ft])
            S.op("dve", lambda e: e.memset(sr[:, :, 0:1], 0.0), w=[ft])
            S.op("dve", lambda e: e.tensor_copy(cr[:, :, 1], c16.ap), r=[c16], w=[ft])
            S.op("dve", lambda e: e.tensor_copy(sr[:, :, 1], s16.ap), r=[s16], w=[ft])
            pr = nb("pr", [64, 16])
            pi_ = nb("pi", [64, 16])
            S.op("dve", lambda e: e.tensor_copy(pr.ap, c16.ap), r=[c16], w=[pr])
            S.op("dve", lambda e: e.tensor_copy(pi_.ap, s16.ap), r=[s16], w=[pi_])
            tq = nb("tq", [64, 16, 32])
            tq2 = nb("tq2", [64, 16, 32])
            n = 2
            while n < 128:
                S.op("dve", lambda e: e.tensor_tensor(sl(9), pr.ap, pr.ap, ALU.mult), r=[pr], w=[sm])
                S.op("dve", lambda e: e.tensor_tensor(sl(10), pi_.ap, pi_.ap, ALU.mult), r=[pi_], w=[sm])
                S.op("dve", lambda e: e.tensor_tensor(sl(11), pr.ap, pi_.ap, ALU.mult), r=[pr, pi_], w=[sm])
                S.op("dve", lambda e: e.tensor_tensor(pr.ap, sl(9), sl(10), ALU.subtract), r=[sm], w=[pr])
                S.op("dve", lambda e: e.tensor_scalar(pi_.ap, sl(11), 2.0, None, ALU.mult), r=[sm], w=[pi_])
                if n == 64:
                    break
                bcp = lambda ap, n=n: ap.unsqueeze(2).to_broadcast([64, 16, n])
                self.cmul(cr[:, :, n:2 * n], sr[:, :, n:2 * n], cr[:, :, 0:n], sr[:, :, 0:n], bcp(pr.ap), bcp(pi_.ap),
                          tq.t[:, :, 0:n], tq2.t[:, :, 0:n], [ft, pr, pi_], [ft, tq, tq2])
                n *= 2
            rho = ft.t[:, 2048:3072].rearrange("p (a b) -> p a b", b=64)
            S.op("dve", lambda e: e.tensor_tensor(rho, sl(8).unsqueeze(2).to_broadcast([64, 16, 64]), M0.unsqueeze(1).to_broadcast([64, 16, 64]), ALU.mult),
                 r=[sm, self.Bcst], w=[ft])
            S.op("dve", lambda e: e.tensor_copy(ft.t[:, 3072:3088], sl(8)), r=[sm], w=[ft])
            S.op("dve", lambda e: e.tensor_copy(ft.t[:, 3088:3104], pr.ap), r=[pr], w=[ft])
            S.op("dve", lambda e: e.tensor_copy(ft.t[:, 3104:3120], pi_.ap), r=[pi_], w=[ft])
            S.dma("s5t", self.s5tf_s[l], ft.ap, r=[ft])
            S.barrier()

    def s5(self, l, tt, hT, Bh, mixT, Bmix):
        S = self.S
        P = self.par[l]
        BP = self.Bpar[l]
        pc = lambda name, i=0, n=1: P[:, PO[name] + i: PO[name] + i + n]
        TB = self.s5tb_s[l]
        with ExitStack() as es:
            nb = lambda n, s, dt=F32, es_=es: self.nb("s5_" + n, s, dt, es_)
            Ub = nb("Ub", [64, 16, 16, 16], BF16)
            UT = nb("UT", [128, 16, 2, 64], BF16)
            Xb = [nb(f"Xb{ri}", [64, 16, 65], BF16) for ri in range(2)]
            zbm = nb("zbm", [64, 16, 256], BF16)
            E = [nb(f"E{ri}", [64, 1024]) for ri in range(2)]
            Zc = self.s5_Zc[l]
            Xc = self.s5_Xc[l]
            dbc = self.s5d[l]
            with ExitStack() as esa:
                w1t = nb("w1t", [128, 4096], BF16, esa)
                S.dma("s5l", w1t.ap, TB[:, 0:4096], w=[w1t])
                W1 = lambda ri, kt, g: w1t.t[:, ri * 2048 + kt * 1024 + g * 64: ri * 2048 + kt * 1024 + (g + 1) * 64]
                sl0 = self.ring_load(self.win_s[l * NCH + 8][:, 0:1024], 1024)
                sl1 = self.ring_load(self.win_s[l * NCH + 9][:, 0:1024], 1024)
                for i in range(16):
                    ps = self.pb()
                    for half, slot in ((0, sl0), (1, sl1)):
                        wt = self.wring_t[slot]
                        for dt in range(8):
                            self.mm(ps, ps.ap[0:64, half * 128:(half + 1) * 128], hT[:, dt, i:TT:16], wt[:, dt * 128:(dt + 1) * 128],
                                    r=[self.wring[slot], Bh], start=(dt == 0), stop=(dt == 7))
                    eng = "act" if i % 2 == 0 else "dve"
                    src_ = ps.ap[0:64, 0:256].rearrange("p (g h) -> p g h", h=16)
                    if eng == "act":
                        S.op("act", lambda e, i=i, src_=src_: e.copy(Ub.t[:, :, i, :], src_), r=[ps], w=[Ub])
                    else:
                        S.op("dve", lambda e, i=i, src_=src_: e.tensor_copy(Ub.t[:, :, i, :], src_), r=[ps], w=[Ub])
                for g8 in range(2):
                    ps = self.pb()
                    psb = ps.ap.bitcast(BF16)
                    for gg in range(8):
                        g = g8 * 8 + gg
                        for kt in range(2):
                            self.tr(ps, psb[:, (gg * 2 + kt) * 64:(gg * 2 + kt + 1) * 64],
                                    Ub.t[:, g, kt * 8:(kt + 1) * 8, :].rearrange("p a b -> p (a b)"), r=[Ub])
                    dst = UT.t[:, g8 * 8:(g8 + 1) * 8, :, :].rearrange("p a b c -> p (a b c)")
                    if g8 == 0:
                        S.op("act", lambda e, psb=psb, dst=dst: e.copy(dst, psb[:, 0:1024]), r=[ps], w=[UT])
                    else:
                        S.op("dve", lambda e, psb=psb, dst=dst: e.tensor_copy(dst, psb[:, 0:1024]), r=[ps], w=[UT])
                for ri in range(2):
                    for g8 in range(2):
                        ps = self.pb()
                        for gg in range(8):
                            g = g8 * 8 + gg
                            for kt in range(2):
                                self.mm(ps, ps.ap[0:64, gg * 64:(gg + 1) * 64], W1(ri, kt, g), UT.t[:, g, kt, :], r=[w1t, UT], start=(kt == 0), stop=(kt == 1))
                        S.op("act", lambda e, ps=ps, ri=ri, g8=g8: e.copy(E[ri].t[:, g8 * 512:(g8 + 1) * 512], ps.ap[0:64, :]), r=[ps], w=[E[ri]])
                S.barrier()
            with ExitStack() as esb:
                ft = nb("ft", [64, 3136], F32, esb)
                Et = [nb(f"Et{ri}", [64, 1024], F32, esb) for ri in range(2)]
                t1 = nb("t1", [64, 1024], F32, esb)
                t2 = nb("t2", [64, 1024], F32, esb)
                S.dma("s5l", ft.ap, self.s5tf_s[l], w=[ft])
                cr = ft.t[:, 0:1024]
                sr = ft.t[:, 1024:2048]
                RHO = ft.t[:, 2048:3072]
                rho16 = ft.t[:, 3072:3088]
                c64 = ft.t[:, 3088:3104]
                s64 = ft.t[:, 3104:3120]
                Zs = E
                S.op("dve", lambda e: e.tensor_tensor(t1.ap, cr, E[0].ap, ALU.mult), r=[ft, E[0]], w=[t1])
                S.op("pool", lambda e: e.tensor_tensor(t2.ap, sr, E[1].ap, ALU.mult), r=[ft, E[1]], w=[t2])
                S.op("dve", lambda e: e.tensor_tensor(Et[0].ap, t1.ap, t2.ap, ALU.add), r=[t1, t2], w=[Et[0]])
                S.op("dve", lambda e: e.tensor_tensor(t1.ap, cr, E[1].ap, ALU.mult), r=[ft, E[1], Et[0]], w=[t1])
                S.op("pool", lambda e: e.tensor_tensor(t2.ap, sr, E[0].ap, ALU.mult), r=[ft, E[0], Et[0]], w=[t2])
                S.op("dve", lambda e: e.tensor_tensor(Et[1].ap, t1.ap, t2.ap, ALU.subtract), r=[t1, t2], w=[Et[1]])
                for ri in range(2):
                    e0 = Et[ri].ap.rearrange("p (g m) -> p g m", m=64)[:, :, 0]
                    S.op("dve", lambda e, ri=ri: e.tensor_tensor(t1.t[:, 0:16], rho16, Zc.t[:, ri, :], ALU.mult), r=[ft, Zc, Et[1]], w=[t1])
                    S.op("dve", lambda e, e0=e0: e.tensor_tensor(e0, e0, t1.t[:, 0:16], ALU.add), r=[t1, Et[ri]], w=[Et[ri]])
                    S.op("dve", lambda e, ri=ri: e.tensor_tensor_scan(Zs[ri].ap, RHO, Et[ri].ap, 0.0, ALU.mult, ALU.add), r=[ft, Et[ri], E[0], E[1]], w=[Zs[ri]])
                z3 = lambda b_: b_.ap.rearrange("p (g m) -> p g m", m=64)
                S.op("dve", lambda e: e.tensor_tensor(t1.ap, cr, Zs[0].ap, ALU.mult), r=[ft, Zs[0]], w=[t1])
                S.op("pool", lambda e: e.tensor_tensor(t2.ap, sr, Zs[1].ap, ALU.mult), r=[ft, Zs[1]], w=[t2])
                S.op("act", lambda e: e.copy(Xb[0].t[:, :, 0], Xc.t[:, 0, :]), r=[Xc], w=[Xb[0]])
                S.op("act", lambda e: e.copy(Xb[1].t[:, :, 0], Xc.t[:, 1, :]), r=[Xc], w=[Xb[1]])
                S.op("dve", lambda e: e.tensor_tensor(Xb[0].t[:, :, 1:65], z3(t1), z3(t2), ALU.subtract), r=[t1, t2], w=[Xb[0]])
                S.op("dve", lambda e: e.tensor_tensor(t1.ap, cr, Zs[1].ap, ALU.mult), r=[ft, Zs[1], Xb[0]], w=[t1])
                S.op("pool", lambda e: e.tensor_tensor(t2.ap, sr, Zs[0].ap, ALU.mult), r=[ft, Zs[0], Xb[0]], w=[t2])
                S.op("dve", lambda e: e.tensor_tensor(Xb[1].t[:, :, 1:65], z3(t1), z3(t2), ALU.add), r=[t1, t2], w=[Xb[1]])
                S.op("act", lambda e: e.copy(Xc.t[:, 0, :], Xb[0].t[:, :, 64]), r=[Xb[0]], w=[Xc])
                S.op("act", lambda e: e.copy(Xc.t[:, 1, :], Xb[1].t[:, :, 64]), r=[Xb[1]], w=[Xc])
                zl = lambda ri: z3(Zs[ri])[:, :, 63]
                S.op("dve", lambda e: e.tensor_tensor(t1.t[:, 0:16], c64, zl(0), ALU.mult), r=[ft, Zs[0], Xb[1]], w=[t1])
                S.op("dve", lambda e: e.tensor_tensor(t1.t[:, 16:32], s64, zl(1), ALU.mult), r=[ft, Zs[1]], w=[t1])
                S.op("dve", lambda e: e.tensor_tensor(t1.t[:, 32:48], c64, zl(1), ALU.mult), r=[ft, Zs[1]], w=[t1])
                S.op("dve", lambda e: e.tensor_tensor(t1.t[:, 48:64], s64, zl(0), ALU.mult), r=[ft, Zs[0]], w=[t1])
                S.op("dve", lambda e: e.tensor_tensor(Zc.t[:, 0, :], t1.t[:, 0:16], t1.t[:, 16:32], ALU.subtract), r=[t1], w=[Zc])
                S.op("dve", lambda e: e.tensor_tensor(Zc.t[:, 1, :], t1.t[:, 32:48], t1.t[:, 48:64], ALU.add), r=[t1], w=[Zc])
                S.barrier()
            with ExitStack() as esc:
                toe = nb("toe", [128, 8192], BF16, esc)
                w2t = nb("w2t", [64, 8192], BF16, esc)
                yv = [nb(f"yv{i}", [64, 512], F32, esc) for i in range(3)]
                S.dma("s5l", toe.ap, TB[:, 4096:12288], w=[toe])
                S.dma("s5l", w2t.ap, TB[0:64, 12288:20480], w=[w2t])
                TOE = lambda kt, g: toe.t[:, kt * 4096 + g * 256: kt * 4096 + (g + 1) * 256]
                W2 = lambda ri, g: w2t.t[:, ri * 4096 + g * 256: ri * 4096 + (g + 1) * 256]
                v4 = lambda ap: ap.rearrange("p (g i h) -> p g i h", i=16, h=16)
                for g2 in range(8):
                    ps = self.pb()
                    for gg in range(2):
                        g = g2 * 2 + gg
                        o = ps.ap[0:64, gg * 256:(gg + 1) * 256]
                        self.mm(ps, o, UT.t[:, g, 0, :], TOE(0, g), r=[UT, toe], start=True, stop=False)
                        self.mm(ps, o, UT.t[:, g, 1, :], TOE(1, g), r=[UT, toe], start=False, stop=False)
                        self.mm(ps, o, Xb[0].t[:, g, 0:64], W2(0, g), r=[Xb[0], w2t], start=False, stop=False)
                        self.mm(ps, o, Xb[1].t[:, g, 0:64], W2(1, g), r=[Xb[1], w2t], start=False, stop=True)
                    y, t_, s_ = yv
                    dview = dbc.t[:, g2 * 32:(g2 + 1) * 32].rearrange("p (g h) -> p g h", h=16).unsqueeze(2).to_broadcast([64, 2, 16, 16])
                    S.op("pool", lambda e: e.tensor_tensor(v4(t_.ap), Ub.t[:, g2 * 2:(g2 + 1) * 2, :, :], dview, ALU.mult), r=[Ub, dbc], w=[t_])
                    S.op("dve", lambda e, ps=ps: e.tensor_tensor(y.ap, ps.ap[0:64, :], t_.ap, ALU.add), r=[ps, t_], w=[y])
                    S.op("act", lambda e: e.activation(out=t_.ap, in_=y.ap, func=AF.Square), r=[y], w=[t_])
                    S.op("dve", lambda e: e.tensor_scalar(t_.ap, t_.ap, 0.044715, 1.0, ALU.mult, ALU.add), r=[t_], w=[t_])
                    S.op("dve", lambda e: e.tensor_tensor(t_.ap, t_.ap, y.ap, ALU.mult), r=[t_, y], w=[t_])
                    S.op("act", lambda e: e.activation(out=s_.ap, in_=t_.ap, func=AF.Sigmoid, scale=1.5957691216057308), r=[t_], w=[s_])
                    dst = zbm.t[:, :, g2 * 32:(g2 + 1) * 32].rearrange("p i (g h) -> p g i h", h=16)
                    S.op("dve", lambda e, dst=dst: e.tensor_tensor(dst, v4(s_.ap), v4(y.ap), ALU.mult), r=[s_, y], w=[zbm])
                S.barrier()
            with ExitStack() as esd:
                zT = nb("zT", [128, 2, TT], BF16, esd)
                sgl = nb("sgl", [128, 512], F32, esd)
                for ct in range(2):
                    ps = self.pb()
                    psb = ps.ap.bitcast(BF16)
                    for i in range(16):
                        self.tr(ps, psb[:, i * 64:(i + 1) * 64], zbm.t[:, i, ct * 128:(ct + 1) * 128], r=[zbm])
                    S.op("act", lambda e, psb=psb, ct=ct: e.copy(zT.t[:, ct, :].rearrange("p (j i) -> p i j", i=16),
                                                                 psb[:, 0:1024].rearrange("p (i j) -> p i j", j=64)), r=[ps], w=[zT])
                GW = self.lorab[l]
                for ct in range(2):
                    for h in range(2):
                        ps = self.pb()
                        for kt in range(2):
                            self.mm(ps, ps.ap, GW[:, 768 + kt * 256 + ct * 128: 768 + kt * 256 + (ct + 1) * 128], zT.t[:, kt, h * 512:(h + 1) * 512],
                                    r=[self.Blorab[l], zT], start=(kt == 0), stop=(kt == 1))
                        S.op("act", lambda e, ps=ps, ct=ct: e.activation(out=sgl.ap, in_=ps.ap, func=AF.Sigmoid, bias=pc("bglu", ct)), r=[ps, BP], w=[sgl])
                        S.op("dve", lambda e, ct=ct, h=h: e.tensor_tensor(mixT[:, 2 + ct, h * 512:(h + 1) * 512], zT.t[:, ct, h * 512:(h + 1) * 512], sgl.ap, ALU.mult),
                             r=[sgl, zT], w=[Bmix])
                S.barrier()

    def mixer(self, l, tt):
        S = self.S
        gi_pre = (l * 6 + 2) * 8
        gi_post = (l * 6 + 3) * 8
        with ExitStack() as es:
            hTb = self.nb("hT", [128, 8, TT], BF16, es)
            mixb = self.nb("mixT", [128, 8, TT], BF16, es)
            hT = hTb.t
            mixT = mixb.t
            with ExitStack() as es2:
                sq = self.nb("msq", [128, 2, 512], BF16, es2)
                rstd = self.nb("mrstd", [128, 512], F32, es2)
                for h in range(2):
                    ps = self.pb()
                    for d in range(8):
                        S.op("act", lambda e, d=d, h=h: e.activation(out=sq.t[:, d % 2, :], in_=self.xT[d][h].ap, func=AF.Square),
                             r=[self.xT[d][h]], w=[sq])
                        self.mm(ps, ps.ap, self.onesb, sq.t[:, d % 2, :], r=[sq, self.Bcstb], start=(d == 0), stop=(d == 7))
                    self.rstd_from_ps(ps, rstd)
                    for d in range(8):
                        S.op("dve", lambda e, d=d, h=h: e.scalar_tensor_tensor(
                            out=hT[:, d, h * 512:(h + 1) * 512], in0=self.xT[d][h].ap, scalar=self.gain[:, gi_pre + d: gi_pre + d + 1],
                            in1=rstd.ap, op0=ALU.mult, op1=ALU.mult), r=[self.xT[d][h], rstd, self.Bgain], w=[hTb])
                S.barrier()
            if not all(self.mix_enable) or self.cut:
                S.op("pool", lambda e: e.memset(mixb.ap, 0.0), w=[mixb])
            try:
                if self.mix_enable[0]:
                    self.rwkv(l, tt, hT, hTb, mixT, mixb)
                if self.mix_enable[1]:
                    self.s5(l, tt, hT, hTb, mixT, mixb)
                if self.mix_enable[2]:
                    self.gdn(l, tt, hT, hTb, mixT, mixb)
            except _Cut:
                pass
            if self.dbg_mix:
                S.dma("dbg", self.dbg_d, mixT[:, :, :].rearrange("p a b -> p (a b)"), r=[mixb])
            with ExitStack() as es2:
                ff = self.nb("mff", [128, 8, TT], F32, es2)
                sq = self.nb("msq2", [128, 2, 512], BF16, es2)
                rstd = [self.nb(f"mrstd2{h}", [128, 512], F32, es2) for h in range(2)]
                tmp = [self.nb(f"mtmp{i}", [128, 512], F32, es2) for i in range(2)]
                pss = [self.pb(), self.pb()]
                k = 0
                for m in range(8):
                    slot = self.ring_load(self.wout_s[l * 8 + m], 1024)
                    wt = self.wring_t[slot]
                    for h in range(2):
                        pp = self.pb()
                        while pp in pss:
                            pp = self.pb()
                        for ct in range(8):
                            self.mm(pp, pp.ap, wt[:, ct * 128:(ct + 1) * 128], mixT[:, ct, h * 512:(h + 1) * 512],
                                    r=[self.wring[slot], mixb], start=(ct == 0), stop=(ct == 7))
                        S.op("act", lambda e, m=m, h=h, pp=pp: e.copy(ff.t[:, m, h * 512:(h + 1) * 512], pp.ap), r=[pp], w=[ff])
                        S.op("act", lambda e, pp=pp, k=k: e.activation(out=sq.t[:, k % 2, :], in_=pp.ap, func=AF.Square), r=[pp], w=[sq])
                        self.mm(pss[h], pss[h].ap, self.onesb, sq.t[:, k % 2, :], r=[sq, self.Bcstb], start=(m == 0), stop=(m == 7))
                        k += 1
                for h in range(2):
                    self.rstd_from_ps(pss[h], rstd[h])
                    for d in range(8):
                        tB = tmp[d % 2]
                        S.op("dve", lambda e, d=d, h=h, tB=tB: e.scalar_tensor_tensor(
                            out=tB.ap, in0=ff.t[:, d, h * 512:(h + 1) * 512], scalar=self.gain[:, gi_post + d: gi_post + d + 1],
                            in1=rstd[h].ap, op0=ALU.mult, op1=ALU.mult), r=[ff, rstd[h], self.Bgain], w=[tB])
                        S.op("pool", lambda e, d=d, h=h, tB=tB: e.tensor_tensor(self.xT[d][h].ap, self.xT[d][h].ap, tB.ap, ALU.add),
                             r=[tB, self.xT[d][h]], w=[self.xT[d][h]])
                S.barrier()

    def mixer_setup(self):
        S = self.S
        nc = self.nc
        dram = lambda n, s, dt=F32, kind="ExternalInput": nc.dram_tensor(n, s, dt, kind=kind).ap()
        self.win_d = dram("w_in", [DEPTH, D, 3336])
        self.wvres_d = dram("w_vres", [D, 32])
        self.wout_d = dram("w_out", [DEPTH, D, D])
        self.par_d = dram("par", [DEPTH, 128, NPAR])
        self.lora_d = dram("lora", [DEPTH, 128, 1280])
        self.s5p_d = dram("s5p", [DEPTH, 64, 1328])
        self.win_s = dram("win_s", [DEPTH * NCH, 128, 1024], BF16, kind="Internal")
        self.wout_s = dram("wout_s", [DEPTH * 8, 128, 1024], BF16, kind="Internal")
        self.s5tb_s = dram("s5tb_s", [DEPTH, 128, 20480], BF16, kind="ExternalOutput" if self.dbg_mix else "Internal")
        self.s5tf_s = dram("s5tf_s", [DEPTH, 64, 3136], F32, kind="ExternalOutput" if self.dbg_mix else "Internal")
        if self.dbg_mix:
            self.dbg_d = dram("dbg", [128, 8 * TT], BF16, kind="ExternalOutput")
        self.par, self.Bpar, self.lorab, self.Blorab = [], [], [], []
        self.rw_Sb, self.rw_Sf, self.rw_halo = [], [], []
        self.gd_Sb, self.gd_Sf, self.gd_halo = [], [], []
        self.s5_Zc, self.s5_Xc, self.s5d = [], [], []
        self.vfirst = self.nb("vfirst", [128, 2, TT], BF16)
        self.rw_gc = self.nb("rw_gc", [128, 2, 4])
        pars, lbs = [], []
        for l in range(DEPTH):
            p = self.nb(f"par{l}", [128, NPAR])
            lb = self.nb(f"lorab{l}", [128, 1280], BF16)
            pars.append(p)
            lbs.append(lb)
            self.par.append(p.t)
            self.Bpar.append(p)
            self.lorab.append(lb.t)
            self.Blorab.append(lb)
            for (lstv, nm, shp, dt) in ((self.rw_Sb, "rwSb", [128, 2, 128], BF16), (self.rw_Sf, "rwSf", [128, 2, 128], F32),
                                        (self.rw_halo, "rwhalo", [128, 8], F32),
                                        (self.gd_Sb, "gdSb", [128, 4, 128], BF16), (self.gd_Sf, "gdSf", [128, 4, 128], F32),
                                        (self.gd_halo, "gdhalo", [128, 12, 3], BF16),
                                        (self.s5_Zc, "s5Zc", [64, 2, 16], F32), (self.s5_Xc, "s5Xc", [64, 2, 16], BF16)):
                b_ = self.nb(f"{nm}{l}", shp, dt)
                S.op("pool", lambda e, b_=b_: e.memset(b_.ap, 0.0), w=[b_])
                lstv.append(b_)
            d = self.nb(f"s5d{l}", [64, 256])
            S.dma("par", d.ap, self.s5p_d[l][:, 1072:1328], w=[d])
            self.s5d.append(d)
        with ExitStack() as es:
            lst = self.nb("lora_stage", [128, 1280], F32, es)
            for l in range(DEPTH):
                p = pars[l]
                lb = lbs[l]
                S.dma("par", p.ap, self.par_d[l], w=[p])
                pc = lambda name, i=0, n=1, p=p: p.t[:, PO[name] + i: PO[name] + i + n]
                S.op("dve", lambda e: e.tensor_scalar(pc("omk", 0, 2), pc("ka", 0, 2), -1.0, 1.0, ALU.mult, ALU.add), r=[p], w=[p])
                S.op("act", lambda e: e.activation(out=pc("nA", 0, 4), in_=pc("nA", 0, 4), func=AF.Exp), r=[p], w=[p])
                S.op("dve", lambda e: e.tensor_scalar(pc("nA", 0, 4), pc("nA", 0, 4), -1.0, None, ALU.mult), r=[p], w=[p])
                S.dma("par", lst.ap, self.lora_d[l], w=[lst])
                S.op("dve", lambda e: e.tensor_copy(lb.ap, lst.ap), r=[lst], w=[lb])
            S.barrier()
        for l in range(DEPTH):
            self.s5_setup(l)

    def extra_convert_jobs(self):
        jobs = []
        for l in range(DEPTH):
            for c, (c0, n) in enumerate(CHUNKS):
                if c == 27:
                    if l == 0:
                        continue
                    src = self.wvres_d[:, 0:32].rearrange("(dt p) c -> p dt c", p=128)
                else:
                    src = self.win_d[l][:, c0:c0 + n].rearrange("(dt p) c -> p dt c", p=128)
                dst = self.win_s[l * NCH + c][:, 0:8 * n]
                jobs.append((src, dst, 8, n))
            for m in range(8):
                src = self.wout_d[l][:, m * 128:(m + 1) * 128].rearrange("(ct p) d -> p ct d", p=128)
                jobs.append((src, self.wout_s[l * 8 + m], 8, 128))
        return jobs


def _pp(v, nt):
    return np.asarray(v, np.float32).reshape(nt, 128).T


def _shared_inputs(inputs):
    I = {k: np.asarray(v, np.float32) for k, v in inputs.items() if k != "x"}
    g = I["norm_gain"]
    gain = np.ascontiguousarray(g.reshape(DEPTH * 6, 8, 128).transpose(2, 0, 1).reshape(128, DEPTH * 6 * 8))
    par = np.zeros((DEPTH, 128, NPAR), np.float32)
    lora = np.zeros((DEPTH, 128, 1280), np.float32)
    s5p = np.zeros((DEPTH, 64, 1328), np.float32)
    for l in range(DEPTH):
        P = par[l]
        P[:, PO["mu"]:PO["mu"] + 8] = _pp(I["rwkv_mu"][l], 8)
        P[:, PO["w0"]:PO["w0"] + 2] = _pp(I["rwkv_w0"][l], 2)
        P[:, PO["a0"]:PO["a0"] + 2] = _pp(I["rwkv_a0"][l], 2)
        P[:, PO["kk"]:PO["kk"] + 2] = _pp(I["rwkv_k_k"][l], 2)
        P[:, PO["ka"]:PO["ka"] + 2] = _pp(I["rwkv_k_a"][l], 2)
        P[:, PO["rk"]:PO["rk"] + 2] = _pp(I["rwkv_r_k"][l].reshape(-1), 2)
        P[:, PO["lnw"]:PO["lnw"] + 2] = _pp(I["rwkv_ln_w"][l], 2)
        P[:, PO["lnb"]:PO["lnb"] + 2] = _pp(I["rwkv_ln_b"][l], 2)
        if l > 0:
            P[:, PO["v0"]:PO["v0"] + 2] = _pp(I["rwkv_v0"][l - 1], 2)
        cw = I["gdn_conv_w"][l]
        P[:, PO["conv"]:PO["conv"] + 48] = cw.reshape(4, 12, 128).transpose(2, 1, 0).reshape(128, 48)
        P[:, PO["gnw"]] = I["gdn_norm_w"][l]
        P[:, PO["nA"]:PO["nA"] + 4] = np.broadcast_to(I["gdn_a_log"][l][None, :], (128, 4))
        P[:, PO["dtb"]:PO["dtb"] + 4] = np.broadcast_to(I["gdn_dt_bias"][l][None, :], (128, 4))
        P[:, PO["bglu"]:PO["bglu"] + 2] = _pp(I["s5_b_glu"][l], 2)
        L = lora[l]
        L[0:64, 0:256] = I["rwkv_w_w2"][l]
        L[64:128, 0:256] = I["rwkv_w_a2"][l]
        L[:, 256:512] = I["rwkv_w_g2"][l]
        if l > 0:
            L[0:32, 512:768] = I["rwkv_w_v2"][l - 1]
        L[:, 768:1280] = I["s5_w_glu"][l].reshape(2, 128, 256).transpose(1, 0, 2).reshape(128, 512)
        s = s5p[l]
        s[:, 0:16] = I["s5_a_re"][l].T
        s[:, 16:32] = I["s5_a_im"][l].T
        s[:, 32:48] = np.broadcast_to(I["s5_log_dt"][l][None, :], (64, 16))
        s[:, 48:304] = I["s5_b_re"][l].transpose(1, 0, 2).reshape(64, 256)
        s[:, 304:560] = I["s5_b_im"][l].transpose(1, 0, 2).reshape(64, 256)
        s[:, 560:816] = I["s5_c_re"][l].transpose(2, 0, 1).reshape(64, 256)
        s[:, 816:1072] = I["s5_c_im"][l].transpose(2, 0, 1).reshape(64, 256)
        s[:, 1072:1328] = np.broadcast_to(I["s5_d"][l][None, :], (64, 256))
    return {
        "gain": gain,
        "w1": I["ffn_w1"].reshape(DEPTH * 2, D, DFF),
        "w3": I["ffn_w3"].reshape(DEPTH * 2, D, DFF),
        "w2": I["ffn_w2"].reshape(DEPTH * 2, DFF, D),
        "consts": _consts(),
        "w_in": I["w_in"], "w_vres": I["w_in_vres"][0], "w_out": I["w_out"],
        "par": par, "lora": lora, "s5p": s5p,
    }


def _host_inputs(inputs, b, shared):
    m = dict(shared)
    m["x"] = np.ascontiguousarray(np.asarray(inputs["x"][b], np.float32))
    return m


def run(inputs, n_cores=8, stop=None, ntt=NTT, mix_enable=(1, 1, 1), dbg_mix=False, ret_all=False, cut=None):
    kd = K(stop=stop, ntt=ntt, mix_enable=mix_enable, dbg_mix=dbg_mix)
    kd.cut = cut
    kd.build()
    needed = kd.S.needed
    kd.es.close()
    kb = K(stop=stop, ntt=ntt, mix_enable=mix_enable, dbg_mix=dbg_mix, needed=needed)
    kb.cut = cut
    nc = kb.build()
    print("sem incs:", len(needed), flush=True)
    print("instructions:", kb.S.ninst, flush=True)
    shared = _shared_inputs(inputs)
    in_maps = [_host_inputs(inputs, b, shared) for b in range(n_cores)]
    res = run_bass_kernel_spmd(nc, in_maps, core_ids=list(range(n_cores)))
    if ret_all:
        return res.results
    return np.stack([np.asarray(r["y"]) for r in res.results], axis=0)


def kernel(**inputs):
    out = run(inputs, 8)
    return out.astype(np.float32)
```

```python
import numpy as np
from contextlib import ExitStack as _ExitStack
import concourse.bass as bass
import concourse.mybir as mybir
from concourse.bass_utils import run_bass_kernel_spmd

F32 = mybir.dt.float32
BF16 = mybir.dt.bfloat16
AF = mybir.ActivationFunctionType
ALU = mybir.AluOpType

D = 1024
SEQ = 4096
DEPTH = 2
DFF = 2816
NF = DFF // 128
TT = 1024
NTT = SEQ // TT
RMS_EPS = 1e-6

CHUNKS = ([(i * 128, 128) for i in range(8)] + [(1024, 128), (1152, 128)]
          + [(1280 + i * 128, 128) for i in range(16)] + [(3328, 8), (0, 32)])
NCH = len(CHUNKS)
PO = {"mu": 0, "w0": 8, "a0": 10, "kk": 12, "ka": 14, "rk": 16, "lnw": 18, "lnb": 20, "v0": 22, "omk": 24,
      "conv": 26, "gnw": 74, "nA": 75, "dtb": 79, "bglu": 83}
NPAR = 88
NCONST = 3520


class Buf:
    __slots__ = ("name", "ap", "w", "r", "t", "excl")

    def __init__(self, name, ap, t=None, excl=False):
        self.name = name
        self.ap = ap
        self.t = t
        self.excl = excl
        self.w = None
        self.r = []


class VBuf:
    def __init__(self, parent, t):
        self.parent = parent
        self.t = t
        self.ap = None
        self.name = parent.name + "_v"

    @property
    def w(self):
        return self.parent.w

    @w.setter
    def w(self, v):
        self.parent.w = v

    @property
    def r(self):
        return self.parent.r

    @r.setter
    def r(self, v):
        self.parent.r = v


class _Shift:
    def __init__(self, t):
        self.t = t

    def __getitem__(self, idx):
        p, c, f = idx
        f = slice((f.start or 0) + 1, (f.stop if f.stop is not None else 512) + 1, f.step)
        return self.t[p, c, f]


class _Cut(Exception):
    pass


class ExitStack(_ExitStack):
    def __exit__(self, et, ev, tb):
        if et is _Cut:
            super().__exit__(None, None, None)
            return False
        return super().__exit__(et, ev, tb)


class Sched:
    ENG = ("pe", "act", "dve", "pool", "sp")
    NPOOL = 48

    def __init__(self, nc, es, needed=None):
        self.nc = nc
        self.es = es
        self.dry = needed is None
        self.needed = set() if needed is None else needed
        self.e = {"pe": nc.tensor, "act": nc.scalar, "dve": nc.vector,
                  "pool": nc.gpsimd, "sp": nc.sync}
        self.sem = {}
        self.cnt = {}
        self.seen = {k: {} for k in self.ENG}
        for k in self.ENG:
            self.sem[k] = es.enter_context(nc.semaphore("c_" + k))
            self.cnt[k] = 0
        self.ninst = 0
        self.rank = {}
        if not self.dry:
            for k in self.ENG:
                idxs = sorted(v for (kk, v) in self.needed if kk == k)
                self.rank[k] = {v: i + 1 for i, v in enumerate(idxs)}

    def dma_sem(self, key):
        if key not in self.sem:
            self.sem[key] = self.es.enter_context(self.nc.semaphore("d_" + key))
            self.cnt[key] = 0
        return key

    def _wait(self, eng, ev):
        if ev is None:
            return
        key, val = ev
        if self.seen[eng].get(key, 0) >= val:
            return
        self.seen[eng][key] = val
        self.ninst += 1
        if key in self.ENG:
            if self.dry:
                self.needed.add((key, val))
                return
            self.e[eng].wait_ge(self.sem[key], self.rank[key][val])
        else:
            if not self.dry:
                self.e[eng].wait_ge(self.sem[key], val)

    def _deps(self, eng, r, w):
        for b in r:
            ev = b.w
            if ev is not None:
                if ev[0] == eng and eng == "pe":
                    continue
                self._wait(eng, ev)
        for b in w:
            ev = b.w
            if ev is not None and ev[0] != eng:
                self._wait(eng, ev)
            for ev in b.r:
                if ev[0] != eng:
                    self._wait(eng, ev)

    def _record(self, ev, r, w):
        for b in r:
            b.r.append(ev)
            if len(b.r) > 24:
                last = {}
                for k, v in b.r:
                    if last.get(k, 0) < v:
                        last[k] = v
                b.r = list(last.items())
        for b in w:
            b.w = ev
            b.r = []

    def op(self, eng, fn, r=(), w=()):
        if any(getattr(b, "excl", False) for b in r):
            w = list(w) + [b for b in r if getattr(b, "excl", False)]
            r = [b for b in r if not getattr(b, "excl", False)]
        self._deps(eng, r, w)
        self.cnt[eng] += 1
        self.ninst += 1
        if not self.dry:
            ins = fn(self.e[eng])
            if (eng, self.cnt[eng]) in self.needed:
                ins.then_inc(self.sem[eng], 1)
        self._record((eng, self.cnt[eng]), r, w)

    def dma(self, key, out, in_, r=(), w=(), eng="sp", **kw):
        idx = getattr(self, "dma_rr", 0)
        self.dma_rr = idx + 1
        key = f"p{idx % self.NPOOL}"
        self.dma_sem(key)
        if self.cnt[key] > 0:
            self._wait(eng, (key, self.cnt[key]))
        self._deps(eng, r, w)
        self.cnt[key] += 16
        self.ninst += 1
        if not self.dry:
            ins = self.e[eng].dma_start(out=out, in_=in_, **kw)
            ins.then_inc(self.sem[key], 16)
        self._record((key, self.cnt[key]), r, w)

    def barrier(self):
        keys = list(self.cnt.keys())
        for eng in self.ENG:
            for k in keys:
                if k != eng and self.cnt[k] > 0:
                    self._wait(eng, (k, self.cnt[k]))

    def finish(self, out_keys):
        for k in list(self.cnt.keys()):
            if k not in self.ENG and self.cnt[k] > 0:
                self._wait("sp", (k, self.cnt[k]))


def _consts():
    c = np.zeros((128, NCONST), np.float32)
    p = np.arange(128)[:, None]
    f = np.arange(128)[None, :]
    c[:, 0:128] = np.eye(128, dtype=np.float32)
    c[:, 128:256] = 1.0
    c[:, 256] = RMS_EPS
    c[:, 257] = 64e-5
    c[:, 259] = 1.0
    c[:, 384:512] = (p <= f)
    c[:, 512:640] = (p > f)
    c[:, 640:768] = (p >= f)
    c[:, 768:896] = np.where(f <= p, 0.0, -30000.0)
    c[:, 896:1024] = ((p // 64) == (f // 64))
    rm = np.ones(512, np.float32)
    rm[::128] = 0.0
    c[:, 1024:1536] = rm[None, :]
    c[:, 1536:1664] = -(p > f).astype(np.float32)
    c[:, 1664:1792] = (p > f)
    c[:, 1792:1920] = (p > f)
    c[:, 1920:2048] = (p >= f)
    c[:, 2048:2176] = (p >= f)
    for k in range(4):
        c[:, 3008 + k * 128: 3008 + (k + 1) * 128] = -(p > f).astype(np.float32)
    cc = np.arange(256)[None, :]
    for kt in range(2):
        c[:, 2304 + kt * 256: 2304 + (kt + 1) * 256] = ((cc // 16) >= (kt * 8 + p // 16))
    tau = np.concatenate([15 - np.arange(16), -np.arange(16), np.arange(16), 1 + np.arange(16)]).astype(np.float32)
    c[:, 2816:2880] = tau[None, :]
    c[:, 2880:2944] = np.arange(64, dtype=np.float32)[None, :]
    m0 = np.ones(64, np.float32)
    m0[0] = 0.0
    c[:, 2944:3008] = m0[None, :]
    return c


class K:
    def __init__(self, stop=None, ntt=NTT, mix_enable=(1, 1, 1), dbg_mix=False, needed=None):
        self.needed = needed
        self.stop = stop
        self.ntt = ntt
        self.mix_enable = mix_enable
        self.dbg_mix = dbg_mix
        self.ps_i = 0
        self.cut = None
        self.nc = bass.Bass("TRN2", target_bir_lowering=False)
        self.es = ExitStack()

    def sb(self, name, shape, dt=F32, es=None):
        self.uid = getattr(self, "uid", 0) + 1
        t = (es or self.es).enter_context(self.nc.sbuf_tensor(f"{name}_u{self.uid}", shape, dt))
        return t

    def nb(self, name, shape, dt=F32, es=None):
        t = self.sb(name, shape, dt, es)
        return Buf(name, t[:], t)

    def mm(self, ps, out_ap, lhsT, rhs, r, start=True, stop=True):
        self.S.op("pe", lambda e: e.matmul(out_ap, lhsT, rhs, start=start, stop=stop), r=r, w=[ps])

    def tr(self, ps, out_ap, in_ap, r, f32=False):
        k = in_ap.shape[0]
        idn = (self.ident if f32 else self.identb)[0:k, 0:k]
        cb = self.Bcst if f32 else self.Bcstb
        self.S.op("pe", lambda e: e.transpose(out_ap, in_ap, idn), r=list(r) + [cb], w=[ps])

    def build(self):
        nc = self.nc
        es = self.es
        S = self.S = Sched(nc, es, self.needed)
        dram = lambda n, s, dt=F32, kind="ExternalInput": nc.dram_tensor(n, s, dt, kind=kind).ap()
        self.x_d = dram("x", [SEQ, D])
        self.y_d = dram("y", [SEQ, D], kind="ExternalOutput")
        self.gain_d = dram("gain", [128, DEPTH * 6 * 8])
        self.w1_d = dram("w1", [DEPTH * 2, D, DFF])
        self.w3_d = dram("w3", [DEPTH * 2, D, DFF])
        self.w2_d = dram("w2", [DEPTH * 2, DFF, D])
        self.const_d = dram("consts", [128, NCONST])
        self.wup_s = dram("wup_s", [DEPTH * 2 * NF, 128, 2048], BF16, kind="Internal")
        self.wdn_s = dram("wdn_s", [DEPTH * 2 * 8, 128, DFF], BF16, kind="Internal")

        self.cst = self.sb("cst", [128, NCONST])
        self.cstb = self.sb("cstb", [128, 2304], BF16)
        self.gain = self.sb("gain_sb", [128, DEPTH * 6 * 8])
        self.ghalf = self.sb("ghalf_sb", [128, DEPTH * 6 * 8])
        self.xT_t = self.sb("xT", [128, 8, TT])
        self.xT = [[Buf(f"xT{d}_{h}", self.xT_t[:, d, h * 512:(h + 1) * 512]) for h in range(2)]
                   for d in range(8)]
        self.wring_t = [self.sb(f"wring{i}", [128, 3072], BF16) for i in range(4)]
        self.wring = [Buf(f"wring{i}", self.wring_t[i]) for i in range(4)]
        self.wring_i = 0
        self.psum_t = [es.enter_context(nc.psum_tensor(f"ps{i}", [128, 512], F32)) for i in range(8)]
        self.ps = [Buf(f"ps{i}", self.psum_t[i][:], excl=True) for i in range(8)]
        self.Bcst = Buf("cst", self.cst)
        self.Bcstb = Buf("cstb", self.cstb)
        self.Bgain = Buf("gain", self.gain)

        S.dma("par", self.cst[:], self.const_d[:, :], w=[self.Bcst])
        S.dma("par", self.gain[:], self.gain_d[:, :], w=[self.Bgain])
        S.op("dve", lambda e: e.tensor_copy(self.cstb[:], self.cst[:, 0:2304]), r=[self.Bcst], w=[self.Bcstb])
        S.op("act", lambda e: e.mul(self.ghalf[:], self.gain[:], 0.5), r=[self.Bgain], w=[self.Bgain])
        self.ident = self.cst[:, 0:128]
        self.identb = self.cstb[:, 0:128]
        self.onesb = self.cstb[:, 128:256]
        self.eps_ap = self.cst[:, 256:257]

        self.mixer_setup()
        self.prologue()
        for tt in range(self.ntt):
            self.load_x(tt)
            done = False
            for l in range(DEPTH):
                for step in ("ffn0", "mix", "ffn1"):
                    if step == "ffn0":
                        self.ffn(l, 0)
                    elif step == "mix":
                        self.mixer(l, tt)
                    else:
                        self.ffn(l, 1)
                    if self.stop == (step, l):
                        done = True
                        break
                if done:
                    break
            self.store_x(tt)
        S.finish(["xst"])
        return nc

    def prologue(self):
        S = self.S
        jobs = []
        for lj in range(DEPTH * 2):
            for f in range(NF):
                for k, wd in enumerate((self.w1_d, self.w3_d)):
                    src = wd[lj][:, f * 128:(f + 1) * 128].rearrange("(dt p) c -> p dt c", p=128)
                    dst = self.wup_s[lj * NF + f][:, k * 1024:(k + 1) * 1024]
                    jobs.append((src, dst, 8, 128))
            for m in range(8):
                for (f0, nf) in ((0, 8), (8, 8), (16, 6)):
                    src = self.w2_d[lj][f0 * 128:(f0 + nf) * 128, m * 128:(m + 1) * 128].rearrange(
                        "(f p) c -> p f c", p=128)
                    dst = self.wdn_s[lj * 8 + m][:, f0 * 128:(f0 + nf) * 128]
                    jobs.append((src, dst, nf, 128))
        jobs += self.extra_convert_jobs()
        with ExitStack() as es:
            stg = [self.sb(f"pstg{i}", [128, 1024], F32, es) for i in range(4)]
            stgB = [Buf(f"pstg{i}", stg[i]) for i in range(4)]
            ob = [self.sb(f"pob{i}", [128, 1024], BF16, es) for i in range(4)]
            obB = [Buf(f"pob{i}", ob[i]) for i in range(4)]
            engs = ["dve", "act", "pool"]
            for i, (src, dst, a, b) in enumerate(jobs):
                s = i % 4
                n = a * b
                sv = stg[s][:, 0:n].rearrange("p (a b) -> p a b", b=b)
                S.dma(f"pst{s}", sv, src, w=[stgB[s]])
                eng = engs[i % 3]
                if eng == "act":
                    S.op("act", lambda e, o=ob[s], i_=stg[s], n=n: e.copy(o[:, 0:n], i_[:, 0:n]), r=[stgB[s]], w=[obB[s]])
                else:
                    S.op(eng, lambda e, o=ob[s], i_=stg[s], n=n: e.tensor_copy(o[:, 0:n], i_[:, 0:n]),
                         r=[stgB[s]], w=[obB[s]])
                S.dma(f"pob{s}", dst, ob[s][:, 0:n], r=[obB[s]])
            S.barrier()


    def load_x(self, tt):
        S = self.S
        with ExitStack() as es:
            xtok = self.sb("xtok", [128, 8, D], F32, es)
            Bx = [Buf(f"xtok{i}", xtok[:, i, :]) for i in range(8)]
            for i in range(8):
                S.dma("xld", xtok[:, i, :], self.x_d[tt * TT + i * 128: tt * TT + (i + 1) * 128, :], w=[Bx[i]])
            k = 0
            for d in range(8):
                for h in range(2):
                    ps = self.ps[k % 4]
                    for q in range(4):
                        i = h * 4 + q
                        S.op("pe", lambda e, ps=ps, q=q, i=i, d=d: e.transpose(
                            ps.ap[:, q * 128:(q + 1) * 128], xtok[:, i, d * 128:(d + 1) * 128], self.ident),
                            r=[Bx[i], self.Bcst], w=[ps])
                    dstB = self.xT[d][h]
                    if k % 2 == 0:
                        S.op("act", lambda e, a=dstB.ap, b=ps.ap: e.copy(a, b), r=[ps], w=[dstB])
                    else:
                        S.op("dve", lambda e, a=dstB.ap, b=ps.ap: e.tensor_copy(a, b), r=[ps], w=[dstB])
                    k += 1
            S.barrier()

    def store_x(self, tt):
        S = self.S
        with ExitStack() as es:
            xtok = self.sb("xtok_o", [128, 8, D], F32, es)
            Bx = [Buf(f"xtoko{i}", xtok[:, i, :]) for i in range(8)]
            k = 0
            for i in range(8):
                h, q = divmod(i, 4)
                for dh in range(2):
                    ps = self.ps[k % 4]
                    for dq in range(4):
                        d = dh * 4 + dq
                        S.op("pe", lambda e, ps=ps, dq=dq, d=d, h=h, q=q: e.transpose(
                            ps.ap[:, dq * 128:(dq + 1) * 128],
                            self.xT_t[:, d, h * 512 + q * 128: h * 512 + (q + 1) * 128], self.ident),
                            r=[self.xT[d][h], self.Bcst], w=[ps])
                    dst = xtok[:, i, dh * 512:(dh + 1) * 512]
                    if k % 2 == 0:
                        S.op("act", lambda e, a=dst, b=ps.ap: e.copy(a, b), r=[ps], w=[Bx[i]])
                    else:
                        S.op("dve", lambda e, a=dst, b=ps.ap: e.tensor_copy(a, b), r=[ps], w=[Bx[i]])
                    k += 1
                S.dma("xst", self.y_d[tt * TT + i * 128: tt * TT + (i + 1) * 128, :], xtok[:, i, :], r=[Bx[i]])
            S.barrier()

    def ring_load(self, src_ap, n):
        S = self.S
        slot = self.wring_i % 4
        self.wring_i += 1
        S.dma(f"wr{slot}", self.wring_t[slot][:, 0:n], src_ap, w=[self.wring[slot]])
        return slot

    def rstd_from_ps(self, ps, Brs):
        S = self.S
        S.op("act", lambda e, a=Brs.ap, ps=ps: e.activation(out=a, in_=ps.ap, func=AF.Sqrt,
                                                             scale=1.0 / D, bias=self.eps_ap),
             r=[ps, self.Bcst], w=[Brs])
        S.op("dve", lambda e, a=Brs.ap: e.reciprocal(a, a), r=[Brs], w=[Brs])

    def ffn(self, l, j):
        S = self.S
        lj = l * 2 + j
        gi_pre = (l * 6 + (0 if j == 0 else 4)) * 8
        gi_post = (l * 6 + (1 if j == 0 else 5)) * 8
        with ExitStack() as es:
            xn = self.sb("xn", [128, 8, TT], BF16, es)
            Bxn = [[Buf(f"xn{d}_{h}", xn[:, d, h * 512:(h + 1) * 512]) for h in range(2)] for d in range(8)]
            hid = self.sb("hid", [128, NF, TT], BF16, es)
            Bhid = [[Buf(f"hid{f}_{h}", hid[:, f, h * 512:(h + 1) * 512]) for h in range(2)] for f in range(NF)]
            ff = self.sb("ff", [128, 8, TT], F32, es)
            Bff = [[Buf(f"ff{d}_{h}", ff[:, d, h * 512:(h + 1) * 512]) for h in range(2)] for d in range(8)]
            sq = self.sb("sq", [128, 2, 512], BF16, es)
            Bsq = [Buf(f"sq{i}", sq[:, i, :]) for i in range(2)]
            rstd = self.sb("rstd", [128, 2, 512], F32, es)
            Brstd = [Buf(f"rstd{h}", rstd[:, h, :]) for h in range(2)]
            sil = self.sb("sil", [128, 2, 512], F32, es)
            Bsil = [Buf(f"sil{i}", sil[:, i, :]) for i in range(2)]
            tmp = self.sb("tmpf", [128, 2, 512], F32, es)
            Btmp = [Buf(f"tmpf{i}", tmp[:, i, :]) for i in range(2)]

            PRE = 3
            slots = {}
            for f in range(PRE):
                slots[f] = self.ring_load(self.wup_s[lj * NF + f], 2048)

            for h in range(2):
                ps = self.ps[6 + h]
                for d in range(8):
                    sb_ = Bsq[d % 2]
                    S.op("act", lambda e, a=sb_.ap, b=self.xT[d][h].ap: e.activation(out=a, in_=b, func=AF.Square),
                         r=[self.xT[d][h]], w=[sb_])
                    S.op("pe", lambda e, ps=ps, a=sb_.ap, d=d: e.matmul(ps.ap, self.onesb, a, start=(d == 0), stop=(d == 7)),
                         r=[sb_, self.Bcstb], w=[ps])
                self.rstd_from_ps(ps, Brstd[h])
                for d in range(8):
                    S.op("dve", lambda e, d=d, h=h: e.scalar_tensor_tensor(
                        out=Bxn[d][h].ap, in0=self.xT[d][h].ap, scalar=self.gain[:, gi_pre + d: gi_pre + d + 1],
                        in1=Brstd[h].ap, op0=ALU.mult, op1=ALU.mult),
                        r=[self.xT[d][h], Brstd[h], self.Bgain], w=[Bxn[d][h]])

            k = 0
            dn_slots = {}
            for f in range(NF):
                slot = slots[f]
                wt = self.wring_t[slot]
                wB = self.wring[slot]
                for h in range(2):
                    p1 = self.ps[(k % 2) * 2]
                    p3 = self.ps[(k % 2) * 2 + 1]
                    for (pp, off) in ((p1, 0), (p3, 1024)):
                        for d in range(8):
                            S.op("pe", lambda e, pp=pp, off=off, d=d, h=h, wt=wt: e.matmul(
                                pp.ap, wt[:, off + d * 128: off + (d + 1) * 128], Bxn[d][h].ap,
                                start=(d == 0), stop=(d == 7)),
                                r=[wB, Bxn[d][h]], w=[pp])
                    sB = Bsil[k % 2]
                    S.op("act", lambda e, a=sB.ap, b=p1.ap: e.activation(out=a, in_=b, func=AF.Silu), r=[p1], w=[sB])
                    S.op("dve", lambda e, a=Bhid[f][h].ap, b=sB.ap, c=p3.ap: e.tensor_tensor(a, b, c, ALU.mult),
                         r=[sB, p3], w=[Bhid[f][h]])
                    k += 1
                nf_ = f + PRE
                if nf_ < NF:
                    slots[nf_] = self.ring_load(self.wup_s[lj * NF + nf_], 2048)
                elif nf_ - NF < 8:
                    m = nf_ - NF
                    dn_slots[m] = self.ring_load(self.wdn_s[lj * 8 + m], DFF)

            k = 0
            for m in range(8):
                slot = dn_slots[m]
                wt = self.wring_t[slot]
                wB = self.wring[slot]
                for h in range(2):
                    pp = self.ps[4 + (k % 2)]
                    for f in range(NF):
                        S.op("pe", lambda e, pp=pp, f=f, h=h, wt=wt: e.matmul(
                            pp.ap, wt[:, f * 128:(f + 1) * 128], Bhid[f][h].ap, start=(f == 0), stop=(f == NF - 1)),
                            r=[wB, Bhid[f][h]], w=[pp])
                    S.op("act", lambda e, a=Bff[m][h].ap, b=pp.ap: e.copy(a, b), r=[pp], w=[Bff[m][h]])
                    sb_ = Bsq[k % 2]
                    S.op("act", lambda e, a=sb_.ap, b=pp.ap: e.activation(out=a, in_=b, func=AF.Square), r=[pp], w=[sb_])
                    S.op("pe", lambda e, h=h, a=sb_.ap, m=m: e.matmul(self.ps[6 + h].ap, self.onesb, a,
                                                                      start=(m == 0), stop=(m == 7)),
                         r=[sb_, self.Bcstb], w=[self.ps[6 + h]])
                    k += 1
                if m + PRE < 8:
                    dn_slots[m + PRE] = self.ring_load(self.wdn_s[lj * 8 + m + PRE], DFF)
            for h in range(2):
                self.rstd_from_ps(self.ps[6 + h], Brstd[h])
                for d in range(8):
                    tB = Btmp[d % 2]
                    S.op("dve", lambda e, d=d, h=h, tB=tB: e.scalar_tensor_tensor(
                        out=tB.ap, in0=Bff[d][h].ap, scalar=self.ghalf[:, gi_post + d: gi_post + d + 1],
                        in1=Brstd[h].ap, op0=ALU.mult, op1=ALU.mult),
                        r=[Bff[d][h], Brstd[h], self.Bgain], w=[tB])
                    S.op("pool", lambda e, d=d, h=h, tB=tB: e.tensor_tensor(
                        self.xT[d][h].ap, self.xT[d][h].ap, tB.ap, ALU.add),
                        r=[tB, self.xT[d][h]], w=[self.xT[d][h]])
            S.barrier()


    def ck(self, name):
        if self.cut == name:
            self.S.barrier()
            raise _Cut()

    def pb(self):
        b = self.ps[self.ps_i % 8]
        self.ps_i += 1
        return b

    def inproj_fm(self, l, chunk, ncols, hT, Bh, tok0, ntok, ps, out_ap=None):
        S = self.S
        slot = self.ring_load(self.win_s[l * NCH + chunk][:, 0:8 * ncols], 8 * ncols)
        wt = self.wring_t[slot]
        wB = self.wring[slot]
        oap = ps.ap[0:ncols, 0:ntok] if out_ap is None else out_ap
        for dt in range(8):
            self.mm(ps, oap, wt[:, dt * ncols:(dt + 1) * ncols], hT[:, dt, tok0:tok0 + ntok],
                    r=[wB, Bh], start=(dt == 0), stop=(dt == 7))

    def rwkv(self, l, tt, hT, Bh, mixT, Bmix):
        S = self.S
        P = self.par[l]
        BP = self.Bpar[l]
        pc = lambda name, i=0, n=1: P[:, PO[name] + i: PO[name] + i + n]
        cst = self.cst
        LEm = cst[:, 384:512]
        GTm = cst[:, 512:640]
        GEm = cst[:, 640:768]
        BLK = self.cstb[:, 896:1024]
        RESET = cst[:, 1024:1536]
        AM = cst[:, 1664:2176].rearrange("p (a b) -> p a b", b=128)
        with ExitStack() as es:
            nb = lambda n, s, dt=F32: self.nb("rw_" + n, s, dt, es)
            Pb = nb("P", [128, 8, 513])
            Z = VBuf(Pb, _Shift(Pb.t))
            t6 = nb("t6", [128, 512], BF16)
            sgd = nb("sgd", [128, 512], BF16)
            vres = nb("vres", [32, 512], BF16)
            a_ = nb("a", [128, 1, 512])
            lw = nb("lw", [128, 1, 512])
            kk = nb("kk", [128, 1, 512])
            kmod = nb("kmod", [128, 1, 512])
            tmp = [nb(f"tmp{i}", [128, 512]) for i in range(4)]
            tmpb = [nb(f"tmpb{i}", [128, 512], BF16) for i in range(2)]
            ex = [nb(f"ex{i}", [128, 512]) for i in range(4)]
            ops_ = {n: nb(n, [128, 2, 512], BF16) for n in ("alT", "btT", "ktT", "rbT", "KhT", "BhT", "vbT")}
            g_ = nb("g", [128, 2, 512], BF16)
            rks = nb("rks", [128, 2, 512], BF16)
            ybuf = nb("ybuf", [128, 2, 512])
            pads = {n: [[nb(f"{n}{i}{hh}", [128, 128], BF16) for hh in range(2)] for i in range(2)]
                    for n in ("Ap", "Kp", "Bp", "Vp")}
            for n in pads:
                for i in range(2):
                    for hh in range(2):
                        S.op("pool", lambda e, b=pads[n][i][hh]: e.memset(b.ap, 0.0), w=[pads[n][i][hh]])
            Amat = [nb(f"Amat{i}", [128, 4, 128], BF16) for i in range(2)]
            NnB = nb("Nn", [128, 2, 128], BF16)
            MmB = nb("Mm", [128, 2, 128], BF16)
            QB = nb("Q", [128, 2, 128], BF16)
            N2B = nb("N2", [128, 2, 128], BF16)
            M2B = nb("M2", [128, 2, 128], BF16)
            ArT = nb("ArT", [128, 2, 2, 128], BF16)
            WtT = nb("WtT", [128, 2, 128], BF16)
            PT = nb("PT", [128, 2, 128], BF16)
            Ut = nb("Ut", [128, 2, 128])
            Up = nb("Up", [128, 2, 128], BF16)
            Sb = self.rw_Sb[l]
            Sf = self.rw_Sf[l]
            halo = self.rw_halo[l]
            for st in range(2):
                t0 = st * 512
                S.op("act", lambda e: e.copy(Pb.t[:, :, 0], halo.ap), r=[halo], w=[Pb])
                for c in range(8):
                    ps = self.pb()
                    self.inproj_fm(l, c, 128, hT, Bh, t0, 512, ps)
                    S.op("act", lambda e, c=c, ps=ps: e.copy(Pb.t[:, c, 1:513], ps.ap), r=[ps], w=[Pb])
                S.op("act", lambda e: e.copy(halo.ap, Pb.t[:, :, 512]), r=[Pb], w=[halo])
                for c in range(8):
                    tq_ = tmp[c % 2]
                    S.op("dve", lambda e, c=c: e.tensor_tensor(tq_.ap, Pb.t[:, c, 0:512], Pb.t[:, c, 1:513], ALU.subtract),
                         r=[Pb], w=[tq_])
                    S.op("dve", lambda e, c=c: e.scalar_tensor_tensor(out=Pb.t[:, c, 1:513], in0=tq_.ap, scalar=pc("mu", c),
                                                                    in1=Pb.t[:, c, 1:513], op0=ALU.mult, op1=ALU.add),
                         r=[Pb, tq_, BP], w=[Pb])
                self.ck('rwA')
                S.op("act", lambda e: e.activation(out=t6.t[0:64, :], in_=Z.t[0:64, 6, :], func=AF.Tanh), r=[Z], w=[t6])
                S.op("act", lambda e: e.copy(t6.t[64:128, :], Z.t[64:128, 6, :]), r=[Z], w=[t6])
                S.op("act", lambda e: e.activation(out=sgd.ap, in_=Z.t[:, 7, :], func=AF.Sigmoid), r=[Z], w=[sgd])
                LW = self.lorab[l]
                BLW = self.Blorab[l]
                for ct in range(2):
                    ps = self.pb()
                    self.mm(ps, ps.ap, LW[:, 256 + ct * 128: 256 + (ct + 1) * 128], sgd.ap, r=[BLW, sgd])
                    S.op("act", lambda e, ps=ps, ct=ct: e.copy(g_.t[:, ct, :], ps.ap), r=[ps], w=[g_])
                vT = lambda ct: Z.t[:, 4 + ct, :]
                if l == 0:
                    for ct in range(2):
                        S.op("pool", lambda e, ct=ct: e.tensor_copy(self.vfirst.t[:, ct, t0:t0 + 512], vT(ct)),
                             r=[Z], w=[self.vfirst])
                else:
                    ps = self.pb()
                    self.inproj_fm(l, 27, 32, hT, Bh, t0, 512, ps)
                    S.op("act", lambda e, ps=ps: e.copy(vres.ap, ps.ap[0:32, :]), r=[ps], w=[vres])
                    for ct in range(2):
                        ps = self.pb()
                        self.mm(ps, ps.ap, LW[0:32, 512 + ct * 128: 512 + (ct + 1) * 128], vres.ap, r=[BLW, vres])
                        S.op("act", lambda e, ps=ps, ct=ct: e.activation(out=tmp[0].ap, in_=ps.ap, func=AF.Sigmoid,
                                                                          bias=pc("v0", ct)), r=[ps, BP], w=[tmp[0]])
                        S.op("dve", lambda e, ct=ct: e.tensor_tensor(tmp[1].ap, self.vfirst.t[:, ct, t0:t0 + 512], vT(ct), ALU.subtract),
                             r=[self.vfirst, Z], w=[tmp[1]])
                        S.op("dve", lambda e: e.tensor_tensor(tmp[1].ap, tmp[1].ap, tmp[0].ap, ALU.mult),
                             r=[tmp[0], tmp[1]], w=[tmp[1]])
                        S.op("dve", lambda e, ct=ct: e.tensor_tensor(vT(ct), vT(ct), tmp[1].ap, ALU.add), r=[tmp[1], Z], w=[Z])
                self.ck('rwC')
                for ct in range(2):
                    rT = Z.t[:, ct, :]
                    kT = Z.t[:, 2 + ct, :]
                    ps = self.pb()
                    self.mm(ps, ps.ap, LW[0:64, ct * 128:(ct + 1) * 128], t6.t[0:64, :], r=[BLW, t6])
                    S.op("act", lambda e, ps=ps, ct=ct: e.activation(out=lw.t[:, 0, :], in_=ps.ap, func=AF.Sigmoid,
                                                                      bias=pc("w0", ct)), r=[ps, BP], w=[lw])
                    ps = self.pb()
                    self.mm(ps, ps.ap, LW[64:128, ct * 128:(ct + 1) * 128], t6.t[64:128, :], r=[BLW, t6])
                    S.op("act", lambda e, ps=ps, ct=ct: e.activation(out=a_.t[:, 0, :], in_=ps.ap, func=AF.Sigmoid,
                                                                      bias=pc("a0", ct)), r=[ps, BP], w=[a_])
                    S.op("dve", lambda e: e.tensor_scalar(kk.t[:, 0, :], kT, pc("kk", ct), None, ALU.mult), r=[Z, BP], w=[kk])
                    S.op("act", lambda e: e.activation(out=tmpb[0].ap, in_=kk.t[:, 0, :], func=AF.Square), r=[kk], w=[tmpb[0]])
                    ps = self.pb()
                    self.mm(ps, ps.ap, BLK, tmpb[0].ap, r=[tmpb[0], self.Bcstb])
                    S.op("act", lambda e, ps=ps: e.activation(out=tmp[0].ap, in_=ps.ap, func=AF.Sqrt), r=[ps], w=[tmp[0]])
                    S.op("dve", lambda e: e.tensor_scalar(tmp[0].ap, tmp[0].ap, 1e-12, None, ALU.max), r=[tmp[0]], w=[tmp[0]])
                    S.op("dve", lambda e: e.reciprocal(tmp[0].ap, tmp[0].ap), r=[tmp[0]], w=[tmp[0]])
                    S.op("dve", lambda e: e.tensor_tensor(kk.t[:, 0, :], kk.t[:, 0, :], tmp[0].ap, ALU.mult), r=[kk, tmp[0]], w=[kk])
                    S.op("dve", lambda e: e.tensor_scalar(tmp[1].ap, a_.t[:, 0, :], pc("ka", ct), pc("omk", ct), ALU.mult, ALU.add),
                         r=[a_, BP], w=[tmp[1]])
                    S.op("dve", lambda e: e.tensor_tensor(kmod.t[:, 0, :], tmp[1].ap, kT, ALU.mult), r=[tmp[1], Z], w=[kmod])
                    S.op("pool", lambda e: e.tensor_scalar(lw.t[:, 0, :], lw.t[:, 0, :], -0.6065306597126334, None, ALU.mult),
                         r=[lw], w=[lw])
                    S.op("dve", lambda e: e.tensor_tensor_scan(tmp[2].ap, RESET, lw.t[:, 0, :], 0.0, ALU.mult, ALU.add),
                         r=[lw, self.Bcst], w=[tmp[2]])
                    Lam = tmp[2]
                    S.op("act", lambda e: e.activation(out=ex[0].ap, in_=Lam.ap, func=AF.Exp), r=[Lam], w=[ex[0]])
                    S.op("dve", lambda e: e.tensor_tensor(tmp[3].ap, Lam.ap, lw.t[:, 0, :], ALU.subtract), r=[Lam, lw], w=[tmp[3]])
                    S.op("act", lambda e: e.activation(out=ex[1].ap, in_=tmp[3].ap, func=AF.Exp), r=[tmp[3]], w=[ex[1]])
                    S.op("act", lambda e: e.activation(out=ex[2].ap, in_=Lam.ap, func=AF.Exp, scale=-1.0), r=[Lam], w=[ex[2]])
                    for cc in range(4):
                        S.op("dve", lambda e, cc=cc: e.tensor_scalar(tmp[3].t[:, cc * 128:(cc + 1) * 128], Lam.t[:, cc * 128:(cc + 1) * 128],
                                                                     -1.0, Lam.t[:, cc * 128 + 127: cc * 128 + 128], ALU.mult, ALU.add),
                             r=[Lam, tmp[3]], w=[tmp[3]])
                    S.op("act", lambda e: e.activation(out=ex[3].ap, in_=tmp[3].ap, func=AF.Exp), r=[tmp[3]], w=[ex[3]])
                    o = ops_
                    S.op("dve", lambda e: e.scalar_tensor_tensor(out=o["alT"].t[:, ct, :], in0=kk.t[:, 0, :], scalar=-1.0, in1=ex[1].ap,
                                                                 op0=ALU.mult, op1=ALU.mult), r=[kk, ex[1]], w=[o["alT"]])
                    S.op("pool", lambda e: e.tensor_tensor(tmp[1].ap, kk.t[:, 0, :], a_.t[:, 0, :], ALU.mult), r=[kk, a_, tmp[1]], w=[tmp[1]])
                    S.op("dve", lambda e: e.tensor_tensor(o["btT"].t[:, ct, :], tmp[1].ap, ex[2].ap, ALU.mult), r=[tmp[1], ex[2]], w=[o["btT"]])
                    S.op("pool", lambda e: e.tensor_tensor(o["BhT"].t[:, ct, :], tmp[1].ap, ex[3].ap, ALU.mult), r=[tmp[1], ex[3]], w=[o["BhT"]])
                    S.op("dve", lambda e: e.tensor_tensor(o["ktT"].t[:, ct, :], kmod.t[:, 0, :], ex[2].ap, ALU.mult), r=[kmod, ex[2]], w=[o["ktT"]])
                    S.op("pool", lambda e: e.tensor_tensor(o["KhT"].t[:, ct, :], kmod.t[:, 0, :], ex[3].ap, ALU.mult), r=[kmod, ex[3]], w=[o["KhT"]])
                    S.op("dve", lambda e: e.tensor_tensor(o["rbT"].t[:, ct, :], rT, ex[0].ap, ALU.mult), r=[Z, ex[0]], w=[o["rbT"]])
                    S.op("act", lambda e: e.copy(o["vbT"].t[:, ct, :], vT(ct)), r=[Z], w=[o["vbT"]])
                    S.op("dve", lambda e: e.scalar_tensor_tensor(out=tmpb[1].ap, in0=rT, scalar=pc("rk", ct), in1=kmod.t[:, 0, :],
                                                                 op0=ALU.mult, op1=ALU.mult), r=[Z, kmod, BP], w=[tmpb[1]])
                    ps = self.pb()
                    self.mm(ps, ps.ap, BLK, tmpb[1].ap, r=[tmpb[1], self.Bcstb])
                    S.op("act", lambda e, ps=ps: e.copy(rks.t[:, ct, :], ps.ap), r=[ps], w=[rks])
                    S.op("pool", lambda e: e.tensor_copy(self.rw_gc.t[:, ct, :], ex[0].t[:, 127:512:128]), r=[ex[0]], w=[self.rw_gc])

                self.ck('rwD')
                for cc in range(4):
                    csl = slice(cc * 128, (cc + 1) * 128)
                    for ct in range(2):
                        pi = (cc * 2 + ct) % 2
                        o = ops_
                        pst = self.pb()
                        psb = pst.ap.bitcast(BF16)
                        for j, n in enumerate(("alT", "KhT", "BhT", "vbT")):
                            self.tr(pst, psb[:, j * 128:(j + 1) * 128], o[n].t[:, ct, csl], r=[o[n]])
                        self.ck('rwE1a')
                        for j, n in enumerate(("Ap", "Kp", "Bp", "Vp")):
                            if j == 1:
                                self.ck('rwE1b')
                            for hh in range(2):
                                dst = pads[n][pi][hh]
                                eng = "act" if (j + hh) % 2 == 0 else "dve"
                                import os as _os
                                if _os.environ.get("PADENG"):
                                    eng = _os.environ["PADENG"]
                                src_ap = psb[:, j * 128 + hh * 64: j * 128 + hh * 64 + 64]
                                if eng == "act":
                                    S.op("act", lambda e, d=dst, s_=src_ap, hh=hh: e.copy(d.t[:, hh * 64: hh * 64 + 64], s_), r=[pst], w=[dst])
                                else:
                                    S.op("dve", lambda e, d=dst, s_=src_ap, hh=hh: e.tensor_copy(d.t[:, hh * 64: hh * 64 + 64], s_), r=[pst], w=[dst])
                        self.ck('rwE1')
                        for hh in range(2):
                            hsl = slice(hh * 64, hh * 64 + 64)
                            pa = self.pb()
                            for j, (ln, rn) in enumerate((("alT", "btT"), ("alT", "ktT"), ("rbT", "ktT"), ("rbT", "btT"))):
                                self.mm(pa, pa.ap[:, j * 128:(j + 1) * 128], o[ln].t[hsl, ct, csl], o[rn].t[hsl, ct, csl], r=[o[ln], o[rn]])
                            Am = Amat[hh]
                            S.op("dve", lambda e, pa=pa, Am=Am: e.tensor_tensor(Am.ap, pa.ap.rearrange("p (a b) -> p a b", b=128), AM, ALU.mult),
                                 r=[pa, self.Bcst], w=[Am])
                            S.op("pool", lambda e, Am=Am, hh=hh: e.tensor_copy(NnB.t[:, hh, :], Am.t[:, 0, :]), r=[Am], w=[NnB])
                        self.ck('rwE2')
                        pst = self.pb()
                        psb = pst.ap.bitcast(BF16)
                        for hh in range(2):
                            self.tr(pst, psb[:, hh * 128:(hh + 1) * 128], Amat[hh].t[:, 0, :], r=[Amat[hh]])
                            self.tr(pst, psb[:, 256 + hh * 256: 256 + hh * 256 + 128], Amat[hh].t[:, 2, :], r=[Amat[hh]])
                            self.tr(pst, psb[:, 256 + hh * 256 + 128: 256 + hh * 256 + 256], Amat[hh].t[:, 3, :], r=[Amat[hh]])
                        S.op("act", lambda e, psb=psb: e.copy(MmB.ap.rearrange("p a b -> p (a b)"), psb[:, 0:256]), r=[pst], w=[MmB])
                        S.op("dve", lambda e, psb=psb: e.tensor_copy(ArT.ap.rearrange("p a b c -> p (a b c)"), psb[:, 256:768]), r=[pst], w=[ArT])
                        self.ck('rwE3')
                        self.neumann2("rw", NnB, MmB, N2B, M2B, QB, 2)
                        self.ck('rwE4')
                        pw = self.pb()
                        for hh in range(2):
                            self.mm(pw, pw.ap[:, hh * 128:(hh + 1) * 128], pads["Ap"][pi][hh].ap, QB.t[:, hh, :], r=[pads["Ap"][pi][hh], QB])
                            self.mm(pw, pw.ap[:, 256 + hh * 128: 256 + (hh + 1) * 128], Amat[hh].t[:, 1, :], QB.t[:, hh, :], r=[Amat[hh], QB])
                        S.op("act", lambda e, pw=pw: e.copy(WtT.ap.rearrange("p a b -> p (a b)"), pw.ap[:, 0:256]), r=[pw], w=[WtT])
                        S.op("dve", lambda e, pw=pw: e.tensor_copy(PT.ap.rearrange("p a b -> p (a b)"), pw.ap[:, 256:512]), r=[pw], w=[PT])
                        pu = self.pb()
                        for hh in range(2):
                            self.mm(pu, pu.ap[:, hh * 128:(hh + 1) * 128], PT.t[:, hh, :], pads["Vp"][pi][hh].ap, r=[PT, pads["Vp"][pi][hh]])
                        S.op("act", lambda e, pu=pu: e.copy(Ut.ap.rearrange("p a b -> p (a b)"), pu.ap[:, 0:256]), r=[pu], w=[Ut])
                        pws = self.pb()
                        for hh in range(2):
                            self.mm(pws, pws.ap[:, hh * 128:(hh + 1) * 128], WtT.t[:, hh, :], Sb.t[:, ct, :], r=[WtT, Sb])
                        S.op("dve", lambda e, pws=pws: e.tensor_tensor(Up.ap.rearrange("p a b -> p (a b)"), Ut.ap.rearrange("p a b -> p (a b)"),
                                                                      pws.ap[:, 0:256], ALU.add), r=[pws, Ut], w=[Up])
                        py = self.pb()
                        self.mm(py, py.ap[:, 0:128], Sb.t[:, ct, :], o["rbT"].t[:, ct, csl], r=[Sb, o["rbT"]], start=True, stop=False)
                        for hh in range(2):
                            self.mm(py, py.ap[:, 0:128], pads["Vp"][pi][hh].ap, ArT.t[:, hh, 0, :], r=[pads["Vp"][pi][hh], ArT], start=False, stop=False)
                            self.mm(py, py.ap[:, 0:128], Up.t[:, hh, :], ArT.t[:, hh, 1, :], r=[Up, ArT], start=False, stop=(hh == 1))
                        S.op("act", lambda e, py=py, ct=ct, csl=csl: e.copy(ybuf.t[:, ct, csl], py.ap[:, 0:128]), r=[py], w=[ybuf])
                        pss = self.pb()
                        for hh in range(2):
                            self.mm(pss, pss.ap[:, 0:128], pads["Kp"][pi][hh].ap, pads["Vp"][pi][hh].ap,
                                    r=[pads["Kp"][pi][hh], pads["Vp"][pi][hh]], start=(hh == 0), stop=False)
                            self.mm(pss, pss.ap[:, 0:128], pads["Bp"][pi][hh].ap, Up.t[:, hh, :],
                                    r=[pads["Bp"][pi][hh], Up], start=False, stop=(hh == 1))
                        S.op("dve", lambda e, pss=pss, ct=ct, cc=cc: e.scalar_tensor_tensor(
                            out=Sf.t[:, ct, :], in0=Sf.t[:, ct, :], scalar=self.rw_gc.t[:, ct, cc:cc + 1], in1=pss.ap[:, 0:128],
                            op0=ALU.mult, op1=ALU.add), r=[pss, Sf, self.rw_gc], w=[Sf])
                        S.op("act", lambda e, ct=ct: e.copy(Sb.t[:, ct, :], Sf.t[:, ct, :]), r=[Sf], w=[Sb])
                self.ck('rwF')
                for ct in range(2):
                    S.op("act", lambda e: e.copy(tmpb[0].ap, ybuf.t[:, ct, :]), r=[ybuf], w=[tmpb[0]])
                    ps = self.pb()
                    self.mm(ps, ps.ap, BLK, tmpb[0].ap, r=[tmpb[0], self.Bcstb])
                    S.op("dve", lambda e, ps=ps: e.scalar_tensor_tensor(out=tmp[0].ap, in0=ps.ap, scalar=-1.0 / 64, in1=ybuf.t[:, ct, :],
                                                                       op0=ALU.mult, op1=ALU.add), r=[ps, ybuf], w=[tmp[0]])
                    S.op("act", lambda e: e.activation(out=tmpb[1].ap, in_=tmp[0].ap, func=AF.Square), r=[tmp[0]], w=[tmpb[1]])
                    ps = self.pb()
                    self.mm(ps, ps.ap, BLK, tmpb[1].ap, r=[tmpb[1], self.Bcstb])
                    S.op("act", lambda e, ps=ps: e.activation(out=tmp[1].ap, in_=ps.ap, func=AF.Sqrt, scale=1.0 / 64, bias=cst[:, 257:258]),
                         r=[ps, self.Bcst], w=[tmp[1]])
                    S.op("dve", lambda e: e.reciprocal(tmp[1].ap, tmp[1].ap), r=[tmp[1]], w=[tmp[1]])
                    S.op("dve", lambda e: e.tensor_tensor(tmp[0].ap, tmp[0].ap, tmp[1].ap, ALU.mult), r=[tmp[0], tmp[1]], w=[tmp[0]])
                    S.op("act", lambda e: e.activation(out=tmp[0].ap, in_=tmp[0].ap, func=AF.Identity, scale=pc("lnw", ct), bias=pc("lnb", ct)),
                         r=[tmp[0], BP], w=[tmp[0]])
                    S.op("dve", lambda e: e.tensor_tensor(tmp[1].ap, rks.t[:, ct, :], vT(ct), ALU.mult), r=[rks, Z, tmp[1]], w=[tmp[1]])
                    S.op("dve", lambda e: e.tensor_tensor(tmp[0].ap, tmp[0].ap, tmp[1].ap, ALU.add), r=[tmp[0], tmp[1]], w=[tmp[0]])
                    S.op("dve", lambda e: e.tensor_tensor(mixT[:, ct, t0:t0 + 512], tmp[0].ap, g_.t[:, ct, :], ALU.mult),
                         r=[tmp[0], g_], w=[Bmix])
            S.barrier()

    def gdn(self, l, tt, hT, Bh, mixT, Bmix):
        S = self.S
        P = self.par[l]
        BP = self.Bpar[l]
        pc = lambda name, i=0, n=1: P[:, PO[name] + i: PO[name] + i + n]
        cst = self.cst
        LEm = cst[:, 384:512]
        GTm = cst[:, 512:640]
        NEG = cst[:, 768:896]
        ONESF = cst[:, 128:256]
        NGT4 = cst[:, 3008:3520].rearrange("p (a b) -> p a b", b=128)
        with ExitStack() as es:
            nb = lambda n, s, dt=F32: self.nb("gd_" + n, s, dt, es)
            diag = nb("diag", [128, 12, 4, 128], BF16)
            zb = nb("zb", [128, 12, 515], BF16)
            qn = nb("qn", [128, 4, 512], BF16)
            kn = nb("kn", [128, 4, 512], BF16)
            vb = nb("vb", [128, 4, 512], BF16)
            sg = nb("sg", [128, 4, 512], BF16)
            tf = [nb(f"tf{i}", [128, 512]) for i in range(3)]
            tb = [nb(f"tb{i}", [128, 512], BF16) for i in range(2)]
            gt = nb("gt", [128, 8])
            ge = nb("ge", [128, 8])
            beg = nb("beg", [128, 4])
            Lh = nb("Lh", [128, 4, 128])
            dec = nb("dec", [128, 4, 128])
            dsn = nb("dsn", [128, 4, 128])
            egB = nb("egB", [128, 4, 128])
            NnB = nb("Nn", [128, 4, 128], F32)
            MmB = nb("Mm", [128, 4, 128], F32)
            N2 = nb("N2", [128, 4, 128], F32)
            M2 = nb("M2", [128, 4, 128], F32)
            QB = nb("Q", [128, 4, 128], F32)
            Qbf = nb("Qbf", [128, 4, 128], BF16)
            att = nb("att", [128, 4, 128], BF16)
            attT = nb("attT", [128, 4, 128], BF16)
            bv = nb("bv", [128, 4, 128], BF16)
            kbg = nb("kbg", [128, 4, 128], BF16)
            kdec = nb("kdec", [128, 4, 128], BF16)
            u_ = nb("u", [128, 4, 128])
            wT = nb("wT", [128, 4, 128], BF16)
            qdT = nb("qdT", [128, 4, 128], BF16)
            vnew = nb("vnew", [128, 4, 128], BF16)
            osb = nb("osb", [128, 4, 128])
            Sb = self.gd_Sb[l]
            Sf = self.gd_Sf[l]
            halo = self.gd_halo[l]
            for ti in range(12):
                for j in range(4):
                    S.op("pool" if (ti + j) % 2 else "dve",
                         lambda e, ti=ti, j=j: e.tensor_scalar(diag.t[:, ti, j, :], self.ident, pc("conv", ti * 4 + j), None, ALU.mult),
                         r=[self.Bcst, BP], w=[diag])
            for st in range(2):
                t0 = st * 512
                S.op("act", lambda e: e.copy(zb.t[:, :, 0:3], halo.ap), r=[halo], w=[zb])
                for ti in range(12):
                    ps = self.pb()
                    self.inproj_fm(l, 10 + ti, 128, hT, Bh, t0, 512, ps)
                    S.op("act", lambda e, ti=ti, ps=ps: e.copy(zb.t[:, ti, 3:515], ps.ap), r=[ps], w=[zb])
                S.op("act", lambda e: e.copy(halo.ap, zb.t[:, :, 512:515]), r=[zb], w=[halo])
                for ti in range(12):
                    kind, hd = divmod(ti, 4)
                    ps = self.pb()
                    for j in range(4):
                        self.mm(ps, ps.ap, diag.t[:, ti, j, :], zb.t[:, ti, j:j + 512], r=[diag, zb], start=(j == 0), stop=(j == 3))
                    if kind == 2:
                        S.op("act", lambda e, ps=ps, hd=hd: e.activation(out=vb.t[:, hd, :], in_=ps.ap, func=AF.Silu), r=[ps], w=[vb])
                        continue
                    S.op("act", lambda e, ps=ps: e.activation(out=tf[0].ap, in_=ps.ap, func=AF.Silu), r=[ps], w=[tf[0]])
                    S.op("act", lambda e: e.activation(out=tb[0].ap, in_=tf[0].ap, func=AF.Square), r=[tf[0]], w=[tb[0]])
                    p2 = self.pb()
                    self.mm(p2, p2.ap, self.onesb, tb[0].ap, r=[tb[0], self.Bcstb])
                    S.op("act", lambda e, p2=p2: e.activation(out=tf[1].ap, in_=p2.ap, func=AF.Sqrt), r=[p2], w=[tf[1]])
                    S.op("dve", lambda e: e.tensor_scalar(tf[1].ap, tf[1].ap, 1e-12, None, ALU.max), r=[tf[1]], w=[tf[1]])
                    S.op("dve", lambda e: e.reciprocal(tf[1].ap, tf[1].ap), r=[tf[1]], w=[tf[1]])
                    if kind == 0:
                        S.op("dve", lambda e, hd=hd: e.scalar_tensor_tensor(out=qn.t[:, hd, :], in0=tf[0].ap, scalar=128.0 ** -0.5, in1=tf[1].ap,
                                                                           op0=ALU.mult, op1=ALU.mult), r=[tf[0], tf[1]], w=[qn])
                    else:
                        S.op("dve", lambda e, hd=hd: e.tensor_tensor(kn.t[:, hd, :], tf[0].ap, tf[1].ap, ALU.mult), r=[tf[0], tf[1]], w=[kn])
                self.ck('gdA')
                for hd in range(4):
                    ps = self.pb()
                    self.inproj_fm(l, 22 + hd, 128, hT, Bh, t0, 512, ps)
                    S.op("act", lambda e, ps=ps, hd=hd: e.activation(out=sg.t[:, hd, :], in_=ps.ap, func=AF.Silu), r=[ps], w=[sg])
                gslot = self.ring_load(self.win_s[l * NCH + 26][:, 0:64], 64)
                gw = self.wring_t[gslot]
                gB = self.wring[gslot]
                for cc in range(4):
                    c0 = t0 + cc * 128
                    csl = slice(cc * 128, (cc + 1) * 128)
                    pg = self.pb()
                    for dt in range(8):
                        self.mm(pg, pg.ap[:, 0:8], hT[:, dt, c0:c0 + 128], gw[:, dt * 8:(dt + 1) * 8], r=[gB, Bh], start=(dt == 0), stop=(dt == 7))
                    S.op("act", lambda e, pg=pg: e.activation(out=gt.t[:, 0:4], in_=pg.ap[:, 0:4], func=AF.Sigmoid), r=[pg], w=[gt])
                    S.op("dve", lambda e, pg=pg: e.tensor_tensor(gt.t[:, 4:8], pg.ap[:, 4:8], pc("dtb", 0, 4), ALU.add), r=[pg, BP, gt], w=[gt])
                    S.op("act", lambda e: e.activation(out=gt.t[:, 4:8], in_=gt.t[:, 4:8], func=AF.Exp), r=[gt], w=[gt])
                    S.op("act", lambda e: e.activation(out=gt.t[:, 4:8], in_=gt.t[:, 4:8], func=AF.Ln, bias=cst[:, 259:260]), r=[gt, self.Bcst], w=[gt])
                    S.op("dve", lambda e: e.tensor_tensor(gt.t[:, 4:8], gt.t[:, 4:8], pc("nA", 0, 4), ALU.mult), r=[gt, BP], w=[gt])
                    self.ck('gdB')
                    pq = self.pb()
                    self.mm(pq, pq.ap[:, 0:4], LEm, gt.t[:, 4:8], r=[gt, self.Bcst])
                    self.mm(pq, pq.ap[:, 4:8], GTm, gt.t[:, 4:8], r=[gt, self.Bcst])
                    S.op("act", lambda e, pq=pq: e.activation(out=ge.ap, in_=pq.ap[:, 0:8], func=AF.Exp), r=[pq], w=[ge])
                    S.op("dve", lambda e: e.tensor_tensor(beg.ap, gt.t[:, 0:4], ge.t[:, 0:4], ALU.mult), r=[gt, ge], w=[beg])
                    for hd in range(4):
                        S.op("pool" if hd % 2 else "dve", lambda e, hd=hd: e.tensor_scalar(Lh.t[:, hd, :], LEm, gt.t[:, 4 + hd: 5 + hd], None, ALU.mult),
                             r=[gt, self.Bcst], w=[Lh])
                    pd = self.pb()
                    pe_ = self.pb()
                    for hd in range(4):
                        hs = slice(hd * 128, (hd + 1) * 128)
                        self.mm(pd, pd.ap[:, hs], Lh.t[:, hd, :], GTm, r=[Lh, self.Bcst], start=True, stop=False)
                        self.mm(pd, pd.ap[:, hs], self.ident, NEG, r=[self.Bcst], start=False, stop=True)
                        self.mm(pe_, pe_.ap[:, hs], ONESF, Lh.t[:, hd, :], r=[Lh, self.Bcst])
                    S.op("act", lambda e, pd=pd: e.activation(out=dec.ap.rearrange("p a b -> p (a b)"), in_=pd.ap, func=AF.Exp), r=[pd], w=[dec])
                    S.op("act", lambda e, pe_=pe_: e.activation(out=egB.ap.rearrange("p a b -> p (a b)"), in_=pe_.ap, func=AF.Exp), r=[pe_], w=[egB])
                    S.op("pool", lambda e: e.tensor_tensor(dsn.ap, dec.ap, NGT4, ALU.mult), r=[dec, self.Bcst], w=[dsn])
                    self.ck('gdC')
                    pk = self.pb()
                    pqk = self.pb()
                    for hd in range(4):
                        hs = slice(hd * 128, (hd + 1) * 128)
                        self.mm(pk, pk.ap[:, hs], kn.t[:, hd, csl], kn.t[:, hd, csl], r=[kn])
                        self.mm(pqk, pqk.ap[:, hs], qn.t[:, hd, csl], kn.t[:, hd, csl], r=[qn, kn])
                    for hd in range(4):
                        hs = slice(hd * 128, (hd + 1) * 128)
                        S.op("dve", lambda e, hd=hd, hs=hs, pk=pk: e.scalar_tensor_tensor(out=NnB.t[:, hd, :], in0=pk.ap[:, hs], scalar=gt.t[:, hd:hd + 1],
                                                                                         in1=dsn.t[:, hd, :], op0=ALU.mult, op1=ALU.mult),
                             r=[pk, gt, dsn], w=[NnB])
                    S.op("dve", lambda e, pqk=pqk: e.tensor_tensor(att.ap.rearrange("p a b -> p (a b)"), pqk.ap, dec.ap.rearrange("p a b -> p (a b)"), ALU.mult),
                         r=[pqk, dec], w=[att])
                    self.ck('gdD')
                    pt1 = self.pb()
                    pt2 = self.pb()
                    ptn = self.pb()
                    b1 = pt1.ap.bitcast(BF16)
                    b2 = pt2.ap.bitcast(BF16)
                    for hd in range(4):
                        hs = slice(hd * 128, (hd + 1) * 128)
                        self.tr(ptn, ptn.ap[:, hs], NnB.t[:, hd, :], r=[NnB], f32=True)
                        self.tr(pt1, b1[:, 512 + hd * 128: 512 + (hd + 1) * 128], att.t[:, hd, :], r=[att])
                        self.tr(pt2, b2[:, hs], kn.t[:, hd, csl], r=[kn])
                        self.tr(pt2, b2[:, 512 + hd * 128: 512 + (hd + 1) * 128], vb.t[:, hd, csl], r=[vb])
                    S.op("act", lambda e: e.copy(MmB.ap.rearrange("p a b -> p (a b)"), ptn.ap), r=[ptn], w=[MmB])
                    S.op("dve", lambda e: e.tensor_copy(attT.ap.rearrange("p a b -> p (a b)"), b1[:, 512:1024]), r=[pt1], w=[attT])
                    for hd in range(4):
                        hs = slice(hd * 128, (hd + 1) * 128)
                        S.op("act", lambda e, hd=hd, hs=hs: e.activation(out=kbg.t[:, hd, :], in_=b2[:, hs], func=AF.Copy, scale=beg.t[:, hd:hd + 1]),
                             r=[pt2, beg], w=[kbg])
                        S.op("dve", lambda e, hd=hd, hs=hs: e.tensor_scalar(kdec.t[:, hd, :], b2[:, hs], ge.t[:, 4 + hd: 5 + hd], None, ALU.mult),
                             r=[pt2, ge], w=[kdec])
                        S.op("pool" if False else "act", lambda e, hd=hd: e.activation(out=bv.t[:, hd, :], in_=b2[:, 512 + hd * 128: 512 + (hd + 1) * 128],
                                                                                         func=AF.Copy, scale=gt.t[:, hd:hd + 1]),
                             r=[pt2, gt], w=[bv])
                    self.ck('gdE')
                    self.neumann2("gd", NnB, MmB, N2, M2, QB, 4, f32=True)
                    S.op("act", lambda e: e.copy(Qbf.ap, QB.ap), r=[QB], w=[Qbf])
                    self.ck('gdF')
                    pu = self.pb()
                    pw = self.pb()
                    for hd in range(4):
                        hs = slice(hd * 128, (hd + 1) * 128)
                        self.mm(pu, pu.ap[:, hs], Qbf.t[:, hd, :], bv.t[:, hd, :], r=[Qbf, bv])
                        self.mm(pw, pw.ap[:, hs], kbg.t[:, hd, :], Qbf.t[:, hd, :], r=[Qbf, kbg])
                    S.op("act", lambda e, pu=pu: e.copy(u_.ap.rearrange("p a b -> p (a b)"), pu.ap), r=[pu], w=[u_])
                    S.op("dve", lambda e, pw=pw: e.tensor_copy(wT.ap.rearrange("p a b -> p (a b)"), pw.ap), r=[pw], w=[wT])
                    for hd in range(4):
                        S.op("pool", lambda e, hd=hd: e.tensor_tensor(qdT.t[:, hd, :], qn.t[:, hd, csl], egB.t[:, hd, :], ALU.mult), r=[qn, egB], w=[qdT])
                    self.ck('gdG')
                    pws = self.pb()
                    for hd in range(4):
                        hs = slice(hd * 128, (hd + 1) * 128)
                        self.mm(pws, pws.ap[:, hs], wT.t[:, hd, :], Sb.t[:, hd, :], r=[wT, Sb])
                    S.op("dve", lambda e, pws=pws: e.tensor_tensor(vnew.ap.rearrange("p a b -> p (a b)"), u_.ap.rearrange("p a b -> p (a b)"), pws.ap, ALU.subtract),
                         r=[pws, u_], w=[vnew])
                    po = self.pb()
                    pkv = self.pb()
                    for hd in range(4):
                        hs = slice(hd * 128, (hd + 1) * 128)
                        self.mm(po, po.ap[:, hs], Sb.t[:, hd, :], qdT.t[:, hd, :], r=[Sb, qdT], start=True, stop=False)
                        self.mm(po, po.ap[:, hs], vnew.t[:, hd, :], attT.t[:, hd, :], r=[vnew, attT], start=False, stop=True)
                        self.mm(pkv, pkv.ap[:, hs], kdec.t[:, hd, :], vnew.t[:, hd, :], r=[kdec, vnew])
                    for hd in range(4):
                        hs = slice(hd * 128, (hd + 1) * 128)
                        S.op("dve", lambda e, hd=hd, hs=hs, pkv=pkv: e.scalar_tensor_tensor(out=Sf.t[:, hd, :], in0=Sf.t[:, hd, :], scalar=egB.t[:, hd, 127:128],
                                                                                          in1=pkv.ap[:, hs], op0=ALU.mult, op1=ALU.add),
                             r=[pkv, Sf, egB], w=[Sf])
                    S.op("act", lambda e: e.copy(Sb.ap, Sf.ap), r=[Sf], w=[Sb])
                    S.op("act", lambda e, po=po: e.copy(osb.ap.rearrange("p a b -> p (a b)"), po.ap), r=[po], w=[osb])
                    S.op("act", lambda e, po=po: e.activation(out=tb[1].ap, in_=po.ap, func=AF.Square), r=[po], w=[tb[1]])
                    pn = self.pb()
                    self.mm(pn, pn.ap, self.onesb, tb[1].ap, r=[tb[1], self.Bcstb])
                    S.op("act", lambda e, pn=pn: e.activation(out=tf[2].ap, in_=pn.ap, func=AF.Sqrt, scale=1.0 / 128, bias=self.eps_ap), r=[pn, self.Bcst], w=[tf[2]])
                    S.op("dve", lambda e: e.reciprocal(tf[2].ap, tf[2].ap), r=[tf[2]], w=[tf[2]])
                    S.op("dve", lambda e: e.scalar_tensor_tensor(out=tf[2].ap, in0=osb.ap.rearrange("p a b -> p (a b)"), scalar=pc("gnw"), in1=tf[2].ap,
                                                                 op0=ALU.mult, op1=ALU.mult), r=[osb, tf[2], BP], w=[tf[2]])
                    for hd in range(4):
                        S.op("pool" if hd % 2 else "dve", lambda e, hd=hd: e.tensor_tensor(mixT[:, 4 + hd, c0:c0 + 128], tf[2].t[:, hd * 128:(hd + 1) * 128], sg.t[:, hd, csl], ALU.mult),
                             r=[tf[2], sg], w=[Bmix])
            S.barrier()

    def neumann2(self, tag, Nn, Mm, N2, M2, Q, nb, f32=False):
        S = self.S
        idb = (self.ident if f32 else self.identb).unsqueeze(1).to_broadcast([128, nb, 128])
        S.op("pool", lambda e: e.tensor_tensor(Q.ap, Mm.ap, idb, ALU.add), r=[Mm, self.Bcst if f32 else self.Bcstb], w=[Q])
        cur = (Nn, Mm)
        nxt = (N2, M2)
        W = nb * 128
        for lev in range(6):
            last = (lev == 5)
            pn = self.pb()
            for b in range(nb):
                self.mm(pn, pn.ap[:, b * 128:(b + 1) * 128], cur[1].t[:, b, :], cur[0].t[:, b, :], r=[cur[0], cur[1]])
            S.op("act", lambda e, pn=pn, o=nxt[0]: e.copy(o.ap.rearrange("p a b -> p (a b)"), pn.ap[:, 0:W]), r=[pn], w=[nxt[0]])
            if not last:
                pm = self.pb()
                for b in range(nb):
                    self.mm(pm, pm.ap[:, b * 128:(b + 1) * 128], cur[0].t[:, b, :], cur[1].t[:, b, :], r=[cur[0], cur[1]])
                S.op("dve", lambda e, pm=pm, o=nxt[1]: e.tensor_copy(o.ap.rearrange("p a b -> p (a b)"), pm.ap[:, 0:W]), r=[pm], w=[nxt[1]])
            pq = self.pb()
            for b in range(nb):
                self.mm(pq, pq.ap[:, b * 128:(b + 1) * 128], nxt[0].t[:, b, :], Q.t[:, b, :], r=[nxt[0], Q])
            S.op("dve", lambda e, pq=pq: e.tensor_tensor(Q.ap.rearrange("p a b -> p (a b)"), pq.ap[:, 0:W], Q.ap.rearrange("p a b -> p (a b)"), ALU.add),
                 r=[pq, Q], w=[Q])
            cur, nxt = nxt, cur

    def sincos(self, es, tag, ang, n, out_s, out_c, shape):
        S = self.S
        TWO_PI = 6.283185307179586
        es = self.es_sc = ExitStack()
        k = self.nb(tag + "_k", [64, n], F32, es)
        ki = self.nb(tag + "_ki", [64, n], mybir.dt.int32, es)
        r = self.nb(tag + "_r", [64, n], F32, es)
        m = self.nb(tag + "_m", [64, n], F32, es)
        for (off, out) in ((0.0, out_s), (1.5707963267948966, out_c)):
            S.op("dve", lambda e: e.tensor_scalar(k.ap, ang.ap, off, 1.0 / TWO_PI, ALU.add, ALU.mult), r=[ang], w=[k])
            S.op("dve", lambda e: e.tensor_copy(ki.ap, k.ap), r=[k], w=[ki])
            S.op("dve", lambda e: e.tensor_copy(k.ap, ki.ap), r=[ki], w=[k])
            S.op("dve", lambda e: e.tensor_scalar(r.ap, ang.ap, off, None, ALU.add), r=[ang], w=[r])
            S.op("dve", lambda e: e.scalar_tensor_tensor(out=r.ap, in0=k.ap, scalar=-TWO_PI, in1=r.ap, op0=ALU.mult, op1=ALU.add), r=[k, r], w=[r])
            S.op("dve", lambda e: e.tensor_scalar(m.ap, r.ap, 3.141592653589793, None, ALU.is_gt), r=[r], w=[m])
            S.op("dve", lambda e: e.scalar_tensor_tensor(out=r.ap, in0=m.ap, scalar=-TWO_PI, in1=r.ap, op0=ALU.mult, op1=ALU.add), r=[m, r], w=[r])
            S.op("dve", lambda e: e.tensor_scalar(m.ap, r.ap, -3.141592653589793, None, ALU.is_lt), r=[r], w=[m])
            S.op("dve", lambda e: e.scalar_tensor_tensor(out=r.ap, in0=m.ap, scalar=TWO_PI, in1=r.ap, op0=ALU.mult, op1=ALU.add), r=[m, r], w=[r])
            S.op("dve", lambda e: e.tensor_scalar(r.ap, r.ap, 3.1415925, -3.1415925, ALU.min, ALU.max), r=[r], w=[r])
            S.op("act", lambda e, out=out: e.activation(out=out.ap, in_=r.ap, func=AF.Sin), r=[r], w=[out])
        S.barrier()
        es.close()

    def cmul(self, o_re, o_im, a_re, a_im, b_re, b_im, t1, t2, r, w, neg_im=False):
        S = self.S
        S.op("dve", lambda e: e.tensor_tensor(t1, a_re, b_re, ALU.mult), r=r, w=w)
        S.op("pool", lambda e: e.tensor_tensor(t2, a_im, b_im, ALU.mult), r=r, w=w)
        S.op("dve", lambda e: e.tensor_tensor(o_re, t1, t2, ALU.subtract), r=r + w, w=w)
        S.op("dve", lambda e: e.tensor_tensor(t1, a_re, b_im, ALU.mult), r=r + w, w=w)
        S.op("pool", lambda e: e.tensor_tensor(t2, a_im, b_re, ALU.mult), r=r + w, w=w)
        if neg_im:
            S.op("dve", lambda e: e.scalar_tensor_tensor(out=o_im, in0=t1, scalar=-1.0, in1=t2, op0=ALU.mult, op1=ALU.subtract), r=r + w, w=w)
        else:
            S.op("dve", lambda e: e.tensor_tensor(o_im, t1, t2, ALU.add), r=r + w, w=w)

    def s5_setup(self, l):
        S = self.S
        cst = self.cst
        with ExitStack() as es:
            nb = lambda n, s, dt=F32: self.nb("s5s_" + n, s, dt, es)
            sp = nb("sp", [64, 1328])
            S.dma("par", sp.ap, self.s5p_d[l], w=[sp])
            TAU = cst[0:64, 2816:2880]
            MIDX = cst[0:64, 2880:2944]
            M0 = cst[0:64, 2944:3008]
            are, aim, ldt = sp.t[:, 0:16], sp.t[:, 16:32], sp.t[:, 32:48]
            g4 = lambda ap, a, b: ap.rearrange("p (a b) -> p a b", b=b)
            bre, bim = g4(sp.t[:, 48:304], 16, 16), g4(sp.t[:, 304:560], 16, 16)
            cre, cim = g4(sp.t[:, 560:816], 16, 16), g4(sp.t[:, 816:1072], 16, 16)
            sm = nb("sm", [64, 16, 16])
            sl = lambda i: sm.t[:, i, :]
            S.op("act", lambda e: e.activation(out=sl(0), in_=ldt, func=AF.Exp), r=[sp], w=[sm])
            S.op("dve", lambda e: e.tensor_tensor(sl(1), sl(0), are, ALU.mult), r=[sm, sp], w=[sm])
            S.op("dve", lambda e: e.tensor_tensor(sl(2), sl(0), aim, ALU.mult), r=[sm, sp], w=[sm])
            ang = nb("ang", [64, 1024])
            mag = nb("mag", [64, 1024])
            pwr = nb("pwr", [64, 1024])
            pwi = nb("pwi", [64, 1024])
            v3 = lambda b: b.ap.rearrange("p (a b) -> p a b", b=64)
            taub = TAU.unsqueeze(1).to_broadcast([64, 16, 64])
            S.op("dve", lambda e: e.tensor_tensor(v3(ang), sl(2).unsqueeze(2).to_broadcast([64, 16, 64]), taub, ALU.mult), r=[sm, self.Bcst], w=[ang])
            S.op("dve", lambda e: e.tensor_tensor(v3(mag), sl(1).unsqueeze(2).to_broadcast([64, 16, 64]), taub, ALU.mult), r=[sm, self.Bcst], w=[mag])
            S.op("act", lambda e: e.activation(out=mag.ap, in_=mag.ap, func=AF.Exp), r=[mag], w=[mag])
            self.sincos(es, "sc1", ang, 1024, pwi, pwr, None)
            S.op("dve", lambda e: e.tensor_tensor(pwr.ap, pwr.ap, mag.ap, ALU.mult), r=[pwr, mag], w=[pwr])
            S.op("dve", lambda e: e.tensor_tensor(pwi.ap, pwi.ap, mag.ap, ALU.mult), r=[pwi, mag], w=[pwi])
            PWr, PWi = v3(pwr), v3(pwi)
            a1r, a1i = PWr[:, :, 48], PWi[:, :, 48]
            S.op("dve", lambda e: e.tensor_scalar(sl(3), a1r, -1.0, None, ALU.add), r=[pwr], w=[sm])
            S.op("dve", lambda e: e.tensor_tensor(sl(4), are, are, ALU.mult), r=[sp], w=[sm])
            S.op("dve", lambda e: e.tensor_tensor(sl(5), aim, aim, ALU.mult), r=[sp], w=[sm])
            S.op("dve", lambda e: e.tensor_tensor(sl(4), sl(4), sl(5), ALU.add), r=[sm], w=[sm])
            S.op("dve", lambda e: e.reciprocal(sl(4), sl(4)), r=[sm], w=[sm])
            S.op("dve", lambda e: e.tensor_tensor(sl(5), sl(3), are, ALU.mult), r=[sm, sp], w=[sm])
            S.op("dve", lambda e: e.tensor_tensor(sl(6), a1i, aim, ALU.mult), r=[pwi, sp], w=[sm])
            S.op("dve", lambda e: e.tensor_tensor(sl(5), sl(5), sl(6), ALU.add), r=[sm], w=[sm])
            S.op("dve", lambda e: e.tensor_tensor(sl(5), sl(5), sl(4), ALU.mult), r=[sm], w=[sm])
            S.op("dve", lambda e: e.tensor_tensor(sl(6), a1i, are, ALU.mult), r=[pwi, sp], w=[sm])
            S.op("dve", lambda e: e.tensor_tensor(sl(7), sl(3), aim, ALU.mult), r=[sm, sp], w=[sm])
            S.op("dve", lambda e: e.tensor_tensor(sl(6), sl(6), sl(7), ALU.subtract), r=[sm], w=[sm])
            S.op("dve", lambda e: e.tensor_tensor(sl(6), sl(6), sl(4), ALU.mult), r=[sm], w=[sm])
            bbr = nb("bbr", [64, 16, 16])
            bbi = nb("bbi", [64, 16, 16])
            ta = nb("ta", [64, 2048])
            tb_ = nb("tb", [64, 2048])
            bc = lambda ap: ap.unsqueeze(2).to_broadcast([64, 16, 16])
            t3 = lambda b: b.t[:, 0:256].rearrange("p (a b) -> p a b", b=16)
            self.cmul(bbr.ap, bbi.ap, bc(sl(5)), bc(sl(6)), bre, bim, t3(ta), t3(tb_), [sm, sp], [bbr, bbi, ta, tb_])
            big_r = nb("big_r", [64, 8, 16, 16])
            big_i = nb("big_i", [64, 8, 16, 16])
            rm_r = nb("rm_r", [64, 8, 16, 16])
            rm_i = nb("rm_i", [64, 8, 16, 16])
            t4 = lambda b: b.ap.rearrange("p (a b c) -> p a b c", b=16, c=16)
            tabs = self.s5tb_s[l]
            stg = nb("stg", [128, 4096], BF16)
            Bbig = [pwr, pwi, bbr, bbi, sp]
            for gh in range(2):
                gs = slice(gh * 8, (gh + 1) * 8)
                pw4 = lambda T, i0: T[:, gs, i0:i0 + 16].unsqueeze(3).to_broadcast([64, 8, 16, 16])
                x4 = lambda ap: ap[:, gs, :].unsqueeze(2).to_broadcast([64, 8, 16, 16])
                self.cmul(big_r.ap, big_i.ap, pw4(PWr, 0), pw4(PWi, 0), x4(bbr.ap), x4(bbi.ap), t4(ta), t4(tb_), Bbig, [big_r, big_i, ta, tb_])
                for ri, big in enumerate((big_r, big_i)):
                    for kt in range(2):
                        ps = self.pb()
                        for gg in range(8):
                            self.tr(ps, ps.ap[:, gg * 64:(gg + 1) * 64], big.t[:, gg, kt * 8:(kt + 1) * 8, :].rearrange("p a b -> p (a b)"), r=[big], f32=True)
                        dst = stg.t[:, (ri * 2 + kt) * 512:(ri * 2 + kt + 1) * 512]
                        S.op("act", lambda e, ps=ps, dst=dst: e.copy(dst, ps.ap), r=[ps], w=[stg])
                for ri in range(2):
                    for kt in range(2):
                        o0 = ri * 2048 + kt * 1024 + gh * 512
                        S.dma("s5t", tabs[:, o0:o0 + 512], stg.t[:, (ri * 2 + kt) * 512:(ri * 2 + kt + 1) * 512], r=[stg])
                self.cmul(big_r.ap, big_i.ap, pw4(PWr, 16), pw4(PWi, 16), x4(bbr.ap), x4(bbi.ap), t4(ta), t4(tb_), Bbig, [big_r, big_i, ta, tb_])
                self.cmul(rm_r.ap, rm_i.ap, pw4(PWr, 32), pw4(PWi, 32), x4(cre), x4(cim), t4(ta), t4(tb_), Bbig, [rm_r, rm_i, ta, tb_], neg_im=True)
                for kt in range(2):
                    MK = cst[:, 2304 + kt * 256: 2304 + (kt + 1) * 256]
                    for g2 in range(4):
                        ps = self.pb()
                        for gg in range(2):
                            g = g2 * 2 + gg
                            lr = big_r.t[:, g, kt * 8:(kt + 1) * 8, :].rearrange("p a b -> p (a b)")
                            li = big_i.t[:, g, kt * 8:(kt + 1) * 8, :].rearrange("p a b -> p (a b)")
                            self.mm(ps, ps.ap[:, gg * 256:(gg + 1) * 256], lr, rm_r.t[:, g, :, :].rearrange("p a b -> p (a b)"), r=[big_r, rm_r], start=True, stop=False)
                            self.mm(ps, ps.ap[:, gg * 256:(gg + 1) * 256], li, rm_i.t[:, g, :, :].rearrange("p a b -> p (a b)"), r=[big_i, rm_i], start=False, stop=True)
                        dst = stg.t[:, kt * 2048 + g2 * 512: kt * 2048 + (g2 + 1) * 512].rearrange("p (a b) -> p a b", b=256)
                        S.op("dve", lambda e, ps=ps, dst=dst, MK=MK: e.tensor_tensor(dst, ps.ap.rearrange("p (a b) -> p a b", b=256),
                                                                                   MK.unsqueeze(1).to_broadcast([128, 2, 256]), ALU.mult),
                             r=[ps, self.Bcst], w=[stg])
                    o0 = 4096 + kt * 4096 + gh * 2048
                    S.dma("s5t", tabs[:, o0:o0 + 2048], stg.t[:, kt * 2048:(kt + 1) * 2048], r=[stg])
                self.cmul(big_r.ap, big_i.ap, pw4(PWr, 48), pw4(PWi, 48), x4(cre), x4(cim), t4(ta), t4(tb_), Bbig, [big_r, big_i, ta, tb_], neg_im=True)
                S.op("act", lambda e: e.copy(stg.t[0:64, 0:2048], big_r.ap.rearrange("p a b c -> p (a b c)")), r=[big_r], w=[stg])
                S.op("act", lambda e: e.copy(stg.t[0:64, 2048:4096], big_i.ap.rearrange("p a b c -> p (a b c)")), r=[big_i], w=[stg])
                for ri in range(2):
                    o0 = 12288 + ri * 4096 + gh * 2048
                    S.dma("s5t", tabs[0:64, o0:o0 + 2048], stg.t[0:64, ri * 2048:(ri + 1) * 2048], r=[stg])
            ft = nb("ft", [64, 3 * 1024 + 64])
            S.op("pool", lambda e: e.memset(ft.ap, 0.0), w=[ft])
            S.op("act", lambda e: e.activation(out=sl(8), in_=sl(1), func=AF.Exp, scale=16.0), r=[sm], w=[sm])
            a16 = nb("a16", [64, 16])
            s16 = nb("s16", [64, 16])
            c16 = nb("c16", [64, 16])
            S.op("dve", lambda e: e.tensor_scalar(a16.ap, sl(2), 16.0, None, ALU.mult), r=[sm], w=[a16])
            self.sincos(es, "sc2", a16, 16, s16, c16, None)
            cr = ft.t[:, 0:1024].rearrange("p (a b) -> p a b", b=64)
            sr = ft.t[:, 1024:2048].rearrange("p (a b) -> p a b", b=64)
            S.op("dve", lambda e: e.memset(cr[:, :, 0:1], 1.0), w=[ft])
            S.op("dve", lambda e: e.memset(sr[:, :, 0:1], 0.0), w=[ft])
            S.op("dve", lambda e: e.tensor_copy(cr[:, :, 1], c16.ap), r=[c16], w=[ft])
            S.op("dve", lambda e: e.tensor_copy(sr[:, :, 1], s16.ap), r=[s16], w=[ft])
            pr = nb("pr", [64, 16])
            pi_ = nb("pi", [64, 16])
            S.op("dve", lambda e: e.tensor_copy(pr.ap, c16.ap), r=[c16], w=[pr])
            S.op("dve", lambda e: e.tensor_copy(pi_.ap, s16.ap), r=[s16], w=[pi_])
            tq = nb("tq", [64, 16, 32])
            tq2 = nb("tq2", [64, 16, 32])
            n = 2
            while n < 128:
                S.op("dve", lambda e: e.tensor_tensor(sl(9), pr.ap, pr.ap, ALU.mult), r=[pr], w=[sm])
                S.op("dve", lambda e: e.tensor_tensor(sl(10), pi_.ap, pi_.ap, ALU.mult), r=[pi_], w=[sm])
                S.op("dve", lambda e: e.tensor_tensor(sl(11), pr.ap, pi_.ap, ALU.mult), r=[pr, pi_], w=[sm])
                S.op("dve", lambda e: e.tensor_tensor(pr.ap, sl(9), sl(10), ALU.subtract), r=[sm], w=[pr])
                S.op("dve", lambda e: e.tensor_scalar(pi_.ap, sl(11), 2.0, None, ALU.mult), r=[sm], w=[pi_])
                if n == 64:
                    break
                bcp = lambda ap, n=n: ap.unsqueeze(2).to_broadcast([64, 16, n])
                self.cmul(cr[:, :, n:2 * n], sr[:, :, n:2 * n], cr[:, :, 0:n], sr[:, :, 0:n], bcp(pr.ap), bcp(pi_.ap),
                          tq.t[:, :, 0:n], tq2.t[:, :, 0:n], [ft, pr, pi_], [ft, tq, tq2])
                n *= 2
            rho = ft.t[:, 2048:3072].rearrange("p (a b) -> p a b", b=64)
            S.op("dve", lambda e: e.tensor_tensor(rho, sl(8).unsqueeze(2).to_broadcast([64, 16, 64]), M0.unsqueeze(1).to_broadcast([64, 16, 64]), ALU.mult),
                 r=[sm, self.Bcst], w=[ft])
            S.op("dve", lambda e: e.tensor_copy(ft.t[:, 3072:3088], sl(8)), r=[sm], w=[ft])
            S.op("dve", lambda e: e.tensor_copy(ft.t[:, 3088:3104], pr.ap), r=[pr], w=[ft])
            S.op("dve", lambda e: e.tensor_copy(ft.t[:, 3104:3120], pi_.ap), r=[pi_], w=[ft])
            S.dma("s5t", self.s5tf_s[l], ft.ap, r=[ft])
            S.barrier()

    def s5(self, l, tt, hT, Bh, mixT, Bmix):
        S = self.S
        P = self.par[l]
        BP = self.Bpar[l]
        pc = lambda name, i=0, n=1: P[:, PO[name] + i: PO[name] + i + n]
        TB = self.s5tb_s[l]
        with ExitStack() as es:
            nb = lambda n, s, dt=F32, es_=es: self.nb("s5_" + n, s, dt, es_)
            Ub = nb("Ub", [64, 16, 16, 16], BF16)
            UT = nb("UT", [128, 16, 2, 64], BF16)
            Xb = [nb(f"Xb{ri}", [64, 16, 65], BF16) for ri in range(2)]
            zbm = nb("zbm", [64, 16, 256], BF16)
            E = [nb(f"E{ri}", [64, 1024]) for ri in range(2)]
            Zc = self.s5_Zc[l]
            Xc = self.s5_Xc[l]
            dbc = self.s5d[l]
            with ExitStack() as esa:
                w1t = nb("w1t", [128, 4096], BF16, esa)
                S.dma("s5l", w1t.ap, TB[:, 0:4096], w=[w1t])
                W1 = lambda ri, kt, g: w1t.t[:, ri * 2048 + kt * 1024 + g * 64: ri * 2048 + kt * 1024 + (g + 1) * 64]
                sl0 = self.ring_load(self.win_s[l * NCH + 8][:, 0:1024], 1024)
                sl1 = self.ring_load(self.win_s[l * NCH + 9][:, 0:1024], 1024)
                for i in range(16):
                    ps = self.pb()
                    for half, slot in ((0, sl0), (1, sl1)):
                        wt = self.wring_t[slot]
                        for dt in range(8):
                            self.mm(ps, ps.ap[0:64, half * 128:(half + 1) * 128], hT[:, dt, i:TT:16], wt[:, dt * 128:(dt + 1) * 128],
                                    r=[self.wring[slot], Bh], start=(dt == 0), stop=(dt == 7))
                    eng = "act" if i % 2 == 0 else "dve"
                    src_ = ps.ap[0:64, 0:256].rearrange("p (g h) -> p g h", h=16)
                    if eng == "act":
                        S.op("act", lambda e, i=i, src_=src_: e.copy(Ub.t[:, :, i, :], src_), r=[ps], w=[Ub])
                    else:
                        S.op("dve", lambda e, i=i, src_=src_: e.tensor_copy(Ub.t[:, :, i, :], src_), r=[ps], w=[Ub])
                for g8 in range(2):
                    ps = self.pb()
                    psb = ps.ap.bitcast(BF16)
                    for gg in range(8):
                        g = g8 * 8 + gg
                        for kt in range(2):
                            self.tr(ps, psb[:, (gg * 2 + kt) * 64:(gg * 2 + kt + 1) * 64],
                                    Ub.t[:, g, kt * 8:(kt + 1) * 8, :].rearrange("p a b -> p (a b)"), r=[Ub])
                    dst = UT.t[:, g8 * 8:(g8 + 1) * 8, :, :].rearrange("p a b c -> p (a b c)")
                    if g8 == 0:
                        S.op("act", lambda e, psb=psb, dst=dst: e.copy(dst, psb[:, 0:1024]), r=[ps], w=[UT])
                    else:
                        S.op("dve", lambda e, psb=psb, dst=dst: e.tensor_copy(dst, psb[:, 0:1024]), r=[ps], w=[UT])
                for ri in range(2):
                    for g8 in range(2):
                        ps = self.pb()
                        for gg in range(8):
                            g = g8 * 8 + gg
                            for kt in range(2):
                                self.mm(ps, ps.ap[0:64, gg * 64:(gg + 1) * 64], W1(ri, kt, g), UT.t[:, g, kt, :], r=[w1t, UT], start=(kt == 0), stop=(kt == 1))
                        S.op("act", lambda e, ps=ps, ri=ri, g8=g8: e.copy(E[ri].t[:, g8 * 512:(g8 + 1) * 512], ps.ap[0:64, :]), r=[ps], w=[E[ri]])
                S.barrier()
            with ExitStack() as esb:
                ft = nb("ft", [64, 3136], F32, esb)
                Et = [nb(f"Et{ri}", [64, 1024], F32, esb) for ri in range(2)]
                t1 = nb("t1", [64, 1024], F32, esb)
                t2 = nb("t2", [64, 1024], F32, esb)
                S.dma("s5l", ft.ap, self.s5tf_s[l], w=[ft])
                cr = ft.t[:, 0:1024]
                sr = ft.t[:, 1024:2048]
                RHO = ft.t[:, 2048:3072]
                rho16 = ft.t[:, 3072:3088]
                c64 = ft.t[:, 3088:3104]
                s64 = ft.t[:, 3104:3120]
                Zs = E
                S.op("dve", lambda e: e.tensor_tensor(t1.ap, cr, E[0].ap, ALU.mult), r=[ft, E[0]], w=[t1])
                S.op("pool", lambda e: e.tensor_tensor(t2.ap, sr, E[1].ap, ALU.mult), r=[ft, E[1]], w=[t2])
                S.op("dve", lambda e: e.tensor_tensor(Et[0].ap, t1.ap, t2.ap, ALU.add), r=[t1, t2], w=[Et[0]])
                S.op("dve", lambda e: e.tensor_tensor(t1.ap, cr, E[1].ap, ALU.mult), r=[ft, E[1], Et[0]], w=[t1])
                S.op("pool", lambda e: e.tensor_tensor(t2.ap, sr, E[0].ap, ALU.mult), r=[ft, E[0], Et[0]], w=[t2])
                S.op("dve", lambda e: e.tensor_tensor(Et[1].ap, t1.ap, t2.ap, ALU.subtract), r=[t1, t2], w=[Et[1]])
                for ri in range(2):
                    e0 = Et[ri].ap.rearrange("p (g m) -> p g m", m=64)[:, :, 0]
                    S.op("dve", lambda e, ri=ri: e.tensor_tensor(t1.t[:, 0:16], rho16, Zc.t[:, ri, :], ALU.mult), r=[ft, Zc, Et[1]], w=[t1])
                    S.op("dve", lambda e, e0=e0: e.tensor_tensor(e0, e0, t1.t[:, 0:16], ALU.add), r=[t1, Et[ri]], w=[Et[ri]])
                    S.op("dve", lambda e, ri=ri: e.tensor_tensor_scan(Zs[ri].ap, RHO, Et[ri].ap, 0.0, ALU.mult, ALU.add), r=[ft, Et[ri], E[0], E[1]], w=[Zs[ri]])
                z3 = lambda b_: b_.ap.rearrange("p (g m) -> p g m", m=64)
                S.op("dve", lambda e: e.tensor_tensor(t1.ap, cr, Zs[0].ap, ALU.mult), r=[ft, Zs[0]], w=[t1])
                S.op("pool", lambda e: e.tensor_tensor(t2.ap, sr, Zs[1].ap, ALU.mult), r=[ft, Zs[1]], w=[t2])
                S.op("act", lambda e: e.copy(Xb[0].t[:, :, 0], Xc.t[:, 0, :]), r=[Xc], w=[Xb[0]])
                S.op("act", lambda e: e.copy(Xb[1].t[:, :, 0], Xc.t[:, 1, :]), r=[Xc], w=[Xb[1]])
                S.op("dve", lambda e: e.tensor_tensor(Xb[0].t[:, :, 1:65], z3(t1), z3(t2), ALU.subtract), r=[t1, t2], w=[Xb[0]])
                S.op("dve", lambda e: e.tensor_tensor(t1.ap, cr, Zs[1].ap, ALU.mult), r=[ft, Zs[1], Xb[0]], w=[t1])
                S.op("pool", lambda e: e.tensor_tensor(t2.ap, sr, Zs[0].ap, ALU.mult), r=[ft, Zs[0], Xb[0]], w=[t2])
                S.op("dve", lambda e: e.tensor_tensor(Xb[1].t[:, :, 1:65], z3(t1), z3(t2), ALU.add), r=[t1, t2], w=[Xb[1]])
                S.op("act", lambda e: e.copy(Xc.t[:, 0, :], Xb[0].t[:, :, 64]), r=[Xb[0]], w=[Xc])
                S.op("act", lambda e: e.copy(Xc.t[:, 1, :], Xb[1].t[:, :, 64]), r=[Xb[1]], w=[Xc])
                zl = lambda ri: z3(Zs[ri])[:, :, 63]
                S.op("dve", lambda e: e.tensor_tensor(t1.t[:, 0:16], c64, zl(0), ALU.mult), r=[ft, Zs[0], Xb[1]], w=[t1])
                S.op("dve", lambda e: e.tensor_tensor(t1.t[:, 16:32], s64, zl(1), ALU.mult), r=[ft, Zs[1]], w=[t1])
                S.op("dve", lambda e: e.tensor_tensor(t1.t[:, 32:48], c64, zl(1), ALU.mult), r=[ft, Zs[1]], w=[t1])
                S.op("dve", lambda e: e.tensor_tensor(t1.t[:, 48:64], s64, zl(0), ALU.mult), r=[ft, Zs[0]], w=[t1])
                S.op("dve", lambda e: e.tensor_tensor(Zc.t[:, 0, :], t1.t[:, 0:16], t1.t[:, 16:32], ALU.subtract), r=[t1], w=[Zc])
                S.op("dve", lambda e: e.tensor_tensor(Zc.t[:, 1, :], t1.t[:, 32:48], t1.t[:, 48:64], ALU.add), r=[t1], w=[Zc])
                S.barrier()
            with ExitStack() as esc:
                toe = nb("toe", [128, 8192], BF16, esc)
                w2t = nb("w2t", [64, 8192], BF16, esc)
                yv = [nb(f"yv{i}", [64, 512], F32, esc) for i in range(3)]
                S.dma("s5l", toe.ap, TB[:, 4096:12288], w=[toe])
                S.dma("s5l", w2t.ap, TB[0:64, 12288:20480], w=[w2t])
                TOE = lambda kt, g: toe.t[:, kt * 4096 + g * 256: kt * 4096 + (g + 1) * 256]
                W2 = lambda ri, g: w2t.t[:, ri * 4096 + g * 256: ri * 4096 + (g + 1) * 256]
                v4 = lambda ap: ap.rearrange("p (g i h) -> p g i h", i=16, h=16)
                for g2 in range(8):
                    ps = self.pb()
                    for gg in range(2):
                        g = g2 * 2 + gg
                        o = ps.ap[0:64, gg * 256:(gg + 1) * 256]
                        self.mm(ps, o, UT.t[:, g, 0, :], TOE(0, g), r=[UT, toe], start=True, stop=False)
                        self.mm(ps, o, UT.t[:, g, 1, :], TOE(1, g), r=[UT, toe], start=False, stop=False)
                        self.mm(ps, o, Xb[0].t[:, g, 0:64], W2(0, g), r=[Xb[0], w2t], start=False, stop=False)
                        self.mm(ps, o, Xb[1].t[:, g, 0:64], W2(1, g), r=[Xb[1], w2t], start=False, stop=True)
                    y, t_, s_ = yv
                    dview = dbc.t[:, g2 * 32:(g2 + 1) * 32].rearrange("p (g h) -> p g h", h=16).unsqueeze(2).to_broadcast([64, 2, 16, 16])
                    S.op("pool", lambda e: e.tensor_tensor(v4(t_.ap), Ub.t[:, g2 * 2:(g2 + 1) * 2, :, :], dview, ALU.mult), r=[Ub, dbc], w=[t_])
                    S.op("dve", lambda e, ps=ps: e.tensor_tensor(y.ap, ps.ap[0:64, :], t_.ap, ALU.add), r=[ps, t_], w=[y])
                    S.op("act", lambda e: e.activation(out=t_.ap, in_=y.ap, func=AF.Square), r=[y], w=[t_])
                    S.op("dve", lambda e: e.tensor_scalar(t_.ap, t_.ap, 0.044715, 1.0, ALU.mult, ALU.add), r=[t_], w=[t_])
                    S.op("dve", lambda e: e.tensor_tensor(t_.ap, t_.ap, y.ap, ALU.mult), r=[t_, y], w=[t_])
                    S.op("act", lambda e: e.activation(out=s_.ap, in_=t_.ap, func=AF.Sigmoid, scale=1.5957691216057308), r=[t_], w=[s_])
                    dst = zbm.t[:, :, g2 * 32:(g2 + 1) * 32].rearrange("p i (g h) -> p g i h", h=16)
                    S.op("dve", lambda e, dst=dst: e.tensor_tensor(dst, v4(s_.ap), v4(y.ap), ALU.mult), r=[s_, y], w=[zbm])
                S.barrier()
            with ExitStack() as esd:
                zT = nb("zT", [128, 2, TT], BF16, esd)
                sgl = nb("sgl", [128, 512], F32, esd)
                for ct in range(2):
                    ps = self.pb()
                    psb = ps.ap.bitcast(BF16)
                    for i in range(16):
                        self.tr(ps, psb[:, i * 64:(i + 1) * 64], zbm.t[:, i, ct * 128:(ct + 1) * 128], r=[zbm])
                    S.op("act", lambda e, psb=psb, ct=ct: e.copy(zT.t[:, ct, :].rearrange("p (j i) -> p i j", i=16),
                                                                 psb[:, 0:1024].rearrange("p (i j) -> p i j", j=64)), r=[ps], w=[zT])
                GW = self.lorab[l]
                for ct in range(2):
                    for h in range(2):
                        ps = self.pb()
                        for kt in range(2):
                            self.mm(ps, ps.ap, GW[:, 768 + kt * 256 + ct * 128: 768 + kt * 256 + (ct + 1) * 128], zT.t[:, kt, h * 512:(h + 1) * 512],
                                    r=[self.Blorab[l], zT], start=(kt == 0), stop=(kt == 1))
                        S.op("act", lambda e, ps=ps, ct=ct: e.activation(out=sgl.ap, in_=ps.ap, func=AF.Sigmoid, bias=pc("bglu", ct)), r=[ps, BP], w=[sgl])
                        S.op("dve", lambda e, ct=ct, h=h: e.tensor_tensor(mixT[:, 2 + ct, h * 512:(h + 1) * 512], zT.t[:, ct, h * 512:(h + 1) * 512], sgl.ap, ALU.mult),
                             r=[sgl, zT], w=[Bmix])
                S.barrier()

    def mixer(self, l, tt):
        S = self.S
        gi_pre = (l * 6 + 2) * 8
        gi_post = (l * 6 + 3) * 8
        with ExitStack() as es:
            hTb = self.nb("hT", [128, 8, TT], BF16, es)
            mixb = self.nb("mixT", [128, 8, TT], BF16, es)
            hT = hTb.t
            mixT = mixb.t
            with ExitStack() as es2:
                sq = self.nb("msq", [128, 2, 512], BF16, es2)
                rstd = self.nb("mrstd", [128, 512], F32, es2)
                for h in range(2):
                    ps = self.pb()
                    for d in range(8):
                        S.op("act", lambda e, d=d, h=h: e.activation(out=sq.t[:, d % 2, :], in_=self.xT[d][h].ap, func=AF.Square),
                             r=[self.xT[d][h]], w=[sq])
                        self.mm(ps, ps.ap, self.onesb, sq.t[:, d % 2, :], r=[sq, self.Bcstb], start=(d == 0), stop=(d == 7))
                    self.rstd_from_ps(ps, rstd)
                    for d in range(8):
                        S.op("dve", lambda e, d=d, h=h: e.scalar_tensor_tensor(
                            out=hT[:, d, h * 512:(h + 1) * 512], in0=self.xT[d][h].ap, scalar=self.gain[:, gi_pre + d: gi_pre + d + 1],
                            in1=rstd.ap, op0=ALU.mult, op1=ALU.mult), r=[self.xT[d][h], rstd, self.Bgain], w=[hTb])
                S.barrier()
            if not all(self.mix_enable) or self.cut:
                S.op("pool", lambda e: e.memset(mixb.ap, 0.0), w=[mixb])
            try:
                if self.mix_enable[0]:
                    self.rwkv(l, tt, hT, hTb, mixT, mixb)
                if self.mix_enable[1]:
                    self.s5(l, tt, hT, hTb, mixT, mixb)
                if self.mix_enable[2]:
                    self.gdn(l, tt, hT, hTb, mixT, mixb)
            except _Cut:
                pass
            if self.dbg_mix:
                S.dma("dbg", self.dbg_d, mixT[:, :, :].rearrange("p a b -> p (a b)"), r=[mixb])
            with ExitStack() as es2:
                ff = self.nb("mff", [128, 8, TT], F32, es2)
                sq = self.nb("msq2", [128, 2, 512], BF16, es2)
                rstd = [self.nb(f"mrstd2{h}", [128, 512], F32, es2) for h in range(2)]
                tmp = [self.nb(f"mtmp{i}", [128, 512], F32, es2) for i in range(2)]
                pss = [self.pb(), self.pb()]
                k = 0
                for m in range(8):
                    slot = self.ring_load(self.wout_s[l * 8 + m], 1024)
                    wt = self.wring_t[slot]
                    for h in range(2):
                        pp = self.pb()
                        while pp in pss:
                            pp = self.pb()
                        for ct in range(8):
                            self.mm(pp, pp.ap, wt[:, ct * 128:(ct + 1) * 128], mixT[:, ct, h * 512:(h + 1) * 512],
                                    r=[self.wring[slot], mixb], start=(ct == 0), stop=(ct == 7))
                        S.op("act", lambda e, m=m, h=h, pp=pp: e.copy(ff.t[:, m, h * 512:(h + 1) * 512], pp.ap), r=[pp], w=[ff])
                        S.op("act", lambda e, pp=pp, k=k: e.activation(out=sq.t[:, k % 2, :], in_=pp.ap, func=AF.Square), r=[pp], w=[sq])
                        self.mm(pss[h], pss[h].ap, self.onesb, sq.t[:, k % 2, :], r=[sq, self.Bcstb], start=(m == 0), stop=(m == 7))
                        k += 1
                for h in range(2):
                    self.rstd_from_ps(pss[h], rstd[h])
                    for d in range(8):
                        tB = tmp[d % 2]
                        S.op("dve", lambda e, d=d, h=h, tB=tB: e.scalar_tensor_tensor(
                            out=tB.ap, in0=ff.t[:, d, h * 512:(h + 1) * 512], scalar=self.gain[:, gi_post + d: gi_post + d + 1],
                            in1=rstd[h].ap, op0=ALU.mult, op1=ALU.mult), r=[ff, rstd[h], self.Bgain], w=[tB])
                        S.op("pool", lambda e, d=d, h=h, tB=tB: e.tensor_tensor(self.xT[d][h].ap, self.xT[d][h].ap, tB.ap, ALU.add),
                             r=[tB, self.xT[d][h]], w=[self.xT[d][h]])
                S.barrier()

    def mixer_setup(self):
        S = self.S
        nc = self.nc
        dram = lambda n, s, dt=F32, kind="ExternalInput": nc.dram_tensor(n, s, dt, kind=kind).ap()
        self.win_d = dram("w_in", [DEPTH, D, 3336])
        self.wvres_d = dram("w_vres", [D, 32])
        self.wout_d = dram("w_out", [DEPTH, D, D])
        self.par_d = dram("par", [DEPTH, 128, NPAR])
        self.lora_d = dram("lora", [DEPTH, 128, 1280])
        self.s5p_d = dram("s5p", [DEPTH, 64, 1328])
        self.win_s = dram("win_s", [DEPTH * NCH, 128, 1024], BF16, kind="Internal")
        self.wout_s = dram("wout_s", [DEPTH * 8, 128, 1024], BF16, kind="Internal")
        self.s5tb_s = dram("s5tb_s", [DEPTH, 128, 20480], BF16, kind="ExternalOutput" if self.dbg_mix else "Internal")
        self.s5tf_s = dram("s5tf_s", [DEPTH, 64, 3136], F32, kind="ExternalOutput" if self.dbg_mix else "Internal")
        if self.dbg_mix:
            self.dbg_d = dram("dbg", [128, 8 * TT], BF16, kind="ExternalOutput")
        self.par, self.Bpar, self.lorab, self.Blorab = [], [], [], []
        self.rw_Sb, self.rw_Sf, self.rw_halo = [], [], []
        self.gd_Sb, self.gd_Sf, self.gd_halo = [], [], []
        self.s5_Zc, self.s5_Xc, self.s5d = [], [], []
        self.vfirst = self.nb("vfirst", [128, 2, TT], BF16)
        self.rw_gc = self.nb("rw_gc", [128, 2, 4])
        pars, lbs = [], []
        for l in range(DEPTH):
            p = self.nb(f"par{l}", [128, NPAR])
            lb = self.nb(f"lorab{l}", [128, 1280], BF16)
            pars.append(p)
            lbs.append(lb)
            self.par.append(p.t)
            self.Bpar.append(p)
            self.lorab.append(lb.t)
            self.Blorab.append(lb)
            for (lstv, nm, shp, dt) in ((self.rw_Sb, "rwSb", [128, 2, 128], BF16), (self.rw_Sf, "rwSf", [128, 2, 128], F32),
                                        (self.rw_halo, "rwhalo", [128, 8], F32),
                                        (self.gd_Sb, "gdSb", [128, 4, 128], BF16), (self.gd_Sf, "gdSf", [128, 4, 128], F32),
                                        (self.gd_halo, "gdhalo", [128, 12, 3], BF16),
                                        (self.s5_Zc, "s5Zc", [64, 2, 16], F32), (self.s5_Xc, "s5Xc", [64, 2, 16], BF16)):
                b_ = self.nb(f"{nm}{l}", shp, dt)
                S.op("pool", lambda e, b_=b_: e.memset(b_.ap, 0.0), w=[b_])
                lstv.append(b_)
            d = self.nb(f"s5d{l}", [64, 256])
            S.dma("par", d.ap, self.s5p_d[l][:, 1072:1328], w=[d])
            self.s5d.append(d)
        with ExitStack() as es:
            lst = self.nb("lora_stage", [128, 1280], F32, es)
            for l in range(DEPTH):
                p = pars[l]
                lb = lbs[l]
                S.dma("par", p.ap, self.par_d[l], w=[p])
                pc = lambda name, i=0, n=1, p=p: p.t[:, PO[name] + i: PO[name] + i + n]
                S.op("dve", lambda e: e.tensor_scalar(pc("omk", 0, 2), pc("ka", 0, 2), -1.0, 1.0, ALU.mult, ALU.add), r=[p], w=[p])
                S.op("act", lambda e: e.activation(out=pc("nA", 0, 4), in_=pc("nA", 0, 4), func=AF.Exp), r=[p], w=[p])
                S.op("dve", lambda e: e.tensor_scalar(pc("nA", 0, 4), pc("nA", 0, 4), -1.0, None, ALU.mult), r=[p], w=[p])
                S.dma("par", lst.ap, self.lora_d[l], w=[lst])
                S.op("dve", lambda e: e.tensor_copy(lb.ap, lst.ap), r=[lst], w=[lb])
            S.barrier()
        for l in range(DEPTH):
            self.s5_setup(l)

    def extra_convert_jobs(self):
        jobs = []
        for l in range(DEPTH):
            for c, (c0, n) in enumerate(CHUNKS):
                if c == 27:
                    if l == 0:
                        continue
                    src = self.wvres_d[:, 0:32].rearrange("(dt p) c -> p dt c", p=128)
                else:
                    src = self.win_d[l][:, c0:c0 + n].rearrange("(dt p) c -> p dt c", p=128)
                dst = self.win_s[l * NCH + c][:, 0:8 * n]
                jobs.append((src, dst, 8, n))
            for m in range(8):
                src = self.wout_d[l][:, m * 128:(m + 1) * 128].rearrange("(ct p) d -> p ct d", p=128)
                jobs.append((src, self.wout_s[l * 8 + m], 8, 128))
        return jobs


def _pp(v, nt):
    return np.asarray(v, np.float32).reshape(nt, 128).T


def _shared_inputs(inputs):
    I = {k: np.asarray(v, np.float32) for k, v in inputs.items() if k != "x"}
    g = I["norm_gain"]
    gain = np.ascontiguousarray(g.reshape(DEPTH * 6, 8, 128).transpose(2, 0, 1).reshape(128, DEPTH * 6 * 8))
    par = np.zeros((DEPTH, 128, NPAR), np.float32)
    lora = np.zeros((DEPTH, 128, 1280), np.float32)
    s5p = np.zeros((DEPTH, 64, 1328), np.float32)
    for l in range(DEPTH):
        P = par[l]
        P[:, PO["mu"]:PO["mu"] + 8] = _pp(I["rwkv_mu"][l], 8)
        P[:, PO["w0"]:PO["w0"] + 2] = _pp(I["rwkv_w0"][l], 2)
        P[:, PO["a0"]:PO["a0"] + 2] = _pp(I["rwkv_a0"][l], 2)
        P[:, PO["kk"]:PO["kk"] + 2] = _pp(I["rwkv_k_k"][l], 2)
        P[:, PO["ka"]:PO["ka"] + 2] = _pp(I["rwkv_k_a"][l], 2)
        P[:, PO["rk"]:PO["rk"] + 2] = _pp(I["rwkv_r_k"][l].reshape(-1), 2)
        P[:, PO["lnw"]:PO["lnw"] + 2] = _pp(I["rwkv_ln_w"][l], 2)
        P[:, PO["lnb"]:PO["lnb"] + 2] = _pp(I["rwkv_ln_b"][l], 2)
        if l > 0:
            P[:, PO["v0"]:PO["v0"] + 2] = _pp(I["rwkv_v0"][l - 1], 2)
        cw = I["gdn_conv_w"][l]
        P[:, PO["conv"]:PO["conv"] + 48] = cw.reshape(4, 12, 128).transpose(2, 1, 0).reshape(128, 48)
        P[:, PO["gnw"]] = I["gdn_norm_w"][l]
        P[:, PO["nA"]:PO["nA"] + 4] = np.broadcast_to(I["gdn_a_log"][l][None, :], (128, 4))
        P[:, PO["dtb"]:PO["dtb"] + 4] = np.broadcast_to(I["gdn_dt_bias"][l][None, :], (128, 4))
        P[:, PO["bglu"]:PO["bglu"] + 2] = _pp(I["s5_b_glu"][l], 2)
        L = lora[l]
        L[0:64, 0:256] = I["rwkv_w_w2"][l]
        L[64:128, 0:256] = I["rwkv_w_a2"][l]
        L[:, 256:512] = I["rwkv_w_g2"][l]
        if l > 0:
            L[0:32, 512:768] = I["rwkv_w_v2"][l - 1]
        L[:, 768:1280] = I["s5_w_glu"][l].reshape(2, 128, 256).transpose(1, 0, 2).reshape(128, 512)
        s = s5p[l]
        s[:, 0:16] = I["s5_a_re"][l].T
        s[:, 16:32] = I["s5_a_im"][l].T
        s[:, 32:48] = np.broadcast_to(I["s5_log_dt"][l][None, :], (64, 16))
        s[:, 48:304] = I["s5_b_re"][l].transpose(1, 0, 2).reshape(64, 256)
        s[:, 304:560] = I["s5_b_im"][l].transpose(1, 0, 2).reshape(64, 256)
        s[:, 560:816] = I["s5_c_re"][l].transpose(2, 0, 1).reshape(64, 256)
        s[:, 816:1072] = I["s5_c_im"][l].transpose(2, 0, 1).reshape(64, 256)
        s[:, 1072:1328] = np.broadcast_to(I["s5_d"][l][None, :], (64, 256))
    return {
        "gain": gain,
        "w1": I["ffn_w1"].reshape(DEPTH * 2, D, DFF),
        "w3": I["ffn_w3"].reshape(DEPTH * 2, D, DFF),
        "w2": I["ffn_w2"].reshape(DEPTH * 2, DFF, D),
        "consts": _consts(),
        "w_in": I["w_in"], "w_vres": I["w_in_vres"][0], "w_out": I["w_out"],
        "par": par, "lora": lora, "s5p": s5p,
    }


def _host_inputs(inputs, b, shared):
    m = dict(shared)
    m["x"] = np.ascontiguousarray(np.asarray(inputs["x"][b], np.float32))
    return m


def run(inputs, n_cores=8, stop=None, ntt=NTT, mix_enable=(1, 1, 1), dbg_mix=False, ret_all=False, cut=None):
    kd = K(stop=stop, ntt=ntt, mix_enable=mix_enable, dbg_mix=dbg_mix)
    kd.cut = cut
    kd.build()
    needed = kd.S.needed
    kd.es.close()
    kb = K(stop=stop, ntt=ntt, mix_enable=mix_enable, dbg_mix=dbg_mix, needed=needed)
    kb.cut = cut
    nc = kb.build()
    print("sem incs:", len(needed), flush=True)
    print("instructions:", kb.S.ninst, flush=True)
    shared = _shared_inputs(inputs)
    in_maps = [_host_inputs(inputs, b, shared) for b in range(n_cores)]
    res = run_bass_kernel_spmd(nc, in_maps, core_ids=list(range(n_cores)))
    if ret_all:
        return res.results
    return np.stack([np.asarray(r["y"]) for r in res.results], axis=0)


def kernel(**inputs):
    out = run(inputs, 8)
    return out.astype(np.float32)
```
